# Optimizing a Trainium2 kernel written in Bass

```python
import jax, jax.numpy as jnp
from jax import lax
import numpy as np

D_MODEL = 1024
BATCH = 4
SEQ = 4096
DEPTH = 4

CHUNK = 64
N_MEM = 256
BRANCH_WIDTH = 512
N_BRANCH = 3
LRU_WIDTH = BRANCH_WIDTH
LRU_BLOCKS = 8
LRU_BLOCK = LRU_WIDTH // LRU_BLOCKS
CONV_WIDTH = 4
LRU_C = 8.0
POOL_WIDTH = BRANCH_WIDTH
POOL_WINDOWS = (2, 4, 8, 16)
POOL_GROUPS = len(POOL_WINDOWS)
POOL_GROUP = POOL_WIDTH // POOL_GROUPS
ATT_HEADS = 8
ATT_HEAD_DIM = 64
ATT_WIDTH = ATT_HEADS * ATT_HEAD_DIM
IDX_HEADS = 8
IDX_DIM = 64
MAX_TOPK = 256
Q_BLOCK = 128
MEM_HEADS = 4
MEM_HEAD_DIM = 128
MEM_WIDTH = MEM_HEADS * MEM_HEAD_DIM
D_FF = ((8 * D_MODEL // 3 + 255) // 256) * 256
RMS_EPS = 1e-6

IN_SIZES = (LRU_WIDTH,
            LRU_WIDTH,
            POOL_WIDTH,
            ATT_WIDTH,
            ATT_HEAD_DIM,
            ATT_HEAD_DIM,
            IDX_HEADS * IDX_DIM,
            IDX_DIM,
            IDX_HEADS,
            N_BRANCH * D_MODEL)
D_IN = sum(IN_SIZES)

kernel_name = 'hybrid_rglru_pool_dsa_streaming_block'


def rms_norm(x, g):
    xf = x.astype(jnp.float32)
    y = xf * lax.rsqrt(jnp.mean(xf * xf, axis=-1, keepdims=True) + RMS_EPS)
    return (y * g.astype(jnp.float32)).astype(x.dtype)


def split_columns(h, sizes):
    parts, start = [], 0
    for n in sizes:
        parts.append(h[..., start:start + n])
        start += n
    return parts


def causal_depthwise_conv(x, w, b):
    c = x.shape[-1]
    y = lax.conv_general_dilated(x, w[:, None, :].astype(x.dtype), window_strides=(1,),
                                 padding=[(w.shape[0] - 1, 0)],
                                 dimension_numbers=('NWC', 'WIO', 'NWC'),
                                 feature_group_count=c)
    return y + b


def rg_lru(x, w_a, b_a, w_x, b_x, a_param):
    bsz, s, _ = x.shape
    xb = x.reshape(bsz, s, LRU_BLOCKS, LRU_BLOCK)
    r = jax.nn.sigmoid((jnp.einsum('bshi,hij->bshj', xb, w_a).reshape(bsz, s, LRU_WIDTH) + b_a).astype(jnp.float32))
    i = jax.nn.sigmoid((jnp.einsum('bshi,hij->bshj', xb, w_x).reshape(bsz, s, LRU_WIDTH) + b_x).astype(jnp.float32))
    log_a = -LRU_C * r * jax.nn.softplus(-a_param.astype(jnp.float32))
    a = jnp.exp(log_a)
    mult = jnp.sqrt(jnp.maximum(1.0 - jnp.exp(2.0 * log_a), 0.0))
    u = x.astype(jnp.float32) * i * mult

    def combine(left, right):
        return left[0] * right[0], right[0] * left[1] + right[1]

    _, h = lax.associative_scan(combine, (a, u), axis=1)
    return h.astype(x.dtype)


def multiscale_pool(p, w_pool, pool_scale):
    bsz, s, _ = p.shape
    pf = p.astype(jnp.float32).reshape(bsz, s, POOL_GROUPS, POOL_GROUP)
    cs = jnp.pad(jnp.cumsum(pf, axis=1), ((0, 0), (1, 0), (0, 0), (0, 0)))
    t = jnp.arange(s)
    means = []
    for g, w in enumerate(POOL_WINDOWS):
        start = jnp.maximum(t + 1 - w, 0)
        win_sum = cs[:, 1:, g] - cs[:, start, g]
        count = (t + 1 - start).astype(jnp.float32)[None, :, None]
        means.append(win_sum / count)
    pooled = jnp.stack(means, axis=2) - pf
    mixed = jnp.einsum('bsgc,gcd->bsgd', pooled, w_pool.astype(jnp.float32)).reshape(bsz, s, POOL_WIDTH)
    return (mixed * pool_scale.astype(jnp.float32)).astype(p.dtype)


def dsa_attention(q, k, v, q_idx, k_idx, w_idx):
    bsz, s = q.shape[:2]
    top_k = min(MAX_TOPK, s // 4)
    nb = s // Q_BLOCK
    key_chunk = jnp.arange(s) // CHUNK
    k_idx_f = k_idx.astype(jnp.float32)

    def to_blocks(a):
        return jnp.moveaxis(a.reshape(bsz, nb, Q_BLOCK, *a.shape[2:]), 1, 0)

    def one_block(args):
        qb, qib, wb, pos = args
        q_chunk = pos // CHUNK
        admissible = key_chunk[None, :] <= q_chunk[:, None]
        logits = jnp.einsum('bqhd,bsd->bqhs', qib.astype(jnp.float32), k_idx_f) * (IDX_DIM ** -0.5)
        score = jnp.einsum('bqh,bqhs->bqs', wb.astype(jnp.float32) * (IDX_HEADS ** -0.5), jax.nn.relu(logits))
        score = jnp.where(admissible[None], score, -jnp.inf)
        _, sel = lax.top_k(score, top_k)
        kg = jax.vmap(lambda kk, ii: kk[ii])(k, sel)
        vg = jax.vmap(lambda vv, ii: vv[ii])(v, sel)
        valid = key_chunk[sel] <= q_chunk[None, :, None]
        att = jnp.einsum('bqhd,bqkd->bqhk', qb, kg).astype(jnp.float32) * (ATT_HEAD_DIM ** -0.5)
        att = jnp.where(valid[:, :, None, :], att, -jnp.inf)
        prob = jax.nn.softmax(att, axis=-1).astype(vg.dtype)
        return jnp.einsum('bqhk,bqkd->bqhd', prob, vg)

    q_pos = jnp.arange(s).reshape(nb, Q_BLOCK)
    out = lax.map(one_block, (to_blocks(q), to_blocks(q_idx), to_blocks(w_idx), q_pos))
    return jnp.moveaxis(out, 0, 1).reshape(bsz, s, ATT_WIDTH)


def memory_cross_attention(h, m, w_q, w_kv, w_o):
    bsz, s, _ = h.shape
    q = (h @ w_q).reshape(bsz, s, MEM_HEADS, MEM_HEAD_DIM)
    k, v = jnp.split(m @ w_kv, 2, axis=-1)
    k = k.reshape(bsz, -1, MEM_HEADS, MEM_HEAD_DIM)
    v = v.reshape(bsz, -1, MEM_HEADS, MEM_HEAD_DIM)
    att = jnp.einsum('bshd,bmhd->bhsm', q, k).astype(jnp.float32) * (MEM_HEAD_DIM ** -0.5)
    prob = jax.nn.softmax(att, axis=-1).astype(v.dtype)
    o = jnp.einsum('bhsm,bmhd->bshd', prob, v).reshape(bsz, s, MEM_WIDTH)
    return o @ w_o


def setup_inputs(seed: int = 0) -> dict:
    key = jax.random.key(seed)
    ks = iter(jax.random.split(key, 40))

    def nrm(shape, scale):
        return jax.random.normal(next(ks), shape, jnp.float32) * scale

    def gain(shape):
        return 1.0 + nrm(shape, 0.02)

    L = DEPTH
    u = jax.random.uniform(next(ks), (L, LRU_WIDTH), jnp.float32, 0.9, 0.999)
    a0 = u ** (1.0 / LRU_C)
    lru_a_param = jnp.log(a0) - jnp.log1p(-a0)
    return {
        'x': nrm((BATCH, SEQ, D_MODEL), 1.0),
        'mem': nrm((BATCH, N_MEM, D_MODEL), 1.0),
        'g_mix_pre': gain((L, D_MODEL)),
        'w_in': nrm((L, D_MODEL, D_IN), D_MODEL ** -0.5),
        'conv_w': nrm((L, CONV_WIDTH, LRU_WIDTH), CONV_WIDTH ** -0.5),
        'conv_b': nrm((L, LRU_WIDTH), 0.02),
        'lru_w_a': nrm((L, LRU_BLOCKS, LRU_BLOCK, LRU_BLOCK), LRU_BLOCK ** -0.5),
        'lru_b_a': nrm((L, LRU_WIDTH), 0.02),
        'lru_w_x': nrm((L, LRU_BLOCKS, LRU_BLOCK, LRU_BLOCK), LRU_BLOCK ** -0.5),
        'lru_b_x': nrm((L, LRU_WIDTH), 0.02),
        'lru_a_param': lru_a_param,
        'w_pool': nrm((L, POOL_GROUPS, POOL_GROUP, POOL_GROUP), POOL_GROUP ** -0.5),
        'pool_scale': gain((L, POOL_WIDTH)),
        'w_branch': nrm((L, N_BRANCH, BRANCH_WIDTH, D_MODEL), BRANCH_WIDTH ** -0.5),
        'b_gate': nrm((L, N_BRANCH, D_MODEL), 0.02),
        'w_out': nrm((L, D_MODEL, D_MODEL), D_MODEL ** -0.5),
        'g_mix_post': gain((L, D_MODEL)),
        'g_mem_pre': gain((L, D_MODEL)),
        'g_mem_kv': gain((L, D_MODEL)),
        'w_mem_q': nrm((L, D_MODEL, MEM_WIDTH), D_MODEL ** -0.5),
        'w_mem_kv': nrm((L, D_MODEL, 2 * MEM_WIDTH), D_MODEL ** -0.5),
        'w_mem_o': nrm((L, MEM_WIDTH, D_MODEL), MEM_WIDTH ** -0.5),
        'g_mem_post': gain((L, D_MODEL)),
        'g_ffn_pre': gain((L, D_MODEL)),
        'w_ffn_in': nrm((L, D_MODEL, 2 * D_FF), D_MODEL ** -0.5),
        'w_ffn_out': nrm((L, D_FF, D_MODEL), D_FF ** -0.5),
        'g_ffn_post': gain((L, D_MODEL)),
    }


def reference(x, mem, g_mix_pre, w_in, conv_w, conv_b, lru_w_a, lru_b_a, lru_w_x, lru_b_x,
              lru_a_param, w_pool, pool_scale, w_branch, b_gate, w_out, g_mix_post,
              g_mem_pre, g_mem_kv, w_mem_q, w_mem_kv, w_mem_o, g_mem_post,
              g_ffn_pre, w_ffn_in, w_ffn_out, g_ffn_post):
    bsz, s, _ = x.shape
    for l in range(DEPTH):
        h = rms_norm(x, g_mix_pre[l])
        (lru_x, lru_gate, pool_in, q, k, v, q_idx, k_idx, w_idx,
         gate_logits) = split_columns(h @ w_in[l], IN_SIZES)
        ua = causal_depthwise_conv(lru_x, conv_w[l], conv_b[l])
        ya = rg_lru(ua, lru_w_a[l], lru_b_a[l], lru_w_x[l], lru_b_x[l], lru_a_param[l]) * jax.nn.gelu(lru_gate)
        yb = multiscale_pool(pool_in, w_pool[l], pool_scale[l])
        yc = dsa_attention(q.reshape(bsz, s, ATT_HEADS, ATT_HEAD_DIM), k, v,
                           q_idx.reshape(bsz, s, IDX_HEADS, IDX_DIM), k_idx, w_idx)
        up = jnp.einsum('bsnc,ncd->bsnd', jnp.stack([ya, yb, yc], axis=2), w_branch[l])
        gates = jax.nn.sigmoid(gate_logits.reshape(bsz, s, N_BRANCH, D_MODEL) + b_gate[l])
        merged = jnp.sum(gates * up, axis=2)
        x = x + rms_norm(merged @ w_out[l], g_mix_post[l])
        h = rms_norm(x, g_mem_pre[l])
        m = rms_norm(mem, g_mem_kv[l])
        x = x + rms_norm(memory_cross_attention(h, m, w_mem_q[l], w_mem_kv[l], w_mem_o[l]), g_mem_post[l])
        h = rms_norm(x, g_ffn_pre[l])
        f_gate, f_up = jnp.split(h @ w_ffn_in[l], 2, axis=-1)
        x = x + rms_norm((jax.nn.silu(f_gate) * f_up) @ w_ffn_out[l], g_ffn_post[l])
    return x
```

```python
import numpy as np
import concourse.bass as bass
import concourse.mybir as mybir
from concourse.bass_utils import run_bass_kernel_spmd

F32 = mybir.dt.float32
BF16 = mybir.dt.bfloat16
AF = mybir.ActivationFunctionType
ALU = mybir.AluOpType
AX = mybir.AxisListType

D = 1024
S = 4096
NB = 4
DEPTH = 4
NMEM = 256
DIN = 5832
DINP = 5888
DFF = 2816
NV = 116
IT = 22
TM = 256
TX = 512
NCST = 128 + 128 + 32 + 16
G_MIX_PRE, G_MIX_POST, G_MEM_PRE, G_MEM_KV, G_MEM_POST, G_FFN_PRE, G_FFN_POST = 0, 8, 16, 24, 32, 40, 48
CONV_W, CONV_B, B_A, B_X, A_PARAM, POOL_SCALE, B_GATE = 56, 72, 76, 80, 84, 88, 92
GATE_BLK0 = 22
RMS_EPS = 1e-6


class Buf:
    __slots__ = ("name", "w", "r", "excl")

    def __init__(self, name="", excl=False):
        self.name = name
        self.w = None
        self.r = {}
        self.excl = excl


class KB:
    def __init__(self, nc):
        self.nc = nc
        self.E = {}
        for nm, obj in (("pe", nc.tensor), ("act", nc.scalar), ("dve", nc.vector),
                        ("pool", nc.gpsimd), ("sp", nc.sync)):
            self.E[nm] = dict(obj=obj, sem=nc.alloc_semaphore("s_" + nm), n=0, known={})
        self.dsem = [[nc.alloc_semaphore("dq%d" % i), 0] for i in range(32)]
        self.di = 0
        self.ninst = 0

    def _wait(self, eng, sem, val):
        E = self.E[eng]
        if E["known"].get(sem.num, 0) < val:
            E["obj"].wait_ge(sem, val)
            E["known"][sem.num] = val

    def _deps(self, eng, r, w):
        deps = {}

        def add(ev):
            if ev is None:
                return
            sem, val, src = ev
            if src == "pe" and eng == "pe":
                return
            if sem.num not in deps or deps[sem.num][1] < val:
                deps[sem.num] = (sem, val)
        for b in r:
            add(b.w)
        for b in w:
            add(b.w)
            for ev in b.r.values():
                add(ev)
        for sem, val in deps.values():
            self._wait(eng, sem, val)

    @staticmethod
    def _mark(ev, r, w):
        for b in r:
            b.r[ev[0].num] = ev
        for b in w:
            b.w = ev
            b.r = {}

    def op(self, eng, fn, r=(), w=()):
        w = list(w) + [b for b in r if b.excl]
        r = [b for b in r if not b.excl]
        self._deps(eng, r, w)
        E = self.E[eng]
        ins = fn(E["obj"])
        E["n"] += 1
        ins.then_inc(E["sem"], 1)
        ev = (E["sem"], E["n"], eng)
        self._mark(ev, r, w)
        self.ninst += 1
        return ev

    def dma(self, q, out, in_, r=(), w=()):
        slot = self.dsem[self.di % len(self.dsem)]
        self.di += 1
        sem, cnt = slot
        if cnt > 0:
            self._wait(q, sem, 16 * cnt)
        self._deps(q, r, w)
        ins = self.E[q]["obj"].dma_start(out=out, in_=in_)
        ins.then_inc(sem, 16)
        slot[1] = cnt + 1
        ev = (sem, 16 * (cnt + 1), "dma")
        self._mark(ev, r, w)
        self.ninst += 1
        return ev

    def barrier(self):
        for a in self.E:
            for b in self.E:
                if a != b and self.E[b]["n"] > 0:
                    self._wait(a, self.E[b]["sem"], self.E[b]["n"])
            for sem, cnt in self.dsem:
                if cnt > 0:
                    self._wait(a, sem, 16 * cnt)


class Ring:
    def __init__(self, aps, name):
        self.aps = aps
        self.bufs = [Buf("%s%d" % (name, i)) for i in range(len(aps))]
        self.i = 0

    def next(self):
        j = self.i % len(self.aps)
        self.i += 1
        return self.aps[j], self.bufs[j]


class _Stop(Exception):
    pass


def build_nc(n_layers=DEPTH, dbg=False, stop=None):
    nc = bass.Bass("TRN2", target_bir_lowering=False)
    L = DEPTH
    LW = n_layers

    def din(name, shape):
        return nc.dram_tensor(name, list(shape), F32, kind="ExternalInput").ap()

    xT_d = din("xT", [D, S])
    memT_d = din("memT", [D, NMEM])
    vecs_d = din("vecs", [128, L * NV])
    cst_d = din("cst", [128, NCST])
    w_in_d = din("w_in", [LW, D, DINP])
    bd_d = din("lru_bd", [LW, 128, 1024])
    wpool_d = din("w_pool", [LW, 128, 512])
    w_branch_d = din("w_branch", [LW, 3, 512, D])
    w_out_d = din("w_out", [LW, D, D])
    w_mq_d = din("w_mem_q", [LW, D, 512])
    w_mkv_d = din("w_mem_kv", [LW, D, 1024])
    w_mo_d = din("w_mem_o", [LW, 512, D])
    w_fi_d = din("w_ffn_in", [LW, D, 2 * DFF])
    w_fo_d = din("w_ffn_out", [LW, DFF, D])
    yT_d = nc.dram_tensor("yT", [D, S], F32, kind="ExternalOutput").ap()
    taps = {}

    k = KB(nc)

    def sb(name, shape, dt):
        return nc.alloc_sbuf_tensor(name, list(shape), dt)

    xT = sb("xTs", [128, 8, S], F32)
    xb = [[Buf("x%d_%d" % (c, t)) for t in range(S // TM)] for c in range(8)]

    def xbufs(c, t0, T):
        return [xb[c][t] for t in range(t0 // TM, (t0 + T) // TM)]

    KK = sb("KK", [128, S], BF16)
    KKb = [Buf("KK%d" % i) for i in range(S // TM)]
    V65 = sb("V65", [128, 32, 66], BF16)
    V65b = [Buf("V%d" % i) for i in range(32)]
    vec = sb("vec", [128, L * NV], F32)
    vecb = Buf("vec")
    cst = sb("cstf", [128, NCST], F32)
    cstb = Buf("cst")
    ident = sb("ident", [128, 128], BF16)
    identrep = sb("identrep", [128, 8, 128], BF16)
    ones = sb("ones", [128, 128], BF16)
    constb = Buf("consts")
    nsp8 = sb("nsp8", [128, L * 4], F32)
    nsp8b = Buf("nsp8")
    bd = sb("bd", [128, 1024], BF16)
    bdb = Buf("bd")
    wpool = sb("wpool", [128, 512], BF16)
    wpoolb = Buf("wpool")
    lruhalo = sb("lruhalo", [128, 4, 4], F32)
    poolhalo = sb("poolhalo", [128, 4, 16], F32)
    carry = sb("carry", [128, 4], F32)
    halob = [Buf("halo%d" % g) for g in range(4)]
    phalob = [Buf("phalo%d" % g) for g in range(4)]
    carryb = [Buf("carry%d" % g) for g in range(4)]
    small = sb("small", [128, 64], F32)
    ringt = sb("ring", [128, 3, 8, 128], BF16)
    ring = Ring([ringt[:, i] for i in range(3)], "ring")
    wvw = sb("wvw", [128, 8, 72], BF16)
    wvwb = Buf("wvw")
    ARENA = 47104
    arena = sb("arena", [128, ARENA // 4], F32)

    def carve(off, shape, dt):
        n = int(np.prod(shape))
        sz = 4 if dt == F32 else 2
        assert off % 4 == 0 and off + n * sz <= ARENA, (off, shape)
        a = arena[:, off // 4:(off + n * sz + 3) // 4]
        if dt != F32:
            a = a.bitcast(dt)
        a = a[:, 0:n]
        if len(shape) == 2:
            a = a.rearrange("p (a b) -> p a b", a=shape[0])
        elif len(shape) == 3:
            a = a.rearrange("p (a b c) -> p a b c", a=shape[0], b=shape[1])
        return a

    ps = [nc.alloc_psum_tensor("ps%d" % i, [128, 512], F32) for i in range(8)]
    psb = [Buf("ps%d" % i, excl=True) for i in range(8)]
    psrot = {"ids": list(range(8)), "i": 0}

    def next_ps():
        ids = psrot["ids"]
        j = ids[psrot["i"] % len(ids)]
        psrot["i"] += 1
        return ps[j], psb[j]

    SM_LO, SM_HI, SM_W0, SM_MID, SM_CNT, SM_TMP, SM_THR = 0, 1, 2, 3, 4, 5, 6
    SM_HW = 8
    SM_REC = 40
    SM_WIDX = 48
    smb = Buf("small")
    recb = Buf("rec")
    widxb = [Buf("widx0"), Buf("widx1")]

    k.dma("sp", vec[:, :], vecs_d[:, :], w=[vecb])
    k.dma("sp", cst[:, :], cst_d[:, :], w=[cstb])
    k.op("dve", lambda e: e.tensor_copy(out=ident[:, :], in_=cst[:, 0:128]), r=[cstb], w=[constb])
    for h in range(8):
        k.op("dve", lambda e: e.tensor_copy(out=identrep[:, h, :], in_=cst[:, 0:128]), r=[cstb], w=[constb])
    k.op("dve", lambda e: e.memset(ones[:, :], 1.0), w=[constb])
    k.op("dve", lambda e: e.memset(V65[:, :, :], 1.0), w=V65b)
    for c in range(8):
        k.dma("sp", xT[:, c, :], xT_d[c * 128:(c + 1) * 128, :], w=xb[c])
    for l in range(L):
        k.op("act", lambda e: e.activation(out=nsp8[:, l * 4:l * 4 + 4],
                                           in_=vec[:, l * NV + A_PARAM:l * NV + A_PARAM + 4],
                                           func=AF.Exp, scale=-1.0), r=[vecb], w=[nsp8b])
    k.op("act", lambda e: e.activation(out=nsp8[:, :], in_=nsp8[:, :], func=AF.Ln, bias=1.0),
         r=[nsp8b], w=[nsp8b])
    k.op("dve", lambda e: e.tensor_scalar(out=nsp8[:, :], in0=nsp8[:, :], scalar1=-8.0, scalar2=None,
                                          op0=ALU.mult), r=[nsp8b], w=[nsp8b])

    cmask = cst[:, 128:256]
    pow2 = cst[:, 256:256 + IT + 1]
    invcnt = cst[:, 288:304]

    def tap(name, ap, bufs):
        if not dbg:
            return
        t = nc.dram_tensor("tap_" + name, list(ap.shape), ap.dtype, kind="ExternalOutput").ap()
        taps[name] = t
        k.dma("sp", t, ap, r=bufs)

    def load_w(scr, blk, kc):
        ap, buf = ring.next()
        k.dma("sp", ap[:, 0:kc, :], scr[blk][:, 0:kc, :], w=[buf])
        return ap, buf

    def wview(w2d):
        return w2d.rearrange("(kc p) n -> p kc n", p=128)

    def mm(out, lhsT, rhs, start, stop, r, w, skip=False):
        if skip:
            k.op("pe", lambda e: e.matmul(out, lhsT, rhs, start=start, stop=stop, skip_group_check=True),
                 r=r, w=w)
        else:
            k.op("pe", lambda e: e.matmul(out, lhsT, rhs, start=start, stop=stop), r=r, w=w)

    def rms_rstd(srcs, T, sqring, rstd, rstdb):
        pa, pb = next_ps()
        n = len(srcs)
        for c, (ap, bufs) in enumerate(srcs):
            sq, sqb = sqring.next()
            k.op("act", lambda e: e.activation(out=sq[:, 0:T], in_=ap, func=AF.Square), r=bufs, w=[sqb])
            mm(pa[:, 0:T], ones[:, :], sq[:, 0:T], c == 0, c == n - 1, [sqb, constb], [pb])
        k.op("act", lambda e: e.activation(out=rstd, in_=pa[:, 0:T], func=AF.Sqrt, scale=1.0 / D,
                                           bias=eps_ap), r=[pb, constb], w=[rstdb])
        k.op("dve", lambda e: e.reciprocal(out=rstd, in_=rstd), r=[rstdb], w=[rstdb])

    eps_t = sb("eps", [128, 1], F32)
    eps_ap = eps_t[:, 0:1]
    k.op("dve", lambda e: e.memset(eps_t[:, :], RMS_EPS), w=[constb])

    def pre_norm(l, gbase, t0, T, hT, hTb, sqring, rstd, rstdb):
        srcs = [(xT[:, c, t0:t0 + T], xbufs(c, t0, T)) for c in range(8)]
        rms_rstd(srcs, T, sqring, rstd, rstdb)
        for c in range(8):
            k.op("dve", lambda e: e.scalar_tensor_tensor(
                out=hT[:, c, :], in0=xT[:, c, t0:t0 + T],
                scalar=vec[:, l * NV + gbase + c:l * NV + gbase + c + 1], in1=rstd,
                op0=ALU.mult, op1=ALU.mult), r=xbufs(c, t0, T) + [rstdb, vecb], w=[hTb[c]])

    def post_norm_res(l, gbase, t0, T, ytmp, ytb, sqring, rstd, rstdb):
        srcs = [(ytmp[:, c, :], [ytb[c]]) for c in range(8)]
        rms_rstd(srcs, T, sqring, rstd, rstdb)
        for c in range(8):
            k.op("dve", lambda e: e.scalar_tensor_tensor(
                out=ytmp[:, c, :], in0=ytmp[:, c, :],
                scalar=vec[:, l * NV + gbase + c:l * NV + gbase + c + 1], in1=rstd,
                op0=ALU.mult, op1=ALU.mult), r=[ytb[c], rstdb, vecb], w=[ytb[c]])
            k.op("dve", lambda e: e.tensor_tensor(out=xT[:, c, t0:t0 + T], in0=xT[:, c, t0:t0 + T],
                                                  in1=ytmp[:, c, :], op=ALU.add),
                 r=[ytb[c]] + xbufs(c, t0, T), w=xbufs(c, t0, T))

    def scratch(name, nblk):
        return nc.dram_tensor("scr_" + name, [LW, nblk, 128, 8, 128], BF16, kind="Internal").ap()

    SCR = dict(w_in=scratch("w_in", 46), w_branch=scratch("w_branch", 24), w_out=scratch("w_out", 8),
               w_mq=scratch("w_mq", 4), w_mkv=scratch("w_mkv", 8), w_mo=scratch("w_mo", 8),
               w_fi=scratch("w_fi", 44), w_fo0=scratch("w_fo0", 8), w_fo1=scratch("w_fo1", 8),
               w_fo2=scratch("w_fo2", 8))
    st32 = Ring([arena[:, i * 2048:(i + 1) * 2048].rearrange("p (k c) -> p k c", k=8) for i in range(2)], "st32")
    st16 = Ring([arena[:, 4096 + i * 1024:4096 + (i + 1) * 1024].bitcast(BF16).rearrange("p (k c) -> p k c", k=8)
                 for i in range(2)], "st16")
    cast_rr = {"i": 0}

    def convert(view, scr_l, blk0, kc, ncols):
        for c0 in range(0, ncols, 256):
            nco = min(256, ncols - c0)
            a32, b32 = st32.next()
            k.dma("sp", a32[:, 0:kc, 0:nco], view[:, :, c0:c0 + nco], w=[b32])
            a16, b16 = st16.next()
            eng = ("act", "dve", "pool")[cast_rr["i"] % 3]
            cast_rr["i"] += 1
            if eng == "act":
                k.op("act", lambda e: e.copy(out=a16[:, 0:kc, 0:nco], in_=a32[:, 0:kc, 0:nco]), r=[b32], w=[b16])
            else:
                k.op(eng, lambda e: e.tensor_copy(out=a16[:, 0:kc, 0:nco], in_=a32[:, 0:kc, 0:nco]), r=[b32], w=[b16])
            for b in range(nco // 128):
                k.dma("sp", scr_l[blk0 + c0 // 128 + b][:, 0:kc, :], a16[:, 0:kc, b * 128:(b + 1) * 128], r=[b16])

    for l in range(n_layers):
        convert(wview(w_in_d[l]), SCR["w_in"][l], 0, 8, DINP)
        for n in range(3):
            convert(wview(w_branch_d[l, n]), SCR["w_branch"][l], n * 8, 4, D)
        convert(wview(w_out_d[l]), SCR["w_out"][l], 0, 8, D)
        convert(wview(w_mq_d[l]), SCR["w_mq"][l], 0, 8, 512)
        convert(wview(w_mkv_d[l]), SCR["w_mkv"][l], 0, 8, 1024)
        convert(wview(w_mo_d[l]), SCR["w_mo"][l], 0, 4, D)
        convert(wview(w_fi_d[l]), SCR["w_fi"][l], 0, 8, 2 * DFF)
        wfo_v = wview(w_fo_d[l])
        for part, (k0, kn) in enumerate(((0, 8), (8, 8), (16, 6))):
            convert(wfo_v[:, k0:k0 + kn, :], SCR["w_fo%d" % part][l], 0, kn, D)
    k.barrier()

    chk_cnt = {}

    def chk(tag):
        chk_cnt[tag] = chk_cnt.get(tag, 0) + 1
        if stop is None:
            return
        st, _, n = stop.partition("@")
        if st == tag and chk_cnt[tag] == int(n or 1):
            raise _Stop()


    def _layers(n_layers):
        for l in range(n_layers):
            vb = l * NV
            k.barrier()
            stg, stgb = arena[:, 0:1536], Buf("stg")
            k.dma("sp", stg[:, 0:1024], bd_d[l], w=[stgb])
            k.dma("sp", stg[:, 1024:1536], wpool_d[l], w=[stgb])
            k.barrier()
            k.op("dve", lambda e: e.tensor_copy(out=bd[:, :], in_=stg[:, 0:1024]), r=[stgb], w=[bdb])
            k.op("dve", lambda e: e.tensor_copy(out=wpool[:, :], in_=stg[:, 1024:1536]), r=[stgb], w=[wpoolb])
            k.barrier()
            for g in range(4):
                k.op("dve", lambda e: e.memset(lruhalo[:, g, :], 0.0), w=[halob[g]])
                k.op("dve", lambda e: e.memset(poolhalo[:, g, :], 0.0), w=[phalob[g]])
                k.op("dve", lambda e: e.memset(carry[:, g:g + 1], 0.0), w=[carryb[g]])
            S_in = SCR["w_in"][l]
            chk("setup")
            T = TM

            score = arena[:, 0:S]
            mbuf = arena[:, S:S + S // 2].bitcast(BF16)
            scoreb, mbufb = Buf("score"), Buf("mbuf")
            o = 24576
            qq = carve(o, [8, T], BF16); o += 8 * T * 2
            qqb = [Buf("qq%d" % h) for h in range(8)]
            yab = [carve(o + n * 4 * T * 2, [4, T], BF16) for n in range(3)]; o += 3 * 4 * T * 2
            ybuf = [[Buf("y%d_%d" % (n, g)) for g in range(4)] for n in range(3)]
            o_free = o
            hT = carve(o_free, [8, T], BF16)
            hTb = [Buf("hT%d" % c) for c in range(8)]
            rstd = arena[:, (o_free + 4096) // 4:(o_free + 4096) // 4 + T]
            rstdb = Buf("rstd")
            sq_aps = [arena[:, (o_free + 5120 + i * 512) // 4:(o_free + 5120 + (i + 1) * 512) // 4].bitcast(BF16)
                      for i in range(2)]
            sqring = Ring(sq_aps, "sq")
            GS = 8256

            def gset(s):
                b = s * GS
                d = {}
                d["lrux"] = arena[:, b // 4:b // 4 + T + 4]; b += 1040
                d["graw"] = arena[:, b // 4:b // 4 + T]; b += 1024
                d["t1"] = arena[:, b // 4:b // 4 + T]; b += 1024
                d["ua"] = arena[:, b // 4:b // 4 + T]; b += 1024
                d["ra"] = arena[:, b // 4:b // 4 + T]; b += 1024
                d["ib"] = arena[:, b // 4:b // 4 + T]; b += 1024
                d["mb"] = arena[:, b // 4:b // 4 + T]; b += 1024
                d["uabf"] = arena[:, b // 4:b // 4 + T // 2].bitcast(BF16); b += 512
                d["ge"] = arena[:, b // 4:b // 4 + T // 2].bitcast(BF16); b += 512
                d["bufs"] = {n: Buf(n + str(s)) for n in ("lrux", "graw", "t1", "ua", "ra", "ib", "mb", "uabf", "ge")}
                return d

            def pset(s):
                b = 16576 + s * 3840
                d = {}
                d["px"] = arena[:, b // 4:b // 4 + T + 16]; b += 1088
                d["pa"] = arena[:, b // 4:b // 4 + T + 16]; b += 1088
                d["pb"] = arena[:, b // 4:b // 4 + T + 16]; b += 1088
                d["pooled"] = arena[:, b // 4:b // 4 + T // 2].bitcast(BF16); b += 512
                d["bufs"] = {n: Buf(n + str(s)) for n in ("px", "pa", "pb", "pooled")}
                return d
            gsets = [gset(0), gset(1)]
            psets = [pset(0), pset(1)]
            o2 = o_free
            relu_aps = []
            for i in range(3):
                relu_aps.append(arena[:, o2 // 4:o2 // 4 + 256].bitcast(BF16)); o2 += 1024
            reluring = Ring(relu_aps, "relu")
            pt_aps = []
            for i in range(4):
                pt_aps.append(arena[:, o2 // 4:o2 // 4 + 256].bitcast(BF16)); o2 += 1024
            ptring = Ring(pt_aps, "pt")
            diag = arena[:, o2 // 4:o2 // 4 + 512].bitcast(BF16).rearrange("p (h q) -> p h q", h=8); o2 += 2048
            diagb = Buf("diag")
            yctm = arena[:, o2 // 4:o2 // 4 + 256].bitcast(BF16); o2 += 1024
            yctmb = Buf("yctm")
            assert o2 <= ARENA
            merged = carve(0, [8, T], BF16)
            mergedb = [Buf("mg%d" % c) for c in range(8)]
            ytmpM = carve(4096, [8, T], F32)
            ytbM = [Buf("yt%d" % c) for c in range(8)]
            acc = arena[:, 12288 // 4:12288 // 4 + T]
            accb = Buf("acc")
            gt_aps = [arena[:, (13312 + i * 1024) // 4:(13312 + i * 1024) // 4 + T] for i in range(2)]
            gtring = Ring(gt_aps, "gt")
            tm_aps = [arena[:, (15360 + i * 1024) // 4:(15360 + i * 1024) // 4 + T] for i in range(2)]
            tmring = Ring(tm_aps, "tm")

            for tt in range(S // T):
                t0 = tt * T
                k.barrier()
                psrot["ids"] = list(range(8))
                pre_norm(l, G_MIX_PRE, t0, T, hT, hTb, sqring, rstd, rstdb)

                chk("m1")
                def proj(blk, evac):
                    wap, wb = load_w(S_in, blk, 8)
                    pa, pb = next_ps()
                    for kc in range(8):
                        mm(pa[:, 0:T], wap[:, kc, :], hT[:, kc, :], kc == 0, kc == 7, [wb, hTb[kc]], [pb])
                    evac(pa, pb)

                for g in range(4):
                    gs = gsets[g % 2]
                    gb = gs["bufs"]
                    lrux = gs["lrux"]
                    k.op("act", lambda e: e.copy(out=lrux[:, 0:3], in_=lruhalo[:, g, 0:3]), r=[halob[g]], w=[gb["lrux"]])
                    proj(g, lambda pa, pb: k.op("act", lambda e: e.copy(out=lrux[:, 3:3 + T], in_=pa[:, 0:T]),
                                                r=[pb], w=[gb["lrux"]]))
                    k.op("act", lambda e: e.copy(out=lruhalo[:, g, 0:3], in_=lrux[:, T:T + 3]), r=[gb["lrux"]], w=[halob[g]])
                    proj(4 + g, lambda pa, pb: k.op("act", lambda e: e.copy(out=gs["graw"], in_=pa[:, 0:T]),
                                                    r=[pb], w=[gb["graw"]]))
                    graw, t1 = gs["graw"], gs["t1"]
                    k.op("dve", lambda e: e.tensor_tensor(out=t1, in0=graw, in1=graw, op=ALU.mult), r=[gb["graw"]], w=[gb["t1"]])
                    k.op("dve", lambda e: e.tensor_scalar(out=t1, in0=t1, scalar1=0.044715, scalar2=1.0, op0=ALU.mult, op1=ALU.add),
                         r=[gb["t1"]], w=[gb["t1"]])
                    k.op("dve", lambda e: e.tensor_tensor(out=t1, in0=t1, in1=graw, op=ALU.mult), r=[gb["t1"], gb["graw"]], w=[gb["t1"]])
                    k.op("act", lambda e: e.activation(out=t1, in_=t1, func=AF.Sigmoid, scale=1.5957691216057308),
                         r=[gb["t1"]], w=[gb["t1"]])
                    k.op("dve", lambda e: e.tensor_tensor(out=gs["ge"], in0=t1, in1=graw, op=ALU.mult),
                         r=[gb["t1"], gb["graw"]], w=[gb["ge"]])
                    ua = gs["ua"]
                    cw = lambda kk: vec[:, vb + CONV_W + kk * 4 + g:vb + CONV_W + kk * 4 + g + 1]
                    k.op("act", lambda e: e.activation(out=ua, in_=lrux[:, 0:T], func=AF.Identity, scale=cw(0),
                                                       bias=vec[:, vb + CONV_B + g:vb + CONV_B + g + 1]),
                         r=[gb["lrux"], vecb], w=[gb["ua"]])
                    for kk in (1, 2, 3):
                        k.op("dve", lambda e: e.scalar_tensor_tensor(out=ua, in0=lrux[:, kk:kk + T], scalar=cw(kk), in1=ua,
                                                                     op0=ALU.mult, op1=ALU.add),
                             r=[gb["lrux"], gb["ua"], vecb], w=[gb["ua"]])
                    k.op("act", lambda e: e.copy(out=gs["uabf"], in_=ua), r=[gb["ua"]], w=[gb["uabf"]])
                    for which, dst, bcol in ((0, "ra", B_A), (1, "ib", B_X)):
                        pa, pb = next_ps()
                        mm(pa[:, 0:T], bd[:, (g * 2 + which) * 128:(g * 2 + which + 1) * 128], gs["uabf"], True, True,
                           [bdb, gb["uabf"]], [pb])
                        k.op("act", lambda e: e.activation(out=gs[dst], in_=pa[:, 0:T], func=AF.Sigmoid,
                                                           bias=vec[:, vb + bcol + g:vb + bcol + g + 1]),
                             r=[pb, vecb], w=[gb[dst]])
                    ra, ib, mb = gs["ra"], gs["ib"], gs["mb"]
                    k.op("act", lambda e: e.activation(out=ra, in_=ra, func=AF.Exp, scale=nsp8[:, l * 4 + g:l * 4 + g + 1]),
                         r=[gb["ra"], nsp8b], w=[gb["ra"]])
                    k.op("dve", lambda e: e.tensor_tensor(out=mb, in0=ra, in1=ra, op=ALU.mult), r=[gb["ra"]], w=[gb["mb"]])
                    k.op("dve", lambda e: e.tensor_scalar(out=mb, in0=mb, scalar1=-1.0, scalar2=1.0, op0=ALU.mult, op1=ALU.add),
                         r=[gb["mb"]], w=[gb["mb"]])
                    k.op("dve", lambda e: e.tensor_scalar(out=mb, in0=mb, scalar1=0.0, scalar2=None, op0=ALU.max),
                         r=[gb["mb"]], w=[gb["mb"]])
                    k.op("act", lambda e: e.activation(out=mb, in_=mb, func=AF.Sqrt), r=[gb["mb"]], w=[gb["mb"]])
                    k.op("dve", lambda e: e.tensor_tensor(out=ib, in0=ib, in1=ua, op=ALU.mult), r=[gb["ib"], gb["ua"]], w=[gb["ib"]])
                    k.op("dve", lambda e: e.tensor_tensor(out=ib, in0=ib, in1=mb, op=ALU.mult), r=[gb["ib"], gb["mb"]], w=[gb["ib"]])
                    k.op("dve", lambda e: e.tensor_tensor_scan(out=mb, data0=ra, data1=ib, initial=carry[:, g:g + 1],
                                                               op0=ALU.mult, op1=ALU.add),
                         r=[gb["ra"], gb["ib"], carryb[g]], w=[gb["mb"]])
                    k.op("dve", lambda e: e.tensor_copy(out=carry[:, g:g + 1], in_=mb[:, T - 1:T]), r=[gb["mb"]], w=[carryb[g]])
                    k.op("dve", lambda e: e.tensor_tensor(out=yab[0][:, g, :], in0=mb, in1=gs["ge"], op=ALU.mult),
                         r=[gb["mb"], gb["ge"]], w=[ybuf[0][g]])

                chk("A")
                for g in range(4):
                    pset_ = psets[g % 2]
                    pbf = pset_["bufs"]
                    px, pA, pB, pooled = pset_["px"], pset_["pa"], pset_["pb"], pset_["pooled"]
                    k.op("act", lambda e: e.copy(out=px[:, 0:15], in_=poolhalo[:, g, 0:15]), r=[phalob[g]], w=[pbf["px"]])
                    proj(8 + g, lambda pa, pb: k.op("act", lambda e: e.copy(out=px[:, 15:15 + T], in_=pa[:, 0:T]),
                                                    r=[pb], w=[pbf["px"]]))
                    k.op("act", lambda e: e.copy(out=poolhalo[:, g, 0:15], in_=px[:, T:T + 15]), r=[pbf["px"]], w=[phalob[g]])
                    Wd = T + 15
                    cur, curb = px, pbf["px"]
                    seq = [(pA, pbf["pa"]), (pB, pbf["pb"]), (pA, pbf["pa"]), (pB, pbf["pb"])]
                    sh = 1
                    for step in range(g + 1):
                        dst, dstb = seq[step]
                        k.op("dve", lambda e: e.tensor_tensor(out=dst[:, sh:Wd], in0=cur[:, sh:Wd], in1=cur[:, 0:Wd - sh], op=ALU.add),
                             r=[curb], w=[dstb])
                        cur, curb = dst, dstb
                        sh *= 2
                    wdw = 2 ** (g + 1)
                    k.op("dve", lambda e: e.scalar_tensor_tensor(out=pooled, in0=cur[:, 15:15 + T], scalar=1.0 / wdw,
                                                                 in1=px[:, 15:15 + T], op0=ALU.mult, op1=ALU.subtract),
                         r=[curb, pbf["px"]], w=[pbf["pooled"]])
                    if tt == 0:
                        nfix = wdw - 1
                        tmpf = pB[:, 0:nfix] if cur is not pB else pA[:, 0:nfix]
                        tmpb = pbf["pb"] if cur is not pB else pbf["pa"]
                        k.op("dve", lambda e: e.tensor_tensor(out=tmpf, in0=cur[:, 15:15 + nfix], in1=invcnt[:, 0:nfix], op=ALU.mult),
                             r=[curb, cstb], w=[tmpb])
                        k.op("dve", lambda e: e.tensor_tensor(out=pooled[:, 0:nfix], in0=tmpf, in1=px[:, 15:15 + nfix], op=ALU.subtract),
                             r=[tmpb, pbf["px"]], w=[pbf["pooled"]])
                    pa_, pb_ = next_ps()
                    mm(pa_[:, 0:T], wpool[:, g * 128:(g + 1) * 128], pooled, True, True, [wpoolb, pbf["pooled"]], [pb_])
                    k.op("act", lambda e: e.activation(out=yab[1][:, g, :], in_=pa_[:, 0:T], func=AF.Copy,
                                                       scale=vec[:, vb + POOL_SCALE + g:vb + POOL_SCALE + g + 1]),
                         r=[pb_, vecb], w=[ybuf[1][g]])

                chk("B")
                for h in range(8):
                    proj(12 + h, lambda pa, pb: k.op("act", lambda e: e.copy(out=qq[:, h, :], in_=pa[:, 0:T]),
                                                     r=[pb], w=[qqb[h]]))
                chk("q")
                proj(20, lambda pa, pb: k.op("act", lambda e: e.copy(out=KK[:, t0:t0 + T], in_=pa[:, 0:T]),
                                             r=[pb], w=[KKb[tt]]))
                chk("kk")
                k.dma("sp", wvw[:, :, :], S_in[21][:, :, 0:72], w=[wvwb])
                chk("wvw")
                for i in range(T // 128):
                    Bg = t0 // 128 + i
                    pa, pb = next_ps()
                    for kc in range(8):
                        mm(pa[:, 0:72], hT[:, kc, i * 128:(i + 1) * 128], wvw[:, kc, :], kc == 0, kc == 7,
                           [wvwb, hTb[kc]], [pb])
                    k.op("act", lambda e: e.copy(out=V65[:, Bg, 0:64], in_=pa[:, 0:64]), r=[pb], w=[V65b[Bg]])
                    k.op("act", lambda e: e.activation(out=small[:, SM_WIDX + i * 8:SM_WIDX + i * 8 + 8], in_=pa[:, 64:72],
                                                       func=AF.Copy, scale=float(8 ** -0.5 * 64 ** -0.5)),
                         r=[pb], w=[widxb[i]])

                chk("qkv")
                k.barrier()
                psO = [ps[6], ps[7]]
                psOb = [psb[6], psb[7]]
                for i in range(T // 128):
                    Bq = t0 // 128 + i
                    N = (Bq + 1) * 128
                    qc = slice(i * 128, (i + 1) * 128)
                    for h in range(8):
                        k.op("dve", lambda e: e.tensor_scalar(out=diag[:, h, :], in0=ident[:, :],
                                                              scalar1=small[:, SM_WIDX + i * 8 + h:SM_WIDX + i * 8 + h + 1],
                                                              scalar2=None, op0=ALU.mult),
                             r=[constb, widxb[i]], w=[diagb])
                    psrot["ids"] = [0, 1, 2, 3]
                    for ci, c0 in enumerate(range(0, N, 512)):
                        Wc = min(512, N - c0)
                        kb_ = [KKb[j] for j in range(c0 // TM, (c0 + Wc + TM - 1) // TM)]
                        sca, scb = ps[4 + ci % 2], psb[4 + ci % 2]
                        pend = None
                        for h in range(8):
                            la, lb = next_ps()
                            mm(la[:, 0:Wc], qq[64:128, h, qc], KK[64:128, c0:c0 + Wc], True, True, [qqb[h]] + kb_, [lb])
                            rl, rlb = reluring.next()
                            k.op("act", lambda e: e.activation(out=rl[:, 0:Wc], in_=la[:, 0:Wc], func=AF.Relu), r=[lb], w=[rlb])
                            if pend is not None:
                                ph, prl, prlb = pend
                                mm(sca[:, 0:Wc], diag[:, ph, :], prl[:, 0:Wc], ph == 0, False, [diagb, prlb], [scb])
                            pend = (h, rl, rlb)
                        ph, prl, prlb = pend
                        mm(sca[:, 0:Wc], diag[:, ph, :], prl[:, 0:Wc], False, True, [diagb, prlb], [scb])
                        k.op("dve", lambda e: e.tensor_copy(out=score[:, c0:c0 + Wc], in_=sca[:, 0:Wc]), r=[scb], w=[scoreb])
                    k.op("dve", lambda e: e.tensor_tensor(out=score[:, N - 128:N], in0=score[:, N - 128:N], in1=cmask, op=ALU.add),
                         r=[scoreb, cstb], w=[scoreb])
                    chk("idx")
                    thr = small[:, SM_LO:SM_LO + 1]
                    if Bq < 2:
                        k.op("dve", lambda e: e.memset(thr, -1e29), w=[smb])
                    else:
                        lo, hi, w0 = thr, small[:, SM_HI:SM_HI + 1], small[:, SM_W0:SM_W0 + 1]
                        mid, cnt, tmp = small[:, SM_MID:SM_MID + 1], small[:, SM_CNT:SM_CNT + 1], small[:, SM_TMP:SM_TMP + 1]
                        hw = small[:, SM_HW:SM_HW + IT + 1]
                        k.op("dve", lambda e: e.tensor_reduce(out=lo, in_=score[:, 0:N - 64], axis=AX.X, op=ALU.min), r=[scoreb], w=[smb])
                        k.op("dve", lambda e: e.tensor_reduce(out=hi, in_=score[:, 0:N], axis=AX.X, op=ALU.max), r=[scoreb, smb], w=[smb])
                        k.op("dve", lambda e: e.tensor_tensor(out=w0, in0=hi, in1=lo, op=ALU.subtract), r=[smb], w=[smb])
                        k.op("dve", lambda e: e.tensor_scalar(out=hw, in0=pow2, scalar1=w0, scalar2=None, op0=ALU.mult), r=[smb, cstb], w=[smb])
                        k.op("dve", lambda e: e.tensor_tensor(out=mid, in0=lo, in1=hw[:, 0:1], op=ALU.add), r=[smb], w=[smb])
                        for it in range(IT):
                            k.op("dve", lambda e: e.tensor_scalar(out=mbuf[:, 0:N], in0=score[:, 0:N], scalar1=mid, scalar2=None,
                                                                  op0=ALU.is_ge, op1=ALU.add, accum_out=cnt),
                                 r=[scoreb, smb, mbufb], w=[mbufb, smb])
                            k.op("dve", lambda e: e.tensor_scalar(out=tmp, in0=cnt, scalar1=255.5, scalar2=hw[:, it:it + 1],
                                                                  op0=ALU.is_ge, op1=ALU.mult), r=[smb], w=[smb])
                            k.op("dve", lambda e: e.tensor_tensor(out=lo, in0=lo, in1=tmp, op=ALU.add), r=[smb], w=[smb])
                            k.op("dve", lambda e: e.tensor_tensor(out=mid, in0=lo, in1=hw[:, it + 1:it + 2], op=ALU.add), r=[smb], w=[smb])
                    k.op("dve", lambda e: e.tensor_scalar(out=mbuf[:, 0:N], in0=score[:, 0:N], scalar1=thr, scalar2=-30000.0,
                                                          op0=ALU.is_lt, op1=ALU.mult), r=[scoreb, smb, mbufb], w=[mbufb])
                    chk("thr")
                    psrot["ids"] = [0, 1, 2, 3, 4, 5]
                    for j in range(Bq + 1):
                        kbj = [KKb[(j * 128) // TM]]
                        for half in range(2):
                            sa, sbf = next_ps()
                            mm(sa[:, 0:512], KK[0:64, j * 128:(j + 1) * 128], qq[0:64, 4 * half:4 * half + 4, qc], True, False,
                               kbj + qqb[4 * half:4 * half + 4], [sbf])
                            mm(sa[:, 0:512], mbuf[:, j * 128:(j + 1) * 128], identrep[:, 0:4, :], False, True,
                               [mbufb, constb], [sbf])
                            pt, ptb = ptring.next()
                            k.op("act", lambda e: e.activation(out=pt[:, 0:512], in_=sa[:, 0:512], func=AF.Exp, scale=0.125),
                                 r=[sbf], w=[ptb])
                            for hh in range(4):
                                mm(psO[half][:, hh * 65:(hh + 1) * 65], pt[:, hh * 128:(hh + 1) * 128], V65[:, j, 0:65],
                                   (j == 0 and hh == 0), (j == Bq and hh == 3), [ptb, V65b[j]], [psOb[half]], skip=True)
                    chk("att")
                    for half in range(2):
                        o3 = psO[half][:, 0:260].rearrange("p (h e) -> p h e", e=65)
                        rec = small[:, SM_REC + 4 * half:SM_REC + 4 * half + 4]
                        k.op("dve", lambda e: e.reciprocal(out=rec, in_=o3[:, :, 64]), r=[psOb[half]], w=[recb])
                        yv = yctm[:, half * 256:(half + 1) * 256].rearrange("p (h d) -> p h d", h=4)
                        k.op("dve", lambda e: e.tensor_tensor(out=yv, in0=o3[:, :, 0:64],
                                                              in1=rec.unsqueeze(2).to_broadcast([128, 4, 64]), op=ALU.mult),
                             r=[psOb[half], recb], w=[yctmb])
                    ta, tb = next_ps()
                    tab = ta[:, :].bitcast(BF16)
                    for j4 in range(4):
                        k.op("pe", lambda e: e.transpose(tab[:, j4 * 128:(j4 + 1) * 128], yctm[:, j4 * 128:(j4 + 1) * 128], ident[:, :]),
                             r=[yctmb, constb], w=[tb])
                    k.op("act", lambda e: e.copy(out=yab[2][:, :, qc], in_=tab[:, 0:512].rearrange("p (j q) -> p j q", j=4)),
                         r=[tb], w=ybuf[2])

                chk("dsa")
                k.barrier()
                psrot["ids"] = list(range(8))
                pre_norm(l, G_MIX_PRE, t0, T, hT, hTb, sqring, rstd, rstdb)
                for c in range(8):
                    for n in range(3):
                        wa_, wb_ = load_w(SCR["w_branch"][l], n * 8 + c, 4)
                        ua_, ub_ = next_ps()
                        for kc in range(4):
                            mm(ua_[:, 0:T], wa_[:, kc, :], yab[n][:, kc, :], kc == 0, kc == 3, [wb_, ybuf[n][kc]], [ub_])
                        wg_, wgb_ = load_w(S_in, GATE_BLK0 + n * 8 + c, 8)
                        ga_, gb_ = next_ps()
                        for kc in range(8):
                            mm(ga_[:, 0:T], wg_[:, kc, :], hT[:, kc, :], kc == 0, kc == 7, [wgb_, hTb[kc]], [gb_])
                        gt, gtb = gtring.next()
                        k.op("act", lambda e: e.activation(out=gt, in_=ga_[:, 0:T], func=AF.Sigmoid,
                                                           bias=vec[:, vb + B_GATE + n * 8 + c:vb + B_GATE + n * 8 + c + 1]),
                             r=[gb_, vecb], w=[gtb])
                        if n == 0:
                            k.op("dve", lambda e: e.tensor_tensor(out=acc, in0=ua_[:, 0:T], in1=gt, op=ALU.mult), r=[ub_, gtb], w=[accb])
                        else:
                            tm_, tmb_ = tmring.next()
                            k.op("dve", lambda e: e.tensor_tensor(out=tm_, in0=ua_[:, 0:T], in1=gt, op=ALU.mult), r=[ub_, gtb], w=[tmb_])
                            if n == 1:
                                k.op("dve", lambda e: e.tensor_tensor(out=acc, in0=acc, in1=tm_, op=ALU.add), r=[accb, tmb_], w=[accb])
                            else:
                                k.op("dve", lambda e: e.tensor_tensor(out=merged[:, c, :], in0=acc, in1=tm_, op=ALU.add),
                                     r=[accb, tmb_], w=[mergedb[c]])
                chk("m6")
                for c in range(8):
                    wa_, wb_ = load_w(SCR["w_out"][l], c, 8)
                    pa, pb = next_ps()
                    for kc in range(8):
                        mm(pa[:, 0:T], wa_[:, kc, :], merged[:, kc, :], kc == 0, kc == 7, [wb_, mergedb[kc]], [pb])
                    k.op("act", lambda e: e.copy(out=ytmpM[:, c, :], in_=pa[:, 0:T]), r=[pb], w=[ytbM[c]])
                post_norm_res(l, G_MIX_POST, t0, T, ytmpM, ytbM, sqring, rstd, rstdb)
                if dbg and l == 0 and tt == 1:
                    tap("ya", yab[0], ybuf[0]); tap("yb", yab[1], ybuf[1]); tap("yc", yab[2], ybuf[2])
                    tap("merged", merged, mergedb)

            chk("m7")
            k.barrier()
            T = TX
            psrot["ids"] = list(range(8))
            o = 0
            hT = carve(o, [8, T], BF16); o += 8192
            hTb = [Buf("hTx%d" % c) for c in range(8)]
            ytmp = carve(o, [8, T], F32); o += 16384
            ytb = [Buf("ytx%d" % c) for c in range(8)]
            rstd = arena[:, o // 4:o // 4 + T]; o += 2048
            rstdb = Buf("rstdx")
            sqring = Ring([arena[:, (o + i * 1024) // 4:(o + (i + 1) * 1024) // 4].bitcast(BF16) for i in range(2)], "sqx"); o += 2048
            qmT = carve(o, [4, T], BF16); o += 4096
            qmb = [Buf("qm%d" % h) for h in range(4)]
            omT = carve(o, [4, T], BF16); o += 4096
            omb = [Buf("om%d" % h) for h in range(4)]
            ptx = Ring([arena[:, (o + i * 1024) // 4:(o + (i + 1) * 1024) // 4].bitcast(BF16) for i in range(4)], "ptx"); o += 4096
            rden = arena[:, o // 4:o // 4 + T]; o += 2048
            rdenb = Buf("rden")
            memKT = carve(o, [4, NMEM], BF16); o += 2048
            memV = carve(o, [2, 512], BF16); o += 2048
            memKb, memVb = Buf("memK"), Buf("memV")
            assert o <= ARENA, o
            mT = arena[:, 8192 // 4:8192 // 4 + 8 * NMEM].rearrange("p (c m) -> p c m", c=8)
            mTb = [Buf("mT%d" % c) for c in range(8)]
            mh = carve(0, [8, NMEM], BF16)
            mhb = [Buf("mh%d" % c) for c in range(8)]
            wkv = arena[:, 16384 // 4:16384 // 4 + 2048].bitcast(BF16).rearrange("p (c n) -> p c n", c=8)
            wkvb = Buf("wkv")
            for c in range(8):
                k.dma("sp", mT[:, c, :], memT_d[c * 128:(c + 1) * 128, :], w=[mTb[c]])
            for b4 in range(4):
                k.dma("sp", wkv[:, :, b4 * 128:(b4 + 1) * 128], SCR["w_mkv"][l][4 + b4], w=[wkvb])
            k.barrier()
            rms_rstd([(mT[:, c, :], [mTb[c]]) for c in range(8)], NMEM, sqring, rstd[:, 0:NMEM], rstdb)
            for c in range(8):
                k.op("dve", lambda e: e.scalar_tensor_tensor(out=mh[:, c, :], in0=mT[:, c, :],
                                                             scalar=vec[:, vb + G_MEM_KV + c:vb + G_MEM_KV + c + 1],
                                                             in1=rstd[:, 0:NMEM], op0=ALU.mult, op1=ALU.mult),
                     r=[mTb[c], rstdb, vecb], w=[mhb[c]])
            for h in range(4):
                wa_, wb_ = load_w(SCR["w_mkv"][l], h, 8)
                pa, pb = next_ps()
                for kc in range(8):
                    mm(pa[:, 0:NMEM], wa_[:, kc, :], mh[:, kc, :], kc == 0, kc == 7, [wb_, mhb[kc]], [pb])
                k.op("act", lambda e: e.copy(out=memKT[:, h, :], in_=pa[:, 0:NMEM]), r=[pb], w=[memKb])
            for mbk in range(2):
                pa, pb = next_ps()
                for kc in range(8):
                    mm(pa[:, 0:512], mh[:, kc, mbk * 128:(mbk + 1) * 128], wkv[:, kc, :], kc == 0, kc == 7, [wkvb, mhb[kc]], [pb])
                k.op("act", lambda e: e.copy(out=memV[:, mbk, :], in_=pa[:, 0:512]), r=[pb], w=[memVb])
            k.barrier()
            for tt in range(S // T):
                t0 = tt * T
                pre_norm(l, G_MEM_PRE, t0, T, hT, hTb, sqring, rstd, rstdb)
                for h in range(4):
                    wa_, wb_ = load_w(SCR["w_mq"][l], h, 8)
                    pa, pb = next_ps()
                    for kc in range(8):
                        mm(pa[:, 0:T], wa_[:, kc, :], hT[:, kc, :], kc == 0, kc == 7, [wb_, hTb[kc]], [pb])
                    k.op("act", lambda e: e.copy(out=qmT[:, h, :], in_=pa[:, 0:T]), r=[pb], w=[qmb[h]])
                for h in range(4):
                    pts = []
                    for mbk in range(2):
                        sa, sbf = next_ps()
                        mm(sa[:, 0:T], memKT[:, h, mbk * 128:(mbk + 1) * 128], qmT[:, h, :], True, True, [memKb, qmb[h]], [sbf])
                        pt, ptb = ptx.next()
                        k.op("act", lambda e: e.activation(out=pt[:, 0:T], in_=sa[:, 0:T], func=AF.Exp, scale=float(128 ** -0.5)),
                             r=[sbf], w=[ptb])
                        pts.append((pt, ptb))
                    da, db = next_ps()
                    oa, ob = next_ps()
                    for mbk in range(2):
                        pt, ptb = pts[mbk]
                        mm(da[:, 0:T], ones[:, :], pt[:, 0:T], mbk == 0, mbk == 1, [constb, ptb], [db])
                    for mbk in range(2):
                        pt, ptb = pts[mbk]
                        mm(oa[:, 0:T], memV[:, mbk, h * 128:(h + 1) * 128], pt[:, 0:T], mbk == 0, mbk == 1, [memVb, ptb], [ob])
                    k.op("dve", lambda e: e.reciprocal(out=rden, in_=da[:, 0:T]), r=[db], w=[rdenb])
                    k.op("dve", lambda e: e.tensor_tensor(out=omT[:, h, :], in0=oa[:, 0:T], in1=rden, op=ALU.mult),
                         r=[ob, rdenb], w=[omb[h]])
                for c in range(8):
                    wa_, wb_ = load_w(SCR["w_mo"][l], c, 4)
                    pa, pb = next_ps()
                    for kc in range(4):
                        mm(pa[:, 0:T], wa_[:, kc, :], omT[:, kc, :], kc == 0, kc == 3, [wb_, omb[kc]], [pb])
                    k.op("act", lambda e: e.copy(out=ytmp[:, c, :], in_=pa[:, 0:T]), r=[pb], w=[ytb[c]])
                post_norm_res(l, G_MEM_POST, t0, T, ytmp, ytb, sqring, rstd, rstdb)

            chk("x")
            k.barrier()
            o = 0
            hid = carve(o, [22, T], BF16); o += 22528
            hidb = [Buf("hid%d" % j) for j in range(22)]
            hT = carve(o, [8, T], BF16)
            hTb = [Buf("hTf%d" % c) for c in range(8)]
            sg_aps = [arena[:, (o + 8192 + i * 2048) // 4:(o + 8192 + (i + 1) * 2048) // 4] for i in range(2)]
            sgring = Ring(sg_aps, "sg")
            ytmp = carve(o, [8, T], F32); o += 16384
            ytb = [Buf("ytf%d" % c) for c in range(8)]
            rstd = arena[:, o // 4:o // 4 + T]; o += 2048
            rstdb = Buf("rstdf")
            sqring = Ring([arena[:, (o + i * 1024) // 4:(o + (i + 1) * 1024) // 4].bitcast(BF16) for i in range(2)], "sqf"); o += 2048
            assert o <= ARENA, o
            for tt in range(S // T):
                t0 = tt * T
                k.barrier()
                pre_norm(l, G_FFN_PRE, t0, T, hT, hTb, sqring, rstd, rstdb)
                for j in range(22):
                    wg_, wgb_ = load_w(SCR["w_fi"][l], j, 8)
                    ga_, gb_ = next_ps()
                    for kc in range(8):
                        mm(ga_[:, 0:T], wg_[:, kc, :], hT[:, kc, :], kc == 0, kc == 7, [wgb_, hTb[kc]], [gb_])
                    wu_, wub_ = load_w(SCR["w_fi"][l], 22 + j, 8)
                    ua_, ub_ = next_ps()
                    for kc in range(8):
                        mm(ua_[:, 0:T], wu_[:, kc, :], hT[:, kc, :], kc == 0, kc == 7, [wub_, hTb[kc]], [ub_])
                    sg, sgb = sgring.next()
                    k.op("act", lambda e: e.activation(out=sg, in_=ga_[:, 0:T], func=AF.Silu), r=[gb_], w=[sgb])
                    k.op("dve", lambda e: e.tensor_tensor(out=hid[:, j, :], in0=ua_[:, 0:T], in1=sg, op=ALU.mult),
                         r=[ub_, sgb], w=[hidb[j]])
                k.barrier()
                for c in range(8):
                    pa, pb = next_ps()
                    for part, (k0, kn) in enumerate(((0, 8), (8, 8), (16, 6))):
                        ap_, buf_ = load_w(SCR["w_fo%d" % part][l], c, kn)
                        for kc in range(kn):
                            mm(pa[:, 0:T], ap_[:, kc, :], hid[:, k0 + kc, :], (k0 + kc) == 0, (k0 + kc) == 21,
                               [buf_, hidb[k0 + kc]], [pb])
                    k.op("act", lambda e: e.copy(out=ytmp[:, c, :], in_=pa[:, 0:T]), r=[pb], w=[ytb[c]])
                post_norm_res(l, G_FFN_POST, t0, T, ytmp, ytb, sqring, rstd, rstdb)

    try:
        _layers(n_layers)
    except _Stop:
        pass

    k.barrier()
    outs = []
    for c in range(8):
        outs.append(k.dma("sp", yT_d[c * 128:(c + 1) * 128, :], xT[:, c, :], r=xb[c]))
    for sem, val, _ in outs:
        k._wait("sp", sem, val)
    return nc, taps, k


def _cols(v, n):
    return np.ascontiguousarray(v.reshape(n, 128).T)


def prep_inputs(x, mem, g_mix_pre, w_in, conv_w, conv_b, lru_w_a, lru_b_a, lru_w_x, lru_b_x,
                lru_a_param, w_pool, pool_scale, w_branch, b_gate, w_out, g_mix_post,
                g_mem_pre, g_mem_kv, w_mem_q, w_mem_kv, w_mem_o, g_mem_post,
                g_ffn_pre, w_ffn_in, w_ffn_out, g_ffn_post):
    f = lambda a: np.asarray(a, dtype=np.float32)
    L = DEPTH
    vecs = np.zeros((128, L * NV), np.float32)
    for l in range(L):
        b = l * NV
        for base, arr in ((G_MIX_PRE, g_mix_pre), (G_MIX_POST, g_mix_post), (G_MEM_PRE, g_mem_pre),
                          (G_MEM_KV, g_mem_kv), (G_MEM_POST, g_mem_post), (G_FFN_PRE, g_ffn_pre),
                          (G_FFN_POST, g_ffn_post)):
            vecs[:, b + base:b + base + 8] = _cols(f(arr)[l], 8)
        for kk in range(4):
            vecs[:, b + CONV_W + kk * 4:b + CONV_W + kk * 4 + 4] = _cols(f(conv_w)[l, kk], 4)
        vecs[:, b + CONV_B:b + CONV_B + 4] = _cols(f(conv_b)[l], 4)
        vecs[:, b + B_A:b + B_A + 4] = _cols(f(lru_b_a)[l], 4)
        vecs[:, b + B_X:b + B_X + 4] = _cols(f(lru_b_x)[l], 4)
        vecs[:, b + A_PARAM:b + A_PARAM + 4] = _cols(f(lru_a_param)[l], 4)
        vecs[:, b + POOL_SCALE:b + POOL_SCALE + 4] = _cols(f(pool_scale)[l], 4)
        for n in range(3):
            vecs[:, b + B_GATE + n * 8:b + B_GATE + n * 8 + 8] = _cols(f(b_gate)[l, n], 8)
    cst = np.zeros((128, NCST), np.float32)
    cst[:, 0:128] = np.eye(128, dtype=np.float32)
    cst[0:64, 128 + 64:256] = -1e30
    cst[:, 256:256 + IT + 1] = (0.5 ** np.arange(1, IT + 2, dtype=np.float64)).astype(np.float32)[None, :]
    cst[:, 288:304] = (1.0 / np.arange(1, 17, dtype=np.float64)).astype(np.float32)[None, :]
    perm = list(range(0, 1536))
    for h in range(8):
        perm += list(range(1536 + 64 * h, 1536 + 64 * h + 64)) + list(range(2176 + 64 * h, 2176 + 64 * h + 64))
    perm += list(range(2048, 2112)) + list(range(2688, 2752)) + list(range(2112, 2176)) + list(range(2752, 2760))
    perm += list(range(2760, 5832))
    assert len(perm) == DIN
    w_in_p = np.zeros((L, D, DINP), np.float32)
    w_in_p[:, :, 0:2760] = f(w_in)[:, :, perm[:2760]]
    w_in_p[:, :, 2816:] = f(w_in)[:, :, 2760:]
    bd = np.zeros((L, 128, 1024), np.float32)
    for g in range(4):
        for which, wsrc in ((0, f(lru_w_a)), (1, f(lru_w_x))):
            c0 = (g * 2 + which) * 128
            bd[:, 0:64, c0:c0 + 64] = wsrc[:, 2 * g]
            bd[:, 64:128, c0 + 64:c0 + 128] = wsrc[:, 2 * g + 1]
    wp = np.ascontiguousarray(np.transpose(f(w_pool), (0, 2, 1, 3)).reshape(L, 128, 512))
    shared = dict(vecs=vecs, cst=cst, w_in=w_in_p, lru_bd=bd, w_pool=wp, w_branch=f(w_branch), w_out=f(w_out),
                  w_mem_q=f(w_mem_q), w_mem_kv=f(w_mem_kv), w_mem_o=f(w_mem_o), w_ffn_in=f(w_ffn_in),
                  w_ffn_out=f(w_ffn_out))
    xs = [np.ascontiguousarray(f(x)[b].T) for b in range(NB)]
    ms = [np.ascontiguousarray(f(mem)[b].T) for b in range(NB)]
    return shared, xs, ms


def kernel(**inputs):
    shared, xs, ms = prep_inputs(**inputs)
    nc, _, _ = build_nc()
    in_maps = []
    for core in range(8):
        m = dict(shared)
        m["xT"] = xs[core % NB]
        m["memT"] = ms[core % NB]
        in_maps.append(m)
    res = run_bass_kernel_spmd(nc, in_maps, core_ids=list(range(8)))
    out = np.stack([np.ascontiguousarray(res.results[b]["yT"].T) for b in range(NB)], axis=0)
    return out.astype(np.float32)
```

```python
import numpy as np
import concourse.bass as bass
import concourse.mybir as mybir
from concourse.bass_utils import run_bass_kernel_spmd

F32 = mybir.dt.float32
BF16 = mybir.dt.bfloat16
AF = mybir.ActivationFunctionType
ALU = mybir.AluOpType
AX = mybir.AxisListType

D = 1024
S = 4096
NB = 4
DEPTH = 4
NMEM = 256
DIN = 5832
DINP = 5888
DFF = 2816
NV = 116
IT = 22
TM = 256
TX = 512
NCST = 128 + 128 + 32 + 16
G_MIX_PRE, G_MIX_POST, G_MEM_PRE, G_MEM_KV, G_MEM_POST, G_FFN_PRE, G_FFN_POST = 0, 8, 16, 24, 32, 40, 48
CONV_W, CONV_B, B_A, B_X, A_PARAM, POOL_SCALE, B_GATE = 56, 72, 76, 80, 84, 88, 92
GATE_BLK0 = 22
RMS_EPS = 1e-6


class Buf:
    __slots__ = ("name", "w", "r", "excl")

    def __init__(self, name="", excl=False):
        self.name = name
        self.w = None
        self.r = {}
        self.excl = excl


class KB:
    def __init__(self, nc):
        self.nc = nc
        self.E = {}
        for nm, obj in (("pe", nc.tensor), ("act", nc.scalar), ("dve", nc.vector),
                        ("pool", nc.gpsimd), ("sp", nc.sync)):
            self.E[nm] = dict(obj=obj, sem=nc.alloc_semaphore("s_" + nm), n=0, known={})
        self.dsem = [[nc.alloc_semaphore("dq%d" % i), 0] for i in range(32)]
        self.di = 0
        self.ninst = 0

    def _wait(self, eng, sem, val):
        E = self.E[eng]
        if E["known"].get(sem.num, 0) < val:
            E["obj"].wait_ge(sem, val)
            E["known"][sem.num] = val

    def _deps(self, eng, r, w):
        deps = {}

        def add(ev):
            if ev is None:
                return
            sem, val, src = ev
            if src == "pe" and eng == "pe":
                return
            if sem.num not in deps or deps[sem.num][1] < val:
                deps[sem.num] = (sem, val)
        for b in r:
            add(b.w)
        for b in w:
            add(b.w)
            for ev in b.r.values():
                add(ev)
        for sem, val in deps.values():
            self._wait(eng, sem, val)

    @staticmethod
    def _mark(ev, r, w):
        for b in r:
            b.r[ev[0].num] = ev
        for b in w:
            b.w = ev
            b.r = {}

    def op(self, eng, fn, r=(), w=()):
        w = list(w) + [b for b in r if b.excl]
        r = [b for b in r if not b.excl]
        self._deps(eng, r, w)
        E = self.E[eng]
        ins = fn(E["obj"])
        E["n"] += 1
        ins.then_inc(E["sem"], 1)
        ev = (E["sem"], E["n"], eng)
        self._mark(ev, r, w)
        self.ninst += 1
        return ev

    def dma(self, q, out, in_, r=(), w=()):
        slot = self.dsem[self.di % len(self.dsem)]
        self.di += 1
        sem, cnt = slot
        if cnt > 0:
            self._wait(q, sem, 16 * cnt)
        self._deps(q, r, w)
        ins = self.E[q]["obj"].dma_start(out=out, in_=in_)
        ins.then_inc(sem, 16)
        slot[1] = cnt + 1
        ev = (sem, 16 * (cnt + 1), "dma")
        self._mark(ev, r, w)
        self.ninst += 1
        return ev

    def barrier(self):
        for a in self.E:
            for b in self.E:
                if a != b and self.E[b]["n"] > 0:
                    self._wait(a, self.E[b]["sem"], self.E[b]["n"])
            for sem, cnt in self.dsem:
                if cnt > 0:
                    self._wait(a, sem, 16 * cnt)


class Ring:
    def __init__(self, aps, name):
        self.aps = aps
        self.bufs = [Buf("%s%d" % (name, i)) for i in range(len(aps))]
        self.i = 0

    def next(self):
        j = self.i % len(self.aps)
        self.i += 1
        return self.aps[j], self.bufs[j]


class _Stop(Exception):
    pass


def build_nc(n_layers=DEPTH, dbg=False, stop=None):
    nc = bass.Bass("TRN2", target_bir_lowering=False)
    L = DEPTH
    LW = n_layers

    def din(name, shape):
        return nc.dram_tensor(name, list(shape), F32, kind="ExternalInput").ap()

    xT_d = din("xT", [D, S])
    memT_d = din("memT", [D, NMEM])
    vecs_d = din("vecs", [128, L * NV])
    cst_d = din("cst", [128, NCST])
    w_in_d = din("w_in", [LW, D, DINP])
    bd_d = din("lru_bd", [LW, 128, 1024])
    wpool_d = din("w_pool", [LW, 128, 512])
    w_branch_d = din("w_branch", [LW, 3, 512, D])
    w_out_d = din("w_out", [LW, D, D])
    w_mq_d = din("w_mem_q", [LW, D, 512])
    w_mkv_d = din("w_mem_kv", [LW, D, 1024])
    w_mo_d = din("w_mem_o", [LW, 512, D])
    w_fi_d = din("w_ffn_in", [LW, D, 2 * DFF])
    w_fo_d = din("w_ffn_out", [LW, DFF, D])
    yT_d = nc.dram_tensor("yT", [D, S], F32, kind="ExternalOutput").ap()
    taps = {}

    k = KB(nc)

    def sb(name, shape, dt):
        return nc.alloc_sbuf_tensor(name, list(shape), dt)

    xT = sb("xTs", [128, 8, S], F32)
    xb = [[Buf("x%d_%d" % (c, t)) for t in range(S // TM)] for c in range(8)]

    def xbufs(c, t0, T):
        return [xb[c][t] for t in range(t0 // TM, (t0 + T) // TM)]

    KK = sb("KK", [128, S], BF16)
    KKb = [Buf("KK%d" % i) for i in range(S // TM)]
    V65 = sb("V65", [128, 32, 66], BF16)
    V65b = [Buf("V%d" % i) for i in range(32)]
    vec = sb("vec", [128, L * NV], F32)
    vecb = Buf("vec")
    cst = sb("cstf", [128, NCST], F32)
    cstb = Buf("cst")
    ident = sb("ident", [128, 128], BF16)
    identrep = sb("identrep", [128, 8, 128], BF16)
    ones = sb("ones", [128, 128], BF16)
    constb = Buf("consts")
    nsp8 = sb("nsp8", [128, L * 4], F32)
    nsp8b = Buf("nsp8")
    bd = sb("bd", [128, 1024], BF16)
    bdb = Buf("bd")
    wpool = sb("wpool", [128, 512], BF16)
    wpoolb = Buf("wpool")
    lruhalo = sb("lruhalo", [128, 4, 4], F32)
    poolhalo = sb("poolhalo", [128, 4, 16], F32)
    carry = sb("carry", [128, 4], F32)
    halob = [Buf("halo%d" % g) for g in range(4)]
    phalob = [Buf("phalo%d" % g) for g in range(4)]
    carryb = [Buf("carry%d" % g) for g in range(4)]
    small = sb("small", [128, 64], F32)
    NRING = 6
    ringt = sb("ring", [128, NRING, 8, 128], BF16)
    ring = Ring([ringt[:, i] for i in range(NRING)], "ring")
    wvw = sb("wvw", [128, 8, 72], BF16)
    wvwb = Buf("wvw")
    ARENA = 45056
    arena = sb("arena", [128, ARENA // 4], F32)

    def carve(off, shape, dt):
        n = int(np.prod(shape))
        sz = 4 if dt == F32 else 2
        assert off % 4 == 0 and off + n * sz <= ARENA, (off, shape)
        a = arena[:, off // 4:(off + n * sz + 3) // 4]
        if dt != F32:
            a = a.bitcast(dt)
        a = a[:, 0:n]
        if len(shape) == 2:
            a = a.rearrange("p (a b) -> p a b", a=shape[0])
        elif len(shape) == 3:
            a = a.rearrange("p (a b c) -> p a b c", a=shape[0], b=shape[1])
        return a

    ps = [nc.alloc_psum_tensor("ps%d" % i, [128, 512], F32) for i in range(8)]
    psb = [Buf("ps%d" % i, excl=True) for i in range(8)]
    psrot = {"ids": list(range(8)), "i": 0}

    def next_ps():
        ids = psrot["ids"]
        j = ids[psrot["i"] % len(ids)]
        psrot["i"] += 1
        return ps[j], psb[j]

    SM_LO, SM_HI, SM_W0, SM_MID, SM_CNT, SM_TMP, SM_THR = 0, 1, 2, 3, 4, 5, 6
    SM_HW = 8
    SM_REC = 40
    SM_WIDX = 48
    smb = Buf("small")
    recb = Buf("rec")
    widxb = [Buf("widx0"), Buf("widx1")]

    k.dma("sp", vec[:, :], vecs_d[:, :], w=[vecb])
    k.dma("sp", cst[:, :], cst_d[:, :], w=[cstb])
    k.op("dve", lambda e: e.tensor_copy(out=ident[:, :], in_=cst[:, 0:128]), r=[cstb], w=[constb])
    for h in range(8):
        k.op("dve", lambda e: e.tensor_copy(out=identrep[:, h, :], in_=cst[:, 0:128]), r=[cstb], w=[constb])
    k.op("dve", lambda e: e.memset(ones[:, :], 1.0), w=[constb])
    k.op("dve", lambda e: e.memset(V65[:, :, :], 1.0), w=V65b)
    for c in range(8):
        k.dma("sp", xT[:, c, :], xT_d[c * 128:(c + 1) * 128, :], w=xb[c])
    for l in range(L):
        k.op("act", lambda e: e.activation(out=nsp8[:, l * 4:l * 4 + 4],
                                           in_=vec[:, l * NV + A_PARAM:l * NV + A_PARAM + 4],
                                           func=AF.Exp, scale=-1.0), r=[vecb], w=[nsp8b])
    k.op("act", lambda e: e.activation(out=nsp8[:, :], in_=nsp8[:, :], func=AF.Ln, bias=1.0),
         r=[nsp8b], w=[nsp8b])
    k.op("dve", lambda e: e.tensor_scalar(out=nsp8[:, :], in0=nsp8[:, :], scalar1=-8.0, scalar2=None,
                                          op0=ALU.mult), r=[nsp8b], w=[nsp8b])

    cmask = cst[:, 128:256]
    pow2 = cst[:, 256:256 + IT + 1]
    invcnt = cst[:, 288:304]

    def tap(name, ap, bufs):
        if not dbg:
            return
        t = nc.dram_tensor("tap_" + name, list(ap.shape), ap.dtype, kind="ExternalOutput").ap()
        taps[name] = t
        k.dma("sp", t, ap, r=bufs)

    def load_w(scr, blk, kc):
        ap, buf = ring.next()
        k.dma("sp", ap[:, 0:kc, :], scr[blk][:, 0:kc, :], w=[buf])
        return ap, buf

    def wview(w2d):
        return w2d.rearrange("(kc p) n -> p kc n", p=128)

    def mm(out, lhsT, rhs, start, stop, r, w, skip=False):
        if skip:
            k.op("pe", lambda e: e.matmul(out, lhsT, rhs, start=start, stop=stop, skip_group_check=True),
                 r=r, w=w)
        else:
            k.op("pe", lambda e: e.matmul(out, lhsT, rhs, start=start, stop=stop), r=r, w=w)

    def rms_rstd(srcs, T, sqring, rstd, rstdb):
        pa, pb = next_ps()
        n = len(srcs)
        for c, (ap, bufs) in enumerate(srcs):
            sq, sqb = sqring.next()
            k.op("act", lambda e: e.activation(out=sq[:, 0:T], in_=ap, func=AF.Square), r=bufs, w=[sqb])
            mm(pa[:, 0:T], ones[:, :], sq[:, 0:T], c == 0, c == n - 1, [sqb, constb], [pb])
        k.op("act", lambda e: e.activation(out=rstd, in_=pa[:, 0:T], func=AF.Sqrt, scale=1.0 / D,
                                           bias=eps_ap), r=[pb, constb], w=[rstdb])
        k.op("dve", lambda e: e.reciprocal(out=rstd, in_=rstd), r=[rstdb], w=[rstdb])

    eps_t = sb("eps", [128, 1], F32)
    eps_ap = eps_t[:, 0:1]
    k.op("dve", lambda e: e.memset(eps_t[:, :], RMS_EPS), w=[constb])

    def pre_norm(l, gbase, t0, T, hT, hTb, sqring, rstd, rstdb):
        srcs = [(xT[:, c, t0:t0 + T], xbufs(c, t0, T)) for c in range(8)]
        rms_rstd(srcs, T, sqring, rstd, rstdb)
        for c in range(8):
            k.op("dve", lambda e: e.scalar_tensor_tensor(
                out=hT[:, c, :], in0=xT[:, c, t0:t0 + T],
                scalar=vec[:, l * NV + gbase + c:l * NV + gbase + c + 1], in1=rstd,
                op0=ALU.mult, op1=ALU.mult), r=xbufs(c, t0, T) + [rstdb, vecb], w=[hTb[c]])

    def post_norm_res(l, gbase, t0, T, ytmp, ytb, sqring, rstd, rstdb):
        srcs = [(ytmp[:, c, :], [ytb[c]]) for c in range(8)]
        rms_rstd(srcs, T, sqring, rstd, rstdb)
        for c in range(8):
            k.op("dve", lambda e: e.scalar_tensor_tensor(
                out=ytmp[:, c, :], in0=ytmp[:, c, :],
                scalar=vec[:, l * NV + gbase + c:l * NV + gbase + c + 1], in1=rstd,
                op0=ALU.mult, op1=ALU.mult), r=[ytb[c], rstdb, vecb], w=[ytb[c]])
            k.op("dve", lambda e: e.tensor_tensor(out=xT[:, c, t0:t0 + T], in0=xT[:, c, t0:t0 + T],
                                                  in1=ytmp[:, c, :], op=ALU.add),
                 r=[ytb[c]] + xbufs(c, t0, T), w=xbufs(c, t0, T))

    def scratch(name, nblk):
        return nc.dram_tensor("scr_" + name, [LW, nblk, 128, 8, 128], BF16, kind="Internal").ap()

    SCR = dict(w_in=scratch("w_in", 46), w_branch=scratch("w_branch", 24), w_out=scratch("w_out", 8),
               w_mq=scratch("w_mq", 4), w_mkv=scratch("w_mkv", 8), w_mo=scratch("w_mo", 8),
               w_fi=scratch("w_fi", 44), w_fo0=scratch("w_fo0", 8), w_fo1=scratch("w_fo1", 8),
               w_fo2=scratch("w_fo2", 8))
    st32 = Ring([arena[:, i * 2048:(i + 1) * 2048].rearrange("p (k c) -> p k c", k=8) for i in range(2)], "st32")
    st16 = Ring([arena[:, 4096 + i * 1024:4096 + (i + 1) * 1024].bitcast(BF16).rearrange("p (k c) -> p k c", k=8)
                 for i in range(2)], "st16")
    cast_rr = {"i": 0}

    def convert(view, scr_l, blk0, kc, ncols):
        for c0 in range(0, ncols, 256):
            nco = min(256, ncols - c0)
            a32, b32 = st32.next()
            k.dma("sp", a32[:, 0:kc, 0:nco], view[:, :, c0:c0 + nco], w=[b32])
            a16, b16 = st16.next()
            eng = ("act", "dve", "pool")[cast_rr["i"] % 3]
            cast_rr["i"] += 1
            if eng == "act":
                k.op("act", lambda e: e.copy(out=a16[:, 0:kc, 0:nco], in_=a32[:, 0:kc, 0:nco]), r=[b32], w=[b16])
            else:
                k.op(eng, lambda e: e.tensor_copy(out=a16[:, 0:kc, 0:nco], in_=a32[:, 0:kc, 0:nco]), r=[b32], w=[b16])
            for b in range(nco // 128):
                k.dma("sp", scr_l[blk0 + c0 // 128 + b][:, 0:kc, :], a16[:, 0:kc, b * 128:(b + 1) * 128], r=[b16])

    for l in range(n_layers):
        convert(wview(w_in_d[l]), SCR["w_in"][l], 0, 8, DINP)
        for n in range(3):
            convert(wview(w_branch_d[l, n]), SCR["w_branch"][l], n * 8, 4, D)
        convert(wview(w_out_d[l]), SCR["w_out"][l], 0, 8, D)
        convert(wview(w_mq_d[l]), SCR["w_mq"][l], 0, 8, 512)
        convert(wview(w_mkv_d[l]), SCR["w_mkv"][l], 0, 8, 1024)
        convert(wview(w_mo_d[l]), SCR["w_mo"][l], 0, 4, D)
        convert(wview(w_fi_d[l]), SCR["w_fi"][l], 0, 8, 2 * DFF)
        wfo_v = wview(w_fo_d[l])
        for part, (k0, kn) in enumerate(((0, 8), (8, 8), (16, 6))):
            convert(wfo_v[:, k0:k0 + kn, :], SCR["w_fo%d" % part][l], 0, kn, D)
    k.barrier()

    chk_cnt = {}

    def chk(tag):
        chk_cnt[tag] = chk_cnt.get(tag, 0) + 1
        if stop is None:
            return
        st, _, n = stop.partition("@")
        if st == tag and chk_cnt[tag] == int(n or 1):
            raise _Stop()


    def _layers(n_layers):
        for l in range(n_layers):
            vb = l * NV
            k.barrier()
            stg, stgb = arena[:, 0:1536], Buf("stg")
            k.dma("sp", stg[:, 0:1024], bd_d[l], w=[stgb])
            k.dma("sp", stg[:, 1024:1536], wpool_d[l], w=[stgb])
            k.barrier()
            k.op("dve", lambda e: e.tensor_copy(out=bd[:, :], in_=stg[:, 0:1024]), r=[stgb], w=[bdb])
            k.op("dve", lambda e: e.tensor_copy(out=wpool[:, :], in_=stg[:, 1024:1536]), r=[stgb], w=[wpoolb])
            k.barrier()
            for g in range(4):
                k.op("dve", lambda e: e.memset(lruhalo[:, g, :], 0.0), w=[halob[g]])
                k.op("dve", lambda e: e.memset(poolhalo[:, g, :], 0.0), w=[phalob[g]])
                k.op("dve", lambda e: e.memset(carry[:, g:g + 1], 0.0), w=[carryb[g]])
            S_in = SCR["w_in"][l]
            chk("setup")
            T = TM

            score = arena[:, 0:S]
            mbuf = arena[:, S:S + S // 2].bitcast(BF16)
            scoreb, mbufb = Buf("score"), Buf("mbuf")
            o = 24576
            qq = carve(o, [8, T], BF16); o += 8 * T * 2
            qqb = [Buf("qq%d" % h) for h in range(8)]
            yab = [carve(o + n * 4 * T * 2, [4, T], BF16) for n in range(3)]; o += 3 * 4 * T * 2
            ybuf = [[Buf("y%d_%d" % (n, g)) for g in range(4)] for n in range(3)]
            o_free = o
            hT = carve(o_free, [8, T], BF16)
            hTb = [Buf("hT%d" % c) for c in range(8)]
            rstd = arena[:, (o_free + 4096) // 4:(o_free + 4096) // 4 + T]
            rstdb = Buf("rstd")
            sq_aps = [arena[:, (o_free + 5120 + i * 512) // 4:(o_free + 5120 + (i + 1) * 512) // 4].bitcast(BF16)
                      for i in range(2)]
            sqring = Ring(sq_aps, "sq")
            GS = 8256

            def gset(s):
                b = s * GS
                d = {}
                d["lrux"] = arena[:, b // 4:b // 4 + T + 4]; b += 1040
                d["graw"] = arena[:, b // 4:b // 4 + T]; b += 1024
                d["t1"] = arena[:, b // 4:b // 4 + T]; b += 1024
                d["ua"] = arena[:, b // 4:b // 4 + T]; b += 1024
                d["ra"] = arena[:, b // 4:b // 4 + T]; b += 1024
                d["ib"] = arena[:, b // 4:b // 4 + T]; b += 1024
                d["mb"] = arena[:, b // 4:b // 4 + T]; b += 1024
                d["uabf"] = arena[:, b // 4:b // 4 + T // 2].bitcast(BF16); b += 512
                d["ge"] = arena[:, b // 4:b // 4 + T // 2].bitcast(BF16); b += 512
                d["bufs"] = {n: Buf(n + str(s)) for n in ("lrux", "graw", "t1", "ua", "ra", "ib", "mb", "uabf", "ge")}
                return d

            def pset(s):
                b = 16576 + s * 3840
                d = {}
                d["px"] = arena[:, b // 4:b // 4 + T + 16]; b += 1088
                d["pa"] = arena[:, b // 4:b // 4 + T + 16]; b += 1088
                d["pb"] = arena[:, b // 4:b // 4 + T + 16]; b += 1088
                d["pooled"] = arena[:, b // 4:b // 4 + T // 2].bitcast(BF16); b += 512
                d["bufs"] = {n: Buf(n + str(s)) for n in ("px", "pa", "pb", "pooled")}
                return d
            gsets = [gset(0), gset(1)]
            psets = [pset(0), pset(1)]
            o2 = o_free
            relu_aps = []
            for i in range(3):
                relu_aps.append(arena[:, o2 // 4:o2 // 4 + 256].bitcast(BF16)); o2 += 1024
            reluring = Ring(relu_aps, "relu")
            pt_aps = []
            for i in range(4):
                pt_aps.append(arena[:, o2 // 4:o2 // 4 + 256].bitcast(BF16)); o2 += 1024
            ptring = Ring(pt_aps, "pt")
            diag = arena[:, o2 // 4:o2 // 4 + 512].bitcast(BF16).rearrange("p (h q) -> p h q", h=8); o2 += 2048
            diagb = Buf("diag")
            yctm = arena[:, o2 // 4:o2 // 4 + 256].bitcast(BF16); o2 += 1024
            yctmb = Buf("yctm")
            assert o2 <= ARENA
            merged = carve(0, [8, T], BF16)
            mergedb = [Buf("mg%d" % c) for c in range(8)]
            ytmpM = carve(4096, [8, T], F32)
            ytbM = [Buf("yt%d" % c) for c in range(8)]
            acc = arena[:, 12288 // 4:12288 // 4 + T]
            accb = Buf("acc")
            gt_aps = [arena[:, (13312 + i * 1024) // 4:(13312 + i * 1024) // 4 + T] for i in range(2)]
            gtring = Ring(gt_aps, "gt")
            tm_aps = [arena[:, (15360 + i * 1024) // 4:(15360 + i * 1024) // 4 + T] for i in range(2)]
            tmring = Ring(tm_aps, "tm")

            for tt in range(S // T):
                t0 = tt * T
                k.barrier()
                psrot["ids"] = list(range(8))
                pre_norm(l, G_MIX_PRE, t0, T, hT, hTb, sqring, rstd, rstdb)

                chk("m1")
                def proj(blk, evac):
                    wap, wb = load_w(S_in, blk, 8)
                    pa, pb = next_ps()
                    for kc in range(8):
                        mm(pa[:, 0:T], wap[:, kc, :], hT[:, kc, :], kc == 0, kc == 7, [wb, hTb[kc]], [pb])
                    evac(pa, pb)

                for g in range(4):
                    gs = gsets[g % 2]
                    gb = gs["bufs"]
                    lrux = gs["lrux"]
                    k.op("act", lambda e: e.copy(out=lrux[:, 0:3], in_=lruhalo[:, g, 0:3]), r=[halob[g]], w=[gb["lrux"]])
                    proj(g, lambda pa, pb: k.op("act", lambda e: e.copy(out=lrux[:, 3:3 + T], in_=pa[:, 0:T]),
                                                r=[pb], w=[gb["lrux"]]))
                    k.op("act", lambda e: e.copy(out=lruhalo[:, g, 0:3], in_=lrux[:, T:T + 3]), r=[gb["lrux"]], w=[halob[g]])
                    proj(4 + g, lambda pa, pb: k.op("act", lambda e: e.copy(out=gs["graw"], in_=pa[:, 0:T]),
                                                    r=[pb], w=[gb["graw"]]))
                    graw, t1 = gs["graw"], gs["t1"]
                    k.op("dve", lambda e: e.tensor_tensor(out=t1, in0=graw, in1=graw, op=ALU.mult), r=[gb["graw"]], w=[gb["t1"]])
                    k.op("dve", lambda e: e.tensor_scalar(out=t1, in0=t1, scalar1=0.044715, scalar2=1.0, op0=ALU.mult, op1=ALU.add),
                         r=[gb["t1"]], w=[gb["t1"]])
                    k.op("dve", lambda e: e.tensor_tensor(out=t1, in0=t1, in1=graw, op=ALU.mult), r=[gb["t1"], gb["graw"]], w=[gb["t1"]])
                    k.op("act", lambda e: e.activation(out=t1, in_=t1, func=AF.Sigmoid, scale=1.5957691216057308),
                         r=[gb["t1"]], w=[gb["t1"]])
                    k.op("dve", lambda e: e.tensor_tensor(out=gs["ge"], in0=t1, in1=graw, op=ALU.mult),
                         r=[gb["t1"], gb["graw"]], w=[gb["ge"]])
                    ua = gs["ua"]
                    cw = lambda kk: vec[:, vb + CONV_W + kk * 4 + g:vb + CONV_W + kk * 4 + g + 1]
                    k.op("act", lambda e: e.activation(out=ua, in_=lrux[:, 0:T], func=AF.Identity, scale=cw(0),
                                                       bias=vec[:, vb + CONV_B + g:vb + CONV_B + g + 1]),
                         r=[gb["lrux"], vecb], w=[gb["ua"]])
                    for kk in (1, 2, 3):
                        k.op("dve", lambda e: e.scalar_tensor_tensor(out=ua, in0=lrux[:, kk:kk + T], scalar=cw(kk), in1=ua,
                                                                     op0=ALU.mult, op1=ALU.add),
                             r=[gb["lrux"], gb["ua"], vecb], w=[gb["ua"]])
                    k.op("act", lambda e: e.copy(out=gs["uabf"], in_=ua), r=[gb["ua"]], w=[gb["uabf"]])
                    for which, dst, bcol in ((0, "ra", B_A), (1, "ib", B_X)):
                        pa, pb = next_ps()
                        mm(pa[:, 0:T], bd[:, (g * 2 + which) * 128:(g * 2 + which + 1) * 128], gs["uabf"], True, True,
                           [bdb, gb["uabf"]], [pb])
                        k.op("act", lambda e: e.activation(out=gs[dst], in_=pa[:, 0:T], func=AF.Sigmoid,
                                                           bias=vec[:, vb + bcol + g:vb + bcol + g + 1]),
                             r=[pb, vecb], w=[gb[dst]])
                    ra, ib, mb = gs["ra"], gs["ib"], gs["mb"]
                    k.op("act", lambda e: e.activation(out=ra, in_=ra, func=AF.Exp, scale=nsp8[:, l * 4 + g:l * 4 + g + 1]),
                         r=[gb["ra"], nsp8b], w=[gb["ra"]])
                    k.op("dve", lambda e: e.tensor_tensor(out=mb, in0=ra, in1=ra, op=ALU.mult), r=[gb["ra"]], w=[gb["mb"]])
                    k.op("dve", lambda e: e.tensor_scalar(out=mb, in0=mb, scalar1=-1.0, scalar2=1.0, op0=ALU.mult, op1=ALU.add),
                         r=[gb["mb"]], w=[gb["mb"]])
                    k.op("dve", lambda e: e.tensor_scalar(out=mb, in0=mb, scalar1=0.0, scalar2=None, op0=ALU.max),
                         r=[gb["mb"]], w=[gb["mb"]])
                    k.op("act", lambda e: e.activation(out=mb, in_=mb, func=AF.Sqrt), r=[gb["mb"]], w=[gb["mb"]])
                    k.op("dve", lambda e: e.tensor_tensor(out=ib, in0=ib, in1=ua, op=ALU.mult), r=[gb["ib"], gb["ua"]], w=[gb["ib"]])
                    k.op("dve", lambda e: e.tensor_tensor(out=ib, in0=ib, in1=mb, op=ALU.mult), r=[gb["ib"], gb["mb"]], w=[gb["ib"]])
                    k.op("dve", lambda e: e.tensor_tensor_scan(out=mb, data0=ra, data1=ib, initial=carry[:, g:g + 1],
                                                               op0=ALU.mult, op1=ALU.add),
                         r=[gb["ra"], gb["ib"], carryb[g]], w=[gb["mb"]])
                    k.op("dve", lambda e: e.tensor_copy(out=carry[:, g:g + 1], in_=mb[:, T - 1:T]), r=[gb["mb"]], w=[carryb[g]])
                    k.op("dve", lambda e: e.tensor_tensor(out=yab[0][:, g, :], in0=mb, in1=gs["ge"], op=ALU.mult),
                         r=[gb["mb"], gb["ge"]], w=[ybuf[0][g]])

                chk("A")
                for g in range(4):
                    pset_ = psets[g % 2]
                    pbf = pset_["bufs"]
                    px, pA, pB, pooled = pset_["px"], pset_["pa"], pset_["pb"], pset_["pooled"]
                    k.op("act", lambda e: e.copy(out=px[:, 0:15], in_=poolhalo[:, g, 0:15]), r=[phalob[g]], w=[pbf["px"]])
                    proj(8 + g, lambda pa, pb: k.op("act", lambda e: e.copy(out=px[:, 15:15 + T], in_=pa[:, 0:T]),
                                                    r=[pb], w=[pbf["px"]]))
                    k.op("act", lambda e: e.copy(out=poolhalo[:, g, 0:15], in_=px[:, T:T + 15]), r=[pbf["px"]], w=[phalob[g]])
                    Wd = T + 15
                    cur, curb = px, pbf["px"]
                    seq = [(pA, pbf["pa"]), (pB, pbf["pb"]), (pA, pbf["pa"]), (pB, pbf["pb"])]
                    sh = 1
                    for step in range(g + 1):
                        dst, dstb = seq[step]
                        k.op("dve", lambda e: e.tensor_tensor(out=dst[:, sh:Wd], in0=cur[:, sh:Wd], in1=cur[:, 0:Wd - sh], op=ALU.add),
                             r=[curb], w=[dstb])
                        cur, curb = dst, dstb
                        sh *= 2
                    wdw = 2 ** (g + 1)
                    k.op("dve", lambda e: e.scalar_tensor_tensor(out=pooled, in0=cur[:, 15:15 + T], scalar=1.0 / wdw,
                                                                 in1=px[:, 15:15 + T], op0=ALU.mult, op1=ALU.subtract),
                         r=[curb, pbf["px"]], w=[pbf["pooled"]])
                    if tt == 0:
                        nfix = wdw - 1
                        tmpf = pB[:, 0:nfix] if cur is not pB else pA[:, 0:nfix]
                        tmpb = pbf["pb"] if cur is not pB else pbf["pa"]
                        k.op("dve", lambda e: e.tensor_tensor(out=tmpf, in0=cur[:, 15:15 + nfix], in1=invcnt[:, 0:nfix], op=ALU.mult),
                             r=[curb, cstb], w=[tmpb])
                        k.op("dve", lambda e: e.tensor_tensor(out=pooled[:, 0:nfix], in0=tmpf, in1=px[:, 15:15 + nfix], op=ALU.subtract),
                             r=[tmpb, pbf["px"]], w=[pbf["pooled"]])
                    pa_, pb_ = next_ps()
                    mm(pa_[:, 0:T], wpool[:, g * 128:(g + 1) * 128], pooled, True, True, [wpoolb, pbf["pooled"]], [pb_])
                    k.op("act", lambda e: e.activation(out=yab[1][:, g, :], in_=pa_[:, 0:T], func=AF.Copy,
                                                       scale=vec[:, vb + POOL_SCALE + g:vb + POOL_SCALE + g + 1]),
                         r=[pb_, vecb], w=[ybuf[1][g]])

                chk("B")
                for h in range(8):
                    proj(12 + h, lambda pa, pb: k.op("act", lambda e: e.copy(out=qq[:, h, :], in_=pa[:, 0:T]),
                                                     r=[pb], w=[qqb[h]]))
                chk("q")
                proj(20, lambda pa, pb: k.op("act", lambda e: e.copy(out=KK[:, t0:t0 + T], in_=pa[:, 0:T]),
                                             r=[pb], w=[KKb[tt]]))
                chk("kk")
                k.dma("sp", wvw[:, :, :], S_in[21][:, :, 0:72], w=[wvwb])
                chk("wvw")
                for i in range(T // 128):
                    Bg = t0 // 128 + i
                    pa, pb = next_ps()
                    for kc in range(8):
                        mm(pa[:, 0:72], hT[:, kc, i * 128:(i + 1) * 128], wvw[:, kc, :], kc == 0, kc == 7,
                           [wvwb, hTb[kc]], [pb])
                    k.op("act", lambda e: e.copy(out=V65[:, Bg, 0:64], in_=pa[:, 0:64]), r=[pb], w=[V65b[Bg]])
                    k.op("act", lambda e: e.activation(out=small[:, SM_WIDX + i * 8:SM_WIDX + i * 8 + 8], in_=pa[:, 64:72],
                                                       func=AF.Copy, scale=float(8 ** -0.5 * 64 ** -0.5)),
                         r=[pb], w=[widxb[i]])

                chk("qkv")
                k.barrier()
                psO = [ps[6], ps[7]]
                psOb = [psb[6], psb[7]]
                for i in range(T // 128):
                    Bq = t0 // 128 + i
                    N = (Bq + 1) * 128
                    qc = slice(i * 128, (i + 1) * 128)
                    for h in range(8):
                        k.op("dve", lambda e: e.tensor_scalar(out=diag[:, h, :], in0=ident[:, :],
                                                              scalar1=small[:, SM_WIDX + i * 8 + h:SM_WIDX + i * 8 + h + 1],
                                                              scalar2=None, op0=ALU.mult),
                             r=[constb, widxb[i]], w=[diagb])
                    psrot["ids"] = [0, 1, 2, 3]
                    for ci, c0 in enumerate(range(0, N, 512)):
                        Wc = min(512, N - c0)
                        kb_ = [KKb[j] for j in range(c0 // TM, (c0 + Wc + TM - 1) // TM)]
                        sca, scb = ps[4 + ci % 2], psb[4 + ci % 2]
                        pend = None
                        for h in range(8):
                            la, lb = next_ps()
                            mm(la[:, 0:Wc], qq[64:128, h, qc], KK[64:128, c0:c0 + Wc], True, True, [qqb[h]] + kb_, [lb])
                            rl, rlb = reluring.next()
                            k.op("act", lambda e: e.activation(out=rl[:, 0:Wc], in_=la[:, 0:Wc], func=AF.Relu), r=[lb], w=[rlb])
                            if pend is not None:
                                ph, prl, prlb = pend
                                mm(sca[:, 0:Wc], diag[:, ph, :], prl[:, 0:Wc], ph == 0, False, [diagb, prlb], [scb])
                            pend = (h, rl, rlb)
                        ph, prl, prlb = pend
                        mm(sca[:, 0:Wc], diag[:, ph, :], prl[:, 0:Wc], False, True, [diagb, prlb], [scb])
                        k.op("dve", lambda e: e.tensor_copy(out=score[:, c0:c0 + Wc], in_=sca[:, 0:Wc]), r=[scb], w=[scoreb])
                    k.op("dve", lambda e: e.tensor_tensor(out=score[:, N - 128:N], in0=score[:, N - 128:N], in1=cmask, op=ALU.add),
                         r=[scoreb, cstb], w=[scoreb])
                    chk("idx")
                    thr = small[:, SM_LO:SM_LO + 1]
                    if Bq < 2:
                        k.op("dve", lambda e: e.memset(thr, -1e29), w=[smb])
                    else:
                        lo, hi, w0 = thr, small[:, SM_HI:SM_HI + 1], small[:, SM_W0:SM_W0 + 1]
                        mid, cnt, tmp = small[:, SM_MID:SM_MID + 1], small[:, SM_CNT:SM_CNT + 1], small[:, SM_TMP:SM_TMP + 1]
                        hw = small[:, SM_HW:SM_HW + IT + 1]
                        k.op("dve", lambda e: e.tensor_reduce(out=lo, in_=score[:, 0:N - 64], axis=AX.X, op=ALU.min), r=[scoreb], w=[smb])
                        k.op("dve", lambda e: e.tensor_reduce(out=hi, in_=score[:, 0:N], axis=AX.X, op=ALU.max), r=[scoreb, smb], w=[smb])
                        k.op("dve", lambda e: e.tensor_tensor(out=w0, in0=hi, in1=lo, op=ALU.subtract), r=[smb], w=[smb])
                        k.op("dve", lambda e: e.tensor_scalar(out=hw, in0=pow2, scalar1=w0, scalar2=None, op0=ALU.mult), r=[smb, cstb], w=[smb])
                        k.op("dve", lambda e: e.tensor_tensor(out=mid, in0=lo, in1=hw[:, 0:1], op=ALU.add), r=[smb], w=[smb])
                        for it in range(IT):
                            k.op("dve", lambda e: e.tensor_scalar(out=mbuf[:, 0:N], in0=score[:, 0:N], scalar1=mid, scalar2=None,
                                                                  op0=ALU.is_ge, op1=ALU.add, accum_out=cnt),
                                 r=[scoreb, smb, mbufb], w=[mbufb, smb])
                            k.op("dve", lambda e: e.tensor_scalar(out=tmp, in0=cnt, scalar1=255.5, scalar2=hw[:, it:it + 1],
                                                                  op0=ALU.is_ge, op1=ALU.mult), r=[smb], w=[smb])
                            k.op("dve", lambda e: e.scalar_tensor_tensor(out=mid, in0=tmp, scalar=hw[:, it + 1:it + 2], in1=mid,
                                                                         op0=ALU.subtract, op1=ALU.add), r=[smb], w=[smb])
                        k.op("dve", lambda e: e.tensor_tensor(out=lo, in0=mid, in1=hw[:, IT:IT + 1], op=ALU.subtract), r=[smb], w=[smb])
                    k.op("dve", lambda e: e.tensor_scalar(out=mbuf[:, 0:N], in0=score[:, 0:N], scalar1=thr, scalar2=-30000.0,
                                                          op0=ALU.is_lt, op1=ALU.mult), r=[scoreb, smb, mbufb], w=[mbufb])
                    chk("thr")
                    psrot["ids"] = [0, 1, 2, 3, 4, 5]
                    for j in range(Bq + 1):
                        kbj = [KKb[(j * 128) // TM]]
                        for half in range(2):
                            sa, sbf = next_ps()
                            mm(sa[:, 0:512], KK[0:64, j * 128:(j + 1) * 128], qq[0:64, 4 * half:4 * half + 4, qc], True, False,
                               kbj + qqb[4 * half:4 * half + 4], [sbf])
                            mm(sa[:, 0:512], mbuf[:, j * 128:(j + 1) * 128], identrep[:, 0:4, :], False, True,
                               [mbufb, constb], [sbf])
                            pt, ptb = ptring.next()
                            k.op("act", lambda e: e.activation(out=pt[:, 0:512], in_=sa[:, 0:512], func=AF.Exp, scale=0.125),
                                 r=[sbf], w=[ptb])
                            for hh in range(4):
                                mm(psO[half][:, hh * 65:(hh + 1) * 65], pt[:, hh * 128:(hh + 1) * 128], V65[:, j, 0:65],
                                   (j == 0 and hh == 0), (j == Bq and hh == 3), [ptb, V65b[j]], [psOb[half]], skip=True)
                    chk("att")
                    for half in range(2):
                        o3 = psO[half][:, 0:260].rearrange("p (h e) -> p h e", e=65)
                        rec = small[:, SM_REC + 4 * half:SM_REC + 4 * half + 4]
                        k.op("dve", lambda e: e.reciprocal(out=rec, in_=o3[:, :, 64]), r=[psOb[half]], w=[recb])
                        yv = yctm[:, half * 256:(half + 1) * 256].rearrange("p (h d) -> p h d", h=4)
                        k.op("dve", lambda e: e.tensor_tensor(out=yv, in0=o3[:, :, 0:64],
                                                              in1=rec.unsqueeze(2).to_broadcast([128, 4, 64]), op=ALU.mult),
                             r=[psOb[half], recb], w=[yctmb])
                    ta, tb = next_ps()
                    tab = ta[:, :].bitcast(BF16)
                    for j4 in range(4):
                        k.op("pe", lambda e: e.transpose(tab[:, j4 * 128:(j4 + 1) * 128], yctm[:, j4 * 128:(j4 + 1) * 128], ident[:, :]),
                             r=[yctmb, constb], w=[tb])
                    k.op("act", lambda e: e.copy(out=yab[2][:, :, qc], in_=tab[:, 0:512].rearrange("p (j q) -> p j q", j=4)),
                         r=[tb], w=ybuf[2])

                chk("dsa")
                k.barrier()
                psrot["ids"] = list(range(8))
                pre_norm(l, G_MIX_PRE, t0, T, hT, hTb, sqring, rstd, rstdb)
                for c in range(8):
                    for n in range(3):
                        wa_, wb_ = load_w(SCR["w_branch"][l], n * 8 + c, 4)
                        ua_, ub_ = next_ps()
                        for kc in range(4):
                            mm(ua_[:, 0:T], wa_[:, kc, :], yab[n][:, kc, :], kc == 0, kc == 3, [wb_, ybuf[n][kc]], [ub_])
                        wg_, wgb_ = load_w(S_in, GATE_BLK0 + n * 8 + c, 8)
                        ga_, gb_ = next_ps()
                        for kc in range(8):
                            mm(ga_[:, 0:T], wg_[:, kc, :], hT[:, kc, :], kc == 0, kc == 7, [wgb_, hTb[kc]], [gb_])
                        gt, gtb = gtring.next()
                        k.op("act", lambda e: e.activation(out=gt, in_=ga_[:, 0:T], func=AF.Sigmoid,
                                                           bias=vec[:, vb + B_GATE + n * 8 + c:vb + B_GATE + n * 8 + c + 1]),
                             r=[gb_, vecb], w=[gtb])
                        if n == 0:
                            k.op("dve", lambda e: e.tensor_tensor(out=acc, in0=ua_[:, 0:T], in1=gt, op=ALU.mult), r=[ub_, gtb], w=[accb])
                        else:
                            tm_, tmb_ = tmring.next()
                            k.op("dve", lambda e: e.tensor_tensor(out=tm_, in0=ua_[:, 0:T], in1=gt, op=ALU.mult), r=[ub_, gtb], w=[tmb_])
                            if n == 1:
                                k.op("dve", lambda e: e.tensor_tensor(out=acc, in0=acc, in1=tm_, op=ALU.add), r=[accb, tmb_], w=[accb])
                            else:
                                k.op("dve", lambda e: e.tensor_tensor(out=merged[:, c, :], in0=acc, in1=tm_, op=ALU.add),
                                     r=[accb, tmb_], w=[mergedb[c]])
                chk("m6")
                for c in range(8):
                    wa_, wb_ = load_w(SCR["w_out"][l], c, 8)
                    pa, pb = next_ps()
                    for kc in range(8):
                        mm(pa[:, 0:T], wa_[:, kc, :], merged[:, kc, :], kc == 0, kc == 7, [wb_, mergedb[kc]], [pb])
                    k.op("act", lambda e: e.copy(out=ytmpM[:, c, :], in_=pa[:, 0:T]), r=[pb], w=[ytbM[c]])
                post_norm_res(l, G_MIX_POST, t0, T, ytmpM, ytbM, sqring, rstd, rstdb)
                if dbg and l == 0 and tt == 1:
                    tap("ya", yab[0], ybuf[0]); tap("yb", yab[1], ybuf[1]); tap("yc", yab[2], ybuf[2])
                    tap("merged", merged, mergedb)

            chk("m7")
            k.barrier()
            T = TX
            psrot["ids"] = list(range(8))
            o = 0
            hT = carve(o, [8, T], BF16); o += 8192
            hTb = [Buf("hTx%d" % c) for c in range(8)]
            ytmp = carve(o, [8, T], F32); o += 16384
            ytb = [Buf("ytx%d" % c) for c in range(8)]
            rstd = arena[:, o // 4:o // 4 + T]; o += 2048
            rstdb = Buf("rstdx")
            sqring = Ring([arena[:, (o + i * 1024) // 4:(o + (i + 1) * 1024) // 4].bitcast(BF16) for i in range(2)], "sqx"); o += 2048
            qmT = carve(o, [4, T], BF16); o += 4096
            qmb = [Buf("qm%d" % h) for h in range(4)]
            omT = carve(o, [4, T], BF16); o += 4096
            omb = [Buf("om%d" % h) for h in range(4)]
            ptx = Ring([arena[:, (o + i * 1024) // 4:(o + (i + 1) * 1024) // 4].bitcast(BF16) for i in range(4)], "ptx"); o += 4096
            rden, rdenb = rstd, rstdb
            memKT = carve(o, [4, NMEM], BF16); o += 2048
            memV = carve(o, [2, 512], BF16); o += 2048
            memKb, memVb = Buf("memK"), Buf("memV")
            assert o <= ARENA, o
            mT = arena[:, 8192 // 4:8192 // 4 + 8 * NMEM].rearrange("p (c m) -> p c m", c=8)
            mTb = [Buf("mT%d" % c) for c in range(8)]
            mh = carve(0, [8, NMEM], BF16)
            mhb = [Buf("mh%d" % c) for c in range(8)]
            wkv = arena[:, 16384 // 4:16384 // 4 + 2048].bitcast(BF16).rearrange("p (c n) -> p c n", c=8)
            wkvb = Buf("wkv")
            for c in range(8):
                k.dma("sp", mT[:, c, :], memT_d[c * 128:(c + 1) * 128, :], w=[mTb[c]])
            for b4 in range(4):
                k.dma("sp", wkv[:, :, b4 * 128:(b4 + 1) * 128], SCR["w_mkv"][l][4 + b4], w=[wkvb])
            k.barrier()
            rms_rstd([(mT[:, c, :], [mTb[c]]) for c in range(8)], NMEM, sqring, rstd[:, 0:NMEM], rstdb)
            for c in range(8):
                k.op("dve", lambda e: e.scalar_tensor_tensor(out=mh[:, c, :], in0=mT[:, c, :],
                                                             scalar=vec[:, vb + G_MEM_KV + c:vb + G_MEM_KV + c + 1],
                                                             in1=rstd[:, 0:NMEM], op0=ALU.mult, op1=ALU.mult),
                     r=[mTb[c], rstdb, vecb], w=[mhb[c]])
            for h in range(4):
                wa_, wb_ = load_w(SCR["w_mkv"][l], h, 8)
                pa, pb = next_ps()
                for kc in range(8):
                    mm(pa[:, 0:NMEM], wa_[:, kc, :], mh[:, kc, :], kc == 0, kc == 7, [wb_, mhb[kc]], [pb])
                k.op("act", lambda e: e.copy(out=memKT[:, h, :], in_=pa[:, 0:NMEM]), r=[pb], w=[memKb])
            for mbk in range(2):
                pa, pb = next_ps()
                for kc in range(8):
                    mm(pa[:, 0:512], mh[:, kc, mbk * 128:(mbk + 1) * 128], wkv[:, kc, :], kc == 0, kc == 7, [wkvb, mhb[kc]], [pb])
                k.op("act", lambda e: e.copy(out=memV[:, mbk, :], in_=pa[:, 0:512]), r=[pb], w=[memVb])
            k.barrier()
            for tt in range(S // T):
                t0 = tt * T
                pre_norm(l, G_MEM_PRE, t0, T, hT, hTb, sqring, rstd, rstdb)
                for h in range(4):
                    wa_, wb_ = load_w(SCR["w_mq"][l], h, 8)
                    pa, pb = next_ps()
                    for kc in range(8):
                        mm(pa[:, 0:T], wa_[:, kc, :], hT[:, kc, :], kc == 0, kc == 7, [wb_, hTb[kc]], [pb])
                    k.op("act", lambda e: e.copy(out=qmT[:, h, :], in_=pa[:, 0:T]), r=[pb], w=[qmb[h]])
                for h in range(4):
                    pts = []
                    for mbk in range(2):
                        sa, sbf = next_ps()
                        mm(sa[:, 0:T], memKT[:, h, mbk * 128:(mbk + 1) * 128], qmT[:, h, :], True, True, [memKb, qmb[h]], [sbf])
                        pt, ptb = ptx.next()
                        k.op("act", lambda e: e.activation(out=pt[:, 0:T], in_=sa[:, 0:T], func=AF.Exp, scale=float(128 ** -0.5)),
                             r=[sbf], w=[ptb])
                        pts.append((pt, ptb))
                    da, db = next_ps()
                    oa, ob = next_ps()
                    for mbk in range(2):
                        pt, ptb = pts[mbk]
                        mm(da[:, 0:T], ones[:, :], pt[:, 0:T], mbk == 0, mbk == 1, [constb, ptb], [db])
                    for mbk in range(2):
                        pt, ptb = pts[mbk]
                        mm(oa[:, 0:T], memV[:, mbk, h * 128:(h + 1) * 128], pt[:, 0:T], mbk == 0, mbk == 1, [memVb, ptb], [ob])
                    k.op("dve", lambda e: e.reciprocal(out=rden, in_=da[:, 0:T]), r=[db], w=[rdenb])
                    k.op("dve", lambda e: e.tensor_tensor(out=omT[:, h, :], in0=oa[:, 0:T], in1=rden, op=ALU.mult),
                         r=[ob, rdenb], w=[omb[h]])
                for c in range(8):
                    wa_, wb_ = load_w(SCR["w_mo"][l], c, 4)
                    pa, pb = next_ps()
                    for kc in range(4):
                        mm(pa[:, 0:T], wa_[:, kc, :], omT[:, kc, :], kc == 0, kc == 3, [wb_, omb[kc]], [pb])
                    k.op("act", lambda e: e.copy(out=ytmp[:, c, :], in_=pa[:, 0:T]), r=[pb], w=[ytb[c]])
                post_norm_res(l, G_MEM_POST, t0, T, ytmp, ytb, sqring, rstd, rstdb)

            chk("x")
            k.barrier()
            o = 0
            hid = carve(o, [22, T], BF16); o += 22528
            hidb = [Buf("hid%d" % j) for j in range(22)]
            hT = carve(o, [8, T], BF16)
            hTb = [Buf("hTf%d" % c) for c in range(8)]
            sg_aps = [arena[:, (o + 8192 + i * 2048) // 4:(o + 8192 + (i + 1) * 2048) // 4] for i in range(2)]
            sgring = Ring(sg_aps, "sg")
            ytmp = carve(o, [8, T], F32); o += 16384
            ytb = [Buf("ytf%d" % c) for c in range(8)]
            rstd = arena[:, o // 4:o // 4 + T]; o += 2048
            rstdb = Buf("rstdf")
            sqring = Ring([arena[:, (o + i * 1024) // 4:(o + (i + 1) * 1024) // 4].bitcast(BF16) for i in range(2)], "sqf"); o += 2048
            assert o <= ARENA, o
            for tt in range(S // T):
                t0 = tt * T
                k.barrier()
                pre_norm(l, G_FFN_PRE, t0, T, hT, hTb, sqring, rstd, rstdb)
                for j in range(22):
                    wg_, wgb_ = load_w(SCR["w_fi"][l], j, 8)
                    ga_, gb_ = next_ps()
                    for kc in range(8):
                        mm(ga_[:, 0:T], wg_[:, kc, :], hT[:, kc, :], kc == 0, kc == 7, [wgb_, hTb[kc]], [gb_])
                    wu_, wub_ = load_w(SCR["w_fi"][l], 22 + j, 8)
                    ua_, ub_ = next_ps()
                    for kc in range(8):
                        mm(ua_[:, 0:T], wu_[:, kc, :], hT[:, kc, :], kc == 0, kc == 7, [wub_, hTb[kc]], [ub_])
                    sg, sgb = sgring.next()
                    k.op("act", lambda e: e.activation(out=sg, in_=ga_[:, 0:T], func=AF.Silu), r=[gb_], w=[sgb])
                    k.op("dve", lambda e: e.tensor_tensor(out=hid[:, j, :], in0=ua_[:, 0:T], in1=sg, op=ALU.mult),
                         r=[ub_, sgb], w=[hidb[j]])
                k.barrier()
                for c in range(8):
                    pa, pb = next_ps()
                    for part, (k0, kn) in enumerate(((0, 8), (8, 8), (16, 6))):
                        ap_, buf_ = load_w(SCR["w_fo%d" % part][l], c, kn)
                        for kc in range(kn):
                            mm(pa[:, 0:T], ap_[:, kc, :], hid[:, k0 + kc, :], (k0 + kc) == 0, (k0 + kc) == 21,
                               [buf_, hidb[k0 + kc]], [pb])
                    k.op("act", lambda e: e.copy(out=ytmp[:, c, :], in_=pa[:, 0:T]), r=[pb], w=[ytb[c]])
                post_norm_res(l, G_FFN_POST, t0, T, ytmp, ytb, sqring, rstd, rstdb)

    try:
        _layers(n_layers)
    except _Stop:
        pass

    k.barrier()
    outs = []
    for c in range(8):
        outs.append(k.dma("sp", yT_d[c * 128:(c + 1) * 128, :], xT[:, c, :], r=xb[c]))
    for sem, val, _ in outs:
        k._wait("sp", sem, val)
    return nc, taps, k


def _cols(v, n):
    return np.ascontiguousarray(v.reshape(n, 128).T)


def prep_inputs(x, mem, g_mix_pre, w_in, conv_w, conv_b, lru_w_a, lru_b_a, lru_w_x, lru_b_x,
                lru_a_param, w_pool, pool_scale, w_branch, b_gate, w_out, g_mix_post,
                g_mem_pre, g_mem_kv, w_mem_q, w_mem_kv, w_mem_o, g_mem_post,
                g_ffn_pre, w_ffn_in, w_ffn_out, g_ffn_post):
    f = lambda a: np.asarray(a, dtype=np.float32)
    L = DEPTH
    vecs = np.zeros((128, L * NV), np.float32)
    for l in range(L):
        b = l * NV
        for base, arr in ((G_MIX_PRE, g_mix_pre), (G_MIX_POST, g_mix_post), (G_MEM_PRE, g_mem_pre),
                          (G_MEM_KV, g_mem_kv), (G_MEM_POST, g_mem_post), (G_FFN_PRE, g_ffn_pre),
                          (G_FFN_POST, g_ffn_post)):
            vecs[:, b + base:b + base + 8] = _cols(f(arr)[l], 8)
        for kk in range(4):
            vecs[:, b + CONV_W + kk * 4:b + CONV_W + kk * 4 + 4] = _cols(f(conv_w)[l, kk], 4)
        vecs[:, b + CONV_B:b + CONV_B + 4] = _cols(f(conv_b)[l], 4)
        vecs[:, b + B_A:b + B_A + 4] = _cols(f(lru_b_a)[l], 4)
        vecs[:, b + B_X:b + B_X + 4] = _cols(f(lru_b_x)[l], 4)
        vecs[:, b + A_PARAM:b + A_PARAM + 4] = _cols(f(lru_a_param)[l], 4)
        vecs[:, b + POOL_SCALE:b + POOL_SCALE + 4] = _cols(f(pool_scale)[l], 4)
        for n in range(3):
            vecs[:, b + B_GATE + n * 8:b + B_GATE + n * 8 + 8] = _cols(f(b_gate)[l, n], 8)
    cst = np.zeros((128, NCST), np.float32)
    cst[:, 0:128] = np.eye(128, dtype=np.float32)
    cst[0:64, 128 + 64:256] = -1e30
    cst[:, 256:256 + IT + 1] = (0.5 ** np.arange(1, IT + 2, dtype=np.float64)).astype(np.float32)[None, :]
    cst[:, 288:304] = (1.0 / np.arange(1, 17, dtype=np.float64)).astype(np.float32)[None, :]
    perm = list(range(0, 1536))
    for h in range(8):
        perm += list(range(1536 + 64 * h, 1536 + 64 * h + 64)) + list(range(2176 + 64 * h, 2176 + 64 * h + 64))
    perm += list(range(2048, 2112)) + list(range(2688, 2752)) + list(range(2112, 2176)) + list(range(2752, 2760))
    perm += list(range(2760, 5832))
    assert len(perm) == DIN
    w_in_p = np.zeros((L, D, DINP), np.float32)
    w_in_p[:, :, 0:2760] = f(w_in)[:, :, perm[:2760]]
    w_in_p[:, :, 2816:] = f(w_in)[:, :, 2760:]
    bd = np.zeros((L, 128, 1024), np.float32)
    for g in range(4):
        for which, wsrc in ((0, f(lru_w_a)), (1, f(lru_w_x))):
            c0 = (g * 2 + which) * 128
            bd[:, 0:64, c0:c0 + 64] = wsrc[:, 2 * g]
            bd[:, 64:128, c0 + 64:c0 + 128] = wsrc[:, 2 * g + 1]
    wp = np.ascontiguousarray(np.transpose(f(w_pool), (0, 2, 1, 3)).reshape(L, 128, 512))
    shared = dict(vecs=vecs, cst=cst, w_in=w_in_p, lru_bd=bd, w_pool=wp, w_branch=f(w_branch), w_out=f(w_out),
                  w_mem_q=f(w_mem_q), w_mem_kv=f(w_mem_kv), w_mem_o=f(w_mem_o), w_ffn_in=f(w_ffn_in),
                  w_ffn_out=f(w_ffn_out))
    xs = [np.ascontiguousarray(f(x)[b].T) for b in range(NB)]
    ms = [np.ascontiguousarray(f(mem)[b].T) for b in range(NB)]
    return shared, xs, ms


N_LAUNCH = 2


def kernel(**inputs):
    shared, xs, ms = prep_inputs(**inputs)
    lpl = DEPTH // N_LAUNCH
    nc, _, _ = build_nc(n_layers=lpl)
    NCORE = 4
    x_cur = xs
    for part in range(N_LAUNCH):
        sh = {}
        for kk, v in shared.items():
            sh[kk] = np.ascontiguousarray(v[part * lpl:(part + 1) * lpl]) if v.ndim >= 3 else v
        vh = np.zeros_like(shared["vecs"])
        vh[:, 0:lpl * NV] = shared["vecs"][:, part * lpl * NV:(part + 1) * lpl * NV]
        sh["vecs"] = vh
        in_maps = []
        for core in range(NCORE):
            m = dict(sh)
            m["xT"] = x_cur[core % NB]
            m["memT"] = ms[core % NB]
            in_maps.append(m)
        res = run_bass_kernel_spmd(nc, in_maps, core_ids=list(range(NCORE)))
        x_cur = [np.ascontiguousarray(res.results[b]["yT"]) for b in range(NB)]
    out = np.stack([np.ascontiguousarray(x_cur[b].T) for b in range(NB)], axis=0)
    return out.astype(np.float32)
```

```python
import numpy as np
import concourse.bass as bass
import concourse.mybir as mybir
from concourse.bass_utils import run_bass_kernel_spmd

F32 = mybir.dt.float32
BF16 = mybir.dt.bfloat16
AF = mybir.ActivationFunctionType
ALU = mybir.AluOpType
AX = mybir.AxisListType

D = 1024
S = 4096
NB = 4
DEPTH = 4
NMEM = 256
DIN = 5832
DINP = 5888
DFF = 2816
NV = 116
IT = 22
TM = 256
TX = 512
NCST = 128 + 128 + 32 + 16
G_MIX_PRE, G_MIX_POST, G_MEM_PRE, G_MEM_KV, G_MEM_POST, G_FFN_PRE, G_FFN_POST = 0, 8, 16, 24, 32, 40, 48
CONV_W, CONV_B, B_A, B_X, A_PARAM, POOL_SCALE, B_GATE = 56, 72, 76, 80, 84, 88, 92
GATE_BLK0 = 22
RMS_EPS = 1e-6


class Buf:
    __slots__ = ("name", "w", "r", "excl")

    def __init__(self, name="", excl=False):
        self.name = name
        self.w = None
        self.r = {}
        self.excl = excl


class KB:
    def __init__(self, nc):
        self.nc = nc
        self.E = {}
        for nm, obj in (("pe", nc.tensor), ("act", nc.scalar), ("dve", nc.vector),
                        ("pool", nc.gpsimd), ("sp", nc.sync)):
            self.E[nm] = dict(obj=obj, sem=nc.alloc_semaphore("s_" + nm), n=0, known={})
        self.dsem = [[nc.alloc_semaphore("dq%d" % i), 0] for i in range(32)]
        self.di = 0
        self.ninst = 0

    def _wait(self, eng, sem, val):
        E = self.E[eng]
        if E["known"].get(sem.num, 0) < val:
            E["obj"].wait_ge(sem, val)
            E["known"][sem.num] = val

    def _deps(self, eng, r, w):
        deps = {}

        def add(ev):
            if ev is None:
                return
            sem, val, src = ev
            if src == "pe" and eng == "pe":
                return
            if sem.num not in deps or deps[sem.num][1] < val:
                deps[sem.num] = (sem, val)
        for b in r:
            add(b.w)
        for b in w:
            add(b.w)
            for ev in b.r.values():
                add(ev)
        for sem, val in deps.values():
            self._wait(eng, sem, val)

    @staticmethod
    def _mark(ev, r, w):
        for b in r:
            b.r[ev[0].num] = ev
        for b in w:
            b.w = ev
            b.r = {}

    def op(self, eng, fn, r=(), w=()):
        w = list(w) + [b for b in r if b.excl]
        r = [b for b in r if not b.excl]
        self._deps(eng, r, w)
        E = self.E[eng]
        ins = fn(E["obj"])
        E["n"] += 1
        ins.then_inc(E["sem"], 1)
        ev = (E["sem"], E["n"], eng)
        self._mark(ev, r, w)
        self.ninst += 1
        return ev

    def dma(self, q, out, in_, r=(), w=()):
        slot = self.dsem[self.di % len(self.dsem)]
        self.di += 1
        sem, cnt = slot
        if cnt > 0:
            self._wait(q, sem, 16 * cnt)
        self._deps(q, r, w)
        ins = self.E[q]["obj"].dma_start(out=out, in_=in_)
        ins.then_inc(sem, 16)
        slot[1] = cnt + 1
        ev = (sem, 16 * (cnt + 1), "dma")
        self._mark(ev, r, w)
        self.ninst += 1
        return ev

    def barrier(self):
        for a in self.E:
            for b in self.E:
                if a != b and self.E[b]["n"] > 0:
                    self._wait(a, self.E[b]["sem"], self.E[b]["n"])
            for sem, cnt in self.dsem:
                if cnt > 0:
                    self._wait(a, sem, 16 * cnt)


class Ring:
    def __init__(self, aps, name):
        self.aps = aps
        self.bufs = [Buf("%s%d" % (name, i)) for i in range(len(aps))]
        self.i = 0

    def next(self):
        j = self.i % len(self.aps)
        self.i += 1
        return self.aps[j], self.bufs[j]


class _Stop(Exception):
    pass


def build_nc(n_layers=DEPTH, dbg=False, stop=None):
    nc = bass.Bass("TRN2", target_bir_lowering=False)
    L = DEPTH
    LW = n_layers

    def din(name, shape):
        return nc.dram_tensor(name, list(shape), F32, kind="ExternalInput").ap()

    xT_d = din("xT", [D, S])
    memT_d = din("memT", [D, NMEM])
    vecs_d = din("vecs", [128, L * NV])
    cst_d = din("cst", [128, NCST])
    w_in_d = din("w_in", [LW, D, DINP])
    bd_d = din("lru_bd", [LW, 128, 1024])
    wpool_d = din("w_pool", [LW, 128, 512])
    w_branch_d = din("w_branch", [LW, 3, 512, D])
    w_out_d = din("w_out", [LW, D, D])
    w_mq_d = din("w_mem_q", [LW, D, 512])
    w_mkv_d = din("w_mem_kv", [LW, D, 1024])
    w_mo_d = din("w_mem_o", [LW, 512, D])
    w_fi_d = din("w_ffn_in", [LW, D, 2 * DFF])
    w_fo_d = din("w_ffn_out", [LW, DFF, D])
    yT_d = nc.dram_tensor("yT", [D, S], F32, kind="ExternalOutput").ap()
    taps = {}

    k = KB(nc)

    def sb(name, shape, dt):
        return nc.alloc_sbuf_tensor(name, list(shape), dt)

    xT = sb("xTs", [128, 8, S], F32)
    xb = [[Buf("x%d_%d" % (c, t)) for t in range(S // TM)] for c in range(8)]

    def xbufs(c, t0, T):
        return [xb[c][t] for t in range(t0 // TM, (t0 + T) // TM)]

    KK = sb("KK", [128, S], BF16)
    KKb = [Buf("KK%d" % i) for i in range(S // TM)]
    V65 = sb("V65", [128, 32, 66], BF16)
    V65b = [Buf("V%d" % i) for i in range(32)]
    vec = sb("vec", [128, L * NV], F32)
    vecb = Buf("vec")
    cst = sb("cstf", [128, NCST], F32)
    cstb = Buf("cst")
    ident = sb("ident", [128, 128], BF16)
    identrep = sb("identrep", [128, 8, 128], BF16)
    ones = sb("ones", [128, 128], BF16)
    constb = Buf("consts")
    nsp8 = sb("nsp8", [128, L * 4], F32)
    nsp8b = Buf("nsp8")
    bd = sb("bd", [128, 1024], BF16)
    bdb = Buf("bd")
    wpool = sb("wpool", [128, 512], BF16)
    wpoolb = Buf("wpool")
    lruhalo = sb("lruhalo", [128, 4, 4], F32)
    poolhalo = sb("poolhalo", [128, 4, 16], F32)
    carry = sb("carry", [128, 4], F32)
    halob = [Buf("halo%d" % g) for g in range(4)]
    phalob = [Buf("phalo%d" % g) for g in range(4)]
    carryb = [Buf("carry%d" % g) for g in range(4)]
    small = sb("small", [128, 64], F32)
    NRING = 6
    ringt = sb("ring", [128, NRING, 8, 128], BF16)
    ring = Ring([ringt[:, i] for i in range(NRING)], "ring")
    wvw = sb("wvw", [128, 8, 72], BF16)
    wvwb = Buf("wvw")
    ARENA = 45056
    arena = sb("arena", [128, ARENA // 4], F32)

    def carve(off, shape, dt):
        n = int(np.prod(shape))
        sz = 4 if dt == F32 else 2
        assert off % 4 == 0 and off + n * sz <= ARENA, (off, shape)
        a = arena[:, off // 4:(off + n * sz + 3) // 4]
        if dt != F32:
            a = a.bitcast(dt)
        a = a[:, 0:n]
        if len(shape) == 2:
            a = a.rearrange("p (a b) -> p a b", a=shape[0])
        elif len(shape) == 3:
            a = a.rearrange("p (a b c) -> p a b c", a=shape[0], b=shape[1])
        return a

    ps = [nc.alloc_psum_tensor("ps%d" % i, [128, 512], F32) for i in range(8)]
    psb = [Buf("ps%d" % i, excl=True) for i in range(8)]
    psrot = {"ids": list(range(8)), "i": 0}

    def next_ps():
        ids = psrot["ids"]
        j = ids[psrot["i"] % len(ids)]
        psrot["i"] += 1
        return ps[j], psb[j]

    SM_LO, SM_HI, SM_W0, SM_MID, SM_CNT, SM_TMP, SM_THR = 0, 1, 2, 3, 4, 5, 6
    SM_HW = 8
    SM_REC = 40
    SM_WIDX = 48
    smb = Buf("small")
    recb = Buf("rec")
    widxb = [Buf("widx0"), Buf("widx1")]

    k.dma("sp", vec[:, :], vecs_d[:, :], w=[vecb])
    k.dma("sp", cst[:, :], cst_d[:, :], w=[cstb])
    k.op("dve", lambda e: e.tensor_copy(out=ident[:, :], in_=cst[:, 0:128]), r=[cstb], w=[constb])
    for h in range(8):
        k.op("dve", lambda e: e.tensor_copy(out=identrep[:, h, :], in_=cst[:, 0:128]), r=[cstb], w=[constb])
    k.op("dve", lambda e: e.memset(ones[:, :], 1.0), w=[constb])
    k.op("dve", lambda e: e.memset(V65[:, :, :], 1.0), w=V65b)
    for c in range(8):
        k.dma("sp", xT[:, c, :], xT_d[c * 128:(c + 1) * 128, :], w=xb[c])
    for l in range(L):
        k.op("act", lambda e: e.activation(out=nsp8[:, l * 4:l * 4 + 4],
                                           in_=vec[:, l * NV + A_PARAM:l * NV + A_PARAM + 4],
                                           func=AF.Exp, scale=-1.0), r=[vecb], w=[nsp8b])
    k.op("act", lambda e: e.activation(out=nsp8[:, :], in_=nsp8[:, :], func=AF.Ln, bias=1.0),
         r=[nsp8b], w=[nsp8b])
    k.op("dve", lambda e: e.tensor_scalar(out=nsp8[:, :], in0=nsp8[:, :], scalar1=-8.0, scalar2=None,
                                          op0=ALU.mult), r=[nsp8b], w=[nsp8b])

    cmask = cst[:, 128:256]
    pow2 = cst[:, 256:256 + IT + 1]
    invcnt = cst[:, 288:304]

    def tap(name, ap, bufs):
        if not dbg:
            return
        t = nc.dram_tensor("tap_" + name, list(ap.shape), ap.dtype, kind="ExternalOutput").ap()
        taps[name] = t
        k.dma("sp", t, ap, r=bufs)

    def load_w(scr, blk, kc):
        ap, buf = ring.next()
        k.dma("sp", ap[:, 0:kc, :], scr[blk][:, 0:kc, :], w=[buf])
        return ap, buf

    def wview(w2d):
        return w2d.rearrange("(kc p) n -> p kc n", p=128)

    def mm(out, lhsT, rhs, start, stop, r, w, skip=False):
        if skip:
            k.op("pe", lambda e: e.matmul(out, lhsT, rhs, start=start, stop=stop, skip_group_check=True),
                 r=r, w=w)
        else:
            k.op("pe", lambda e: e.matmul(out, lhsT, rhs, start=start, stop=stop), r=r, w=w)

    def rms_rstd(srcs, T, sqring, rstd, rstdb):
        pa, pb = next_ps()
        n = len(srcs)
        for c, (ap, bufs) in enumerate(srcs):
            sq, sqb = sqring.next()
            k.op("act", lambda e: e.activation(out=sq[:, 0:T], in_=ap, func=AF.Square), r=bufs, w=[sqb])
            mm(pa[:, 0:T], ones[:, :], sq[:, 0:T], c == 0, c == n - 1, [sqb, constb], [pb])
        k.op("act", lambda e: e.activation(out=rstd, in_=pa[:, 0:T], func=AF.Sqrt, scale=1.0 / D,
                                           bias=eps_ap), r=[pb, constb], w=[rstdb])
        k.op("dve", lambda e: e.reciprocal(out=rstd, in_=rstd), r=[rstdb], w=[rstdb])

    eps_t = sb("eps", [128, 1], F32)
    eps_ap = eps_t[:, 0:1]
    k.op("dve", lambda e: e.memset(eps_t[:, :], RMS_EPS), w=[constb])

    def pre_norm(l, gbase, t0, T, hT, hTb, sqring, rstd, rstdb):
        srcs = [(xT[:, c, t0:t0 + T], xbufs(c, t0, T)) for c in range(8)]
        rms_rstd(srcs, T, sqring, rstd, rstdb)
        for c in range(8):
            k.op("dve", lambda e: e.scalar_tensor_tensor(
                out=hT[:, c, :], in0=xT[:, c, t0:t0 + T],
                scalar=vec[:, l * NV + gbase + c:l * NV + gbase + c + 1], in1=rstd,
                op0=ALU.mult, op1=ALU.mult), r=xbufs(c, t0, T) + [rstdb, vecb], w=[hTb[c]])

    def post_norm_res(l, gbase, t0, T, ytmp, ytb, sqring, rstd, rstdb):
        srcs = [(ytmp[:, c, :], [ytb[c]]) for c in range(8)]
        rms_rstd(srcs, T, sqring, rstd, rstdb)
        for c in range(8):
            k.op("dve", lambda e: e.scalar_tensor_tensor(
                out=ytmp[:, c, :], in0=ytmp[:, c, :],
                scalar=vec[:, l * NV + gbase + c:l * NV + gbase + c + 1], in1=rstd,
                op0=ALU.mult, op1=ALU.mult), r=[ytb[c], rstdb, vecb], w=[ytb[c]])
            k.op("dve", lambda e: e.tensor_tensor(out=xT[:, c, t0:t0 + T], in0=xT[:, c, t0:t0 + T],
                                                  in1=ytmp[:, c, :], op=ALU.add),
                 r=[ytb[c]] + xbufs(c, t0, T), w=xbufs(c, t0, T))

    def scratch(name, nblk):
        return nc.dram_tensor("scr_" + name, [LW, nblk, 128, 8, 128], BF16, kind="Internal").ap()

    SCR = dict(w_in=scratch("w_in", 46), w_branch=scratch("w_branch", 24), w_out=scratch("w_out", 8),
               w_mq=scratch("w_mq", 4), w_mkv=scratch("w_mkv", 8), w_mo=scratch("w_mo", 8),
               w_fi=scratch("w_fi", 44), w_fo0=scratch("w_fo0", 8), w_fo1=scratch("w_fo1", 8),
               w_fo2=scratch("w_fo2", 8))
    st32 = Ring([arena[:, i * 2048:(i + 1) * 2048].rearrange("p (k c) -> p k c", k=8) for i in range(2)], "st32")
    st16 = Ring([arena[:, 4096 + i * 1024:4096 + (i + 1) * 1024].bitcast(BF16).rearrange("p (k c) -> p k c", k=8)
                 for i in range(2)], "st16")
    cast_rr = {"i": 0}

    def convert(view, scr_l, blk0, kc, ncols):
        for c0 in range(0, ncols, 256):
            nco = min(256, ncols - c0)
            a32, b32 = st32.next()
            k.dma("sp", a32[:, 0:kc, 0:nco], view[:, :, c0:c0 + nco], w=[b32])
            a16, b16 = st16.next()
            eng = ("act", "dve", "pool")[cast_rr["i"] % 3]
            cast_rr["i"] += 1
            if eng == "act":
                k.op("act", lambda e: e.copy(out=a16[:, 0:kc, 0:nco], in_=a32[:, 0:kc, 0:nco]), r=[b32], w=[b16])
            else:
                k.op(eng, lambda e: e.tensor_copy(out=a16[:, 0:kc, 0:nco], in_=a32[:, 0:kc, 0:nco]), r=[b32], w=[b16])
            for b in range(nco // 128):
                k.dma("sp", scr_l[blk0 + c0 // 128 + b][:, 0:kc, :], a16[:, 0:kc, b * 128:(b + 1) * 128], r=[b16])

    for l in range(n_layers):
        convert(wview(w_in_d[l]), SCR["w_in"][l], 0, 8, DINP)
        for n in range(3):
            convert(wview(w_branch_d[l, n]), SCR["w_branch"][l], n * 8, 4, D)
        convert(wview(w_out_d[l]), SCR["w_out"][l], 0, 8, D)
        convert(wview(w_mq_d[l]), SCR["w_mq"][l], 0, 8, 512)
        convert(wview(w_mkv_d[l]), SCR["w_mkv"][l], 0, 8, 1024)
        convert(wview(w_mo_d[l]), SCR["w_mo"][l], 0, 4, D)
        convert(wview(w_fi_d[l]), SCR["w_fi"][l], 0, 8, 2 * DFF)
        wfo_v = wview(w_fo_d[l])
        for part, (k0, kn) in enumerate(((0, 8), (8, 8), (16, 6))):
            convert(wfo_v[:, k0:k0 + kn, :], SCR["w_fo%d" % part][l], 0, kn, D)
    k.barrier()

    def fence(new, old):
        ev = {}
        for b in old:
            for e in ([b.w] if b.w else []) + list(b.r.values()):
                if e[0].num not in ev or ev[e[0].num][1] < e[1]:
                    ev[e[0].num] = e
        for b in new:
            for key, e in ev.items():
                if key not in b.r or b.r[key][1] < e[1]:
                    b.r[key] = e

    chk_cnt = {}

    def chk(tag):
        chk_cnt[tag] = chk_cnt.get(tag, 0) + 1
        if stop is None:
            return
        st, _, n = stop.partition("@")
        if st == tag and chk_cnt[tag] == int(n or 1):
            raise _Stop()


    def _layers(n_layers):
        for l in range(n_layers):
            vb = l * NV
            k.barrier()
            stg, stgb = arena[:, 0:1536], Buf("stg")
            k.dma("sp", stg[:, 0:1024], bd_d[l], w=[stgb])
            k.dma("sp", stg[:, 1024:1536], wpool_d[l], w=[stgb])
            k.barrier()
            k.op("dve", lambda e: e.tensor_copy(out=bd[:, :], in_=stg[:, 0:1024]), r=[stgb], w=[bdb])
            k.op("dve", lambda e: e.tensor_copy(out=wpool[:, :], in_=stg[:, 1024:1536]), r=[stgb], w=[wpoolb])
            k.barrier()
            for g in range(4):
                k.op("dve", lambda e: e.memset(lruhalo[:, g, :], 0.0), w=[halob[g]])
                k.op("dve", lambda e: e.memset(poolhalo[:, g, :], 0.0), w=[phalob[g]])
                k.op("dve", lambda e: e.memset(carry[:, g:g + 1], 0.0), w=[carryb[g]])
            S_in = SCR["w_in"][l]
            chk("setup")
            T = TM

            score = arena[:, 0:S]
            mbuf = arena[:, S:S + S // 2].bitcast(BF16)
            scoreb, mbufb = Buf("score"), Buf("mbuf")
            o = 24576
            qq = carve(o, [8, T], BF16); o += 8 * T * 2
            qqb = [Buf("qq%d" % h) for h in range(8)]
            yab = [carve(o + n * 4 * T * 2, [4, T], BF16) for n in range(3)]; o += 3 * 4 * T * 2
            ybuf = [[Buf("y%d_%d" % (n, g)) for g in range(4)] for n in range(3)]
            o_free = o
            hT = carve(o_free, [8, T], BF16)
            hTb = [Buf("hT%d" % c) for c in range(8)]
            rstd = arena[:, (o_free + 4096) // 4:(o_free + 4096) // 4 + T]
            rstdb = Buf("rstd")
            sq_aps = [arena[:, (o_free + 5120 + i * 512) // 4:(o_free + 5120 + (i + 1) * 512) // 4].bitcast(BF16)
                      for i in range(2)]
            sqring = Ring(sq_aps, "sq")
            GS = 8256

            def gset(s):
                b = s * GS
                d = {}
                d["lrux"] = arena[:, b // 4:b // 4 + T + 4]; b += 1040
                d["graw"] = arena[:, b // 4:b // 4 + T]; b += 1024
                d["t1"] = arena[:, b // 4:b // 4 + T]; b += 1024
                d["ua"] = arena[:, b // 4:b // 4 + T]; b += 1024
                d["ra"] = arena[:, b // 4:b // 4 + T]; b += 1024
                d["ib"] = arena[:, b // 4:b // 4 + T]; b += 1024
                d["mb"] = arena[:, b // 4:b // 4 + T]; b += 1024
                d["uabf"] = arena[:, b // 4:b // 4 + T // 2].bitcast(BF16); b += 512
                d["ge"] = arena[:, b // 4:b // 4 + T // 2].bitcast(BF16); b += 512
                d["bufs"] = {n: Buf(n + str(s)) for n in ("lrux", "graw", "t1", "ua", "ra", "ib", "mb", "uabf", "ge")}
                return d

            def pset(s):
                b = 16576 + s * 3840
                d = {}
                d["px"] = arena[:, b // 4:b // 4 + T + 16]; b += 1088
                d["pa"] = arena[:, b // 4:b // 4 + T + 16]; b += 1088
                d["pb"] = arena[:, b // 4:b // 4 + T + 16]; b += 1088
                d["pooled"] = arena[:, b // 4:b // 4 + T // 2].bitcast(BF16); b += 512
                d["bufs"] = {n: Buf(n + str(s)) for n in ("px", "pa", "pb", "pooled")}
                return d
            gsets = [gset(0), gset(1)]
            psets = [pset(0), pset(1)]
            o2 = o_free
            relu_aps = []
            for i in range(3):
                relu_aps.append(arena[:, o2 // 4:o2 // 4 + 256].bitcast(BF16)); o2 += 1024
            reluring = Ring(relu_aps, "relu")
            pt_aps = []
            for i in range(4):
                pt_aps.append(arena[:, o2 // 4:o2 // 4 + 256].bitcast(BF16)); o2 += 1024
            ptring = Ring(pt_aps, "pt")
            diag = arena[:, o2 // 4:o2 // 4 + 512].bitcast(BF16).rearrange("p (h q) -> p h q", h=8); o2 += 2048
            diagb = Buf("diag")
            yctm = arena[:, o2 // 4:o2 // 4 + 256].bitcast(BF16); o2 += 1024
            yctmb = Buf("yctm")
            assert o2 <= ARENA
            merged = carve(0, [8, T], BF16)
            mergedb = [Buf("mg%d" % c) for c in range(8)]
            ytmpM = carve(4096, [8, T], F32)
            ytbM = [Buf("yt%d" % c) for c in range(8)]
            acc = arena[:, 12288 // 4:12288 // 4 + T]
            accb = Buf("acc")
            gt_aps = [arena[:, (13312 + i * 1024) // 4:(13312 + i * 1024) // 4 + T] for i in range(2)]
            gtring = Ring(gt_aps, "gt")
            tm_aps = [arena[:, (15360 + i * 1024) // 4:(15360 + i * 1024) // 4 + T] for i in range(2)]
            tmring = Ring(tm_aps, "tm")

            G_A = [b for gs_ in gsets for b in gs_["bufs"].values()] + [b for ps_ in psets for b in ps_["bufs"].values()]
            G_D = [scoreb, mbufb]
            G_M = mergedb + ytbM + [accb] + gtring.bufs + tmring.bufs
            H_1 = hTb + [rstdb] + sqring.bufs
            H_D = reluring.bufs + ptring.bufs + [diagb, yctmb]
            for tt in range(S // T):
                t0 = tt * T
                if tt == 0:
                    k.barrier()
                else:
                    fence(G_A, G_M)
                psrot["ids"] = list(range(8))
                pre_norm(l, G_MIX_PRE, t0, T, hT, hTb, sqring, rstd, rstdb)

                chk("m1")
                def proj(blk, evac):
                    wap, wb = load_w(S_in, blk, 8)
                    pa, pb = next_ps()
                    for kc in range(8):
                        mm(pa[:, 0:T], wap[:, kc, :], hT[:, kc, :], kc == 0, kc == 7, [wb, hTb[kc]], [pb])
                    evac(pa, pb)

                for g in range(4):
                    gs = gsets[g % 2]
                    gb = gs["bufs"]
                    lrux = gs["lrux"]
                    k.op("act", lambda e: e.copy(out=lrux[:, 0:3], in_=lruhalo[:, g, 0:3]), r=[halob[g]], w=[gb["lrux"]])
                    proj(g, lambda pa, pb: k.op("act", lambda e: e.copy(out=lrux[:, 3:3 + T], in_=pa[:, 0:T]),
                                                r=[pb], w=[gb["lrux"]]))
                    k.op("act", lambda e: e.copy(out=lruhalo[:, g, 0:3], in_=lrux[:, T:T + 3]), r=[gb["lrux"]], w=[halob[g]])
                    proj(4 + g, lambda pa, pb: k.op("act", lambda e: e.copy(out=gs["graw"], in_=pa[:, 0:T]),
                                                    r=[pb], w=[gb["graw"]]))
                    graw, t1 = gs["graw"], gs["t1"]
                    k.op("dve", lambda e: e.tensor_tensor(out=t1, in0=graw, in1=graw, op=ALU.mult), r=[gb["graw"]], w=[gb["t1"]])
                    k.op("dve", lambda e: e.tensor_scalar(out=t1, in0=t1, scalar1=0.044715, scalar2=1.0, op0=ALU.mult, op1=ALU.add),
                         r=[gb["t1"]], w=[gb["t1"]])
                    k.op("dve", lambda e: e.tensor_tensor(out=t1, in0=t1, in1=graw, op=ALU.mult), r=[gb["t1"], gb["graw"]], w=[gb["t1"]])
                    k.op("act", lambda e: e.activation(out=t1, in_=t1, func=AF.Sigmoid, scale=1.5957691216057308),
                         r=[gb["t1"]], w=[gb["t1"]])
                    k.op("dve", lambda e: e.tensor_tensor(out=gs["ge"], in0=t1, in1=graw, op=ALU.mult),
                         r=[gb["t1"], gb["graw"]], w=[gb["ge"]])
                    ua = gs["ua"]
                    cw = lambda kk: vec[:, vb + CONV_W + kk * 4 + g:vb + CONV_W + kk * 4 + g + 1]
                    k.op("act", lambda e: e.activation(out=ua, in_=lrux[:, 0:T], func=AF.Identity, scale=cw(0),
                                                       bias=vec[:, vb + CONV_B + g:vb + CONV_B + g + 1]),
                         r=[gb["lrux"], vecb], w=[gb["ua"]])
                    for kk in (1, 2, 3):
                        k.op("dve", lambda e: e.scalar_tensor_tensor(out=ua, in0=lrux[:, kk:kk + T], scalar=cw(kk), in1=ua,
                                                                     op0=ALU.mult, op1=ALU.add),
                             r=[gb["lrux"], gb["ua"], vecb], w=[gb["ua"]])
                    k.op("act", lambda e: e.copy(out=gs["uabf"], in_=ua), r=[gb["ua"]], w=[gb["uabf"]])
                    for which, dst, bcol in ((0, "ra", B_A), (1, "ib", B_X)):
                        pa, pb = next_ps()
                        mm(pa[:, 0:T], bd[:, (g * 2 + which) * 128:(g * 2 + which + 1) * 128], gs["uabf"], True, True,
                           [bdb, gb["uabf"]], [pb])
                        k.op("act", lambda e: e.activation(out=gs[dst], in_=pa[:, 0:T], func=AF.Sigmoid,
                                                           bias=vec[:, vb + bcol + g:vb + bcol + g + 1]),
                             r=[pb, vecb], w=[gb[dst]])
                    ra, ib, mb = gs["ra"], gs["ib"], gs["mb"]
                    k.op("act", lambda e: e.activation(out=ra, in_=ra, func=AF.Exp, scale=nsp8[:, l * 4 + g:l * 4 + g + 1]),
                         r=[gb["ra"], nsp8b], w=[gb["ra"]])
                    k.op("dve", lambda e: e.tensor_tensor(out=mb, in0=ra, in1=ra, op=ALU.mult), r=[gb["ra"]], w=[gb["mb"]])
                    k.op("dve", lambda e: e.tensor_scalar(out=mb, in0=mb, scalar1=-1.0, scalar2=1.0, op0=ALU.mult, op1=ALU.add),
                         r=[gb["mb"]], w=[gb["mb"]])
                    k.op("dve", lambda e: e.tensor_scalar(out=mb, in0=mb, scalar1=0.0, scalar2=None, op0=ALU.max),
                         r=[gb["mb"]], w=[gb["mb"]])
                    k.op("act", lambda e: e.activation(out=mb, in_=mb, func=AF.Sqrt), r=[gb["mb"]], w=[gb["mb"]])
                    k.op("dve", lambda e: e.tensor_tensor(out=ib, in0=ib, in1=ua, op=ALU.mult), r=[gb["ib"], gb["ua"]], w=[gb["ib"]])
                    k.op("dve", lambda e: e.tensor_tensor(out=ib, in0=ib, in1=mb, op=ALU.mult), r=[gb["ib"], gb["mb"]], w=[gb["ib"]])
                    k.op("dve", lambda e: e.tensor_tensor_scan(out=mb, data0=ra, data1=ib, initial=carry[:, g:g + 1],
                                                               op0=ALU.mult, op1=ALU.add),
                         r=[gb["ra"], gb["ib"], carryb[g]], w=[gb["mb"]])
                    k.op("dve", lambda e: e.tensor_copy(out=carry[:, g:g + 1], in_=mb[:, T - 1:T]), r=[gb["mb"]], w=[carryb[g]])
                    k.op("dve", lambda e: e.tensor_tensor(out=yab[0][:, g, :], in0=mb, in1=gs["ge"], op=ALU.mult),
                         r=[gb["mb"], gb["ge"]], w=[ybuf[0][g]])

                chk("A")
                for g in range(4):
                    pset_ = psets[g % 2]
                    pbf = pset_["bufs"]
                    px, pA, pB, pooled = pset_["px"], pset_["pa"], pset_["pb"], pset_["pooled"]
                    k.op("act", lambda e: e.copy(out=px[:, 0:15], in_=poolhalo[:, g, 0:15]), r=[phalob[g]], w=[pbf["px"]])
                    proj(8 + g, lambda pa, pb: k.op("act", lambda e: e.copy(out=px[:, 15:15 + T], in_=pa[:, 0:T]),
                                                    r=[pb], w=[pbf["px"]]))
                    k.op("act", lambda e: e.copy(out=poolhalo[:, g, 0:15], in_=px[:, T:T + 15]), r=[pbf["px"]], w=[phalob[g]])
                    Wd = T + 15
                    cur, curb = px, pbf["px"]
                    seq = [(pA, pbf["pa"]), (pB, pbf["pb"]), (pA, pbf["pa"]), (pB, pbf["pb"])]
                    sh = 1
                    for step in range(g + 1):
                        dst, dstb = seq[step]
                        k.op("dve", lambda e: e.tensor_tensor(out=dst[:, sh:Wd], in0=cur[:, sh:Wd], in1=cur[:, 0:Wd - sh], op=ALU.add),
                             r=[curb], w=[dstb])
                        cur, curb = dst, dstb
                        sh *= 2
                    wdw = 2 ** (g + 1)
                    k.op("dve", lambda e: e.scalar_tensor_tensor(out=pooled, in0=cur[:, 15:15 + T], scalar=1.0 / wdw,
                                                                 in1=px[:, 15:15 + T], op0=ALU.mult, op1=ALU.subtract),
                         r=[curb, pbf["px"]], w=[pbf["pooled"]])
                    if tt == 0:
                        nfix = wdw - 1
                        tmpf = pB[:, 0:nfix] if cur is not pB else pA[:, 0:nfix]
                        tmpb = pbf["pb"] if cur is not pB else pbf["pa"]
                        k.op("dve", lambda e: e.tensor_tensor(out=tmpf, in0=cur[:, 15:15 + nfix], in1=invcnt[:, 0:nfix], op=ALU.mult),
                             r=[curb, cstb], w=[tmpb])
                        k.op("dve", lambda e: e.tensor_tensor(out=pooled[:, 0:nfix], in0=tmpf, in1=px[:, 15:15 + nfix], op=ALU.subtract),
                             r=[tmpb, pbf["px"]], w=[pbf["pooled"]])
                    pa_, pb_ = next_ps()
                    mm(pa_[:, 0:T], wpool[:, g * 128:(g + 1) * 128], pooled, True, True, [wpoolb, pbf["pooled"]], [pb_])
                    k.op("act", lambda e: e.activation(out=yab[1][:, g, :], in_=pa_[:, 0:T], func=AF.Copy,
                                                       scale=vec[:, vb + POOL_SCALE + g:vb + POOL_SCALE + g + 1]),
                         r=[pb_, vecb], w=[ybuf[1][g]])

                chk("B")
                for h in range(8):
                    proj(12 + h, lambda pa, pb: k.op("act", lambda e: e.copy(out=qq[:, h, :], in_=pa[:, 0:T]),
                                                     r=[pb], w=[qqb[h]]))
                chk("q")
                proj(20, lambda pa, pb: k.op("act", lambda e: e.copy(out=KK[:, t0:t0 + T], in_=pa[:, 0:T]),
                                             r=[pb], w=[KKb[tt]]))
                chk("kk")
                k.dma("sp", wvw[:, :, :], S_in[21][:, :, 0:72], w=[wvwb])
                chk("wvw")
                for i in range(T // 128):
                    Bg = t0 // 128 + i
                    pa, pb = next_ps()
                    for kc in range(8):
                        mm(pa[:, 0:72], hT[:, kc, i * 128:(i + 1) * 128], wvw[:, kc, :], kc == 0, kc == 7,
                           [wvwb, hTb[kc]], [pb])
                    k.op("act", lambda e: e.copy(out=V65[:, Bg, 0:64], in_=pa[:, 0:64]), r=[pb], w=[V65b[Bg]])
                    k.op("act", lambda e: e.activation(out=small[:, SM_WIDX + i * 8:SM_WIDX + i * 8 + 8], in_=pa[:, 64:72],
                                                       func=AF.Copy, scale=float(8 ** -0.5 * 64 ** -0.5)),
                         r=[pb], w=[widxb[i]])

                chk("qkv")
                fence(G_D, G_A)
                fence(H_D, H_1)
                psO = [ps[6], ps[7]]
                psOb = [psb[6], psb[7]]
                for i in range(T // 128):
                    Bq = t0 // 128 + i
                    N = (Bq + 1) * 128
                    qc = slice(i * 128, (i + 1) * 128)
                    for h in range(8):
                        k.op("dve", lambda e: e.tensor_scalar(out=diag[:, h, :], in0=ident[:, :],
                                                              scalar1=small[:, SM_WIDX + i * 8 + h:SM_WIDX + i * 8 + h + 1],
                                                              scalar2=None, op0=ALU.mult),
                             r=[constb, widxb[i]], w=[diagb])
                    psrot["ids"] = [0, 1, 2, 3]
                    for ci, c0 in enumerate(range(0, N, 512)):
                        Wc = min(512, N - c0)
                        kb_ = [KKb[j] for j in range(c0 // TM, (c0 + Wc + TM - 1) // TM)]
                        sca, scb = ps[4 + ci % 2], psb[4 + ci % 2]
                        pend = None
                        for h in range(8):
                            la, lb = next_ps()
                            mm(la[:, 0:Wc], qq[64:128, h, qc], KK[64:128, c0:c0 + Wc], True, True, [qqb[h]] + kb_, [lb])
                            rl, rlb = reluring.next()
                            k.op("act", lambda e: e.activation(out=rl[:, 0:Wc], in_=la[:, 0:Wc], func=AF.Relu), r=[lb], w=[rlb])
                            if pend is not None:
                                ph, prl, prlb = pend
                                mm(sca[:, 0:Wc], diag[:, ph, :], prl[:, 0:Wc], ph == 0, False, [diagb, prlb], [scb])
                            pend = (h, rl, rlb)
                        ph, prl, prlb = pend
                        mm(sca[:, 0:Wc], diag[:, ph, :], prl[:, 0:Wc], False, True, [diagb, prlb], [scb])
                        k.op("dve", lambda e: e.tensor_copy(out=score[:, c0:c0 + Wc], in_=sca[:, 0:Wc]), r=[scb], w=[scoreb])
                    k.op("dve", lambda e: e.tensor_tensor(out=score[:, N - 128:N], in0=score[:, N - 128:N], in1=cmask, op=ALU.add),
                         r=[scoreb, cstb], w=[scoreb])
                    chk("idx")
                    thr = small[:, SM_LO:SM_LO + 1]
                    if Bq < 2:
                        k.op("dve", lambda e: e.memset(thr, -1e29), w=[smb])
                    else:
                        lo, hi, w0 = thr, small[:, SM_HI:SM_HI + 1], small[:, SM_W0:SM_W0 + 1]
                        mid, cnt, tmp = small[:, SM_MID:SM_MID + 1], small[:, SM_CNT:SM_CNT + 1], small[:, SM_TMP:SM_TMP + 1]
                        hw = small[:, SM_HW:SM_HW + IT + 1]
                        k.op("dve", lambda e: e.tensor_reduce(out=lo, in_=score[:, 0:N - 64], axis=AX.X, op=ALU.min), r=[scoreb], w=[smb])
                        k.op("dve", lambda e: e.tensor_reduce(out=hi, in_=score[:, 0:N], axis=AX.X, op=ALU.max), r=[scoreb, smb], w=[smb])
                        k.op("dve", lambda e: e.tensor_tensor(out=w0, in0=hi, in1=lo, op=ALU.subtract), r=[smb], w=[smb])
                        k.op("dve", lambda e: e.tensor_scalar(out=hw, in0=pow2, scalar1=w0, scalar2=None, op0=ALU.mult), r=[smb, cstb], w=[smb])
                        k.op("dve", lambda e: e.tensor_tensor(out=mid, in0=lo, in1=hw[:, 0:1], op=ALU.add), r=[smb], w=[smb])
                        for it in range(IT):
                            k.op("dve", lambda e: e.tensor_scalar(out=mbuf[:, 0:N], in0=score[:, 0:N], scalar1=mid, scalar2=None,
                                                                  op0=ALU.is_ge, op1=ALU.add, accum_out=cnt),
                                 r=[scoreb, smb, mbufb], w=[mbufb, smb])
                            k.op("dve", lambda e: e.tensor_scalar(out=tmp, in0=cnt, scalar1=255.5, scalar2=hw[:, it:it + 1],
                                                                  op0=ALU.is_ge, op1=ALU.mult), r=[smb], w=[smb])
                            k.op("dve", lambda e: e.scalar_tensor_tensor(out=mid, in0=tmp, scalar=hw[:, it + 1:it + 2], in1=mid,
                                                                         op0=ALU.subtract, op1=ALU.add), r=[smb], w=[smb])
                        k.op("dve", lambda e: e.tensor_tensor(out=lo, in0=mid, in1=hw[:, IT:IT + 1], op=ALU.subtract), r=[smb], w=[smb])
                    k.op("dve", lambda e: e.tensor_scalar(out=mbuf[:, 0:N], in0=score[:, 0:N], scalar1=thr, scalar2=-30000.0,
                                                          op0=ALU.is_lt, op1=ALU.mult), r=[scoreb, smb, mbufb], w=[mbufb])
                    chk("thr")
                    psrot["ids"] = [0, 1, 2, 3, 4, 5]
                    for j in range(Bq + 1):
                        kbj = [KKb[(j * 128) // TM]]
                        for half in range(2):
                            sa, sbf = next_ps()
                            mm(sa[:, 0:512], KK[0:64, j * 128:(j + 1) * 128], qq[0:64, 4 * half:4 * half + 4, qc], True, False,
                               kbj + qqb[4 * half:4 * half + 4], [sbf])
                            mm(sa[:, 0:512], mbuf[:, j * 128:(j + 1) * 128], identrep[:, 0:4, :], False, True,
                               [mbufb, constb], [sbf])
                            pt, ptb = ptring.next()
                            k.op("act", lambda e: e.activation(out=pt[:, 0:512], in_=sa[:, 0:512], func=AF.Exp, scale=0.125),
                                 r=[sbf], w=[ptb])
                            for hh in range(4):
                                mm(psO[half][:, hh * 65:(hh + 1) * 65], pt[:, hh * 128:(hh + 1) * 128], V65[:, j, 0:65],
                                   (j == 0 and hh == 0), (j == Bq and hh == 3), [ptb, V65b[j]], [psOb[half]], skip=True)
                    chk("att")
                    for half in range(2):
                        o3 = psO[half][:, 0:260].rearrange("p (h e) -> p h e", e=65)
                        rec = small[:, SM_REC + 4 * half:SM_REC + 4 * half + 4]
                        k.op("dve", lambda e: e.reciprocal(out=rec, in_=o3[:, :, 64]), r=[psOb[half]], w=[recb])
                        yv = yctm[:, half * 256:(half + 1) * 256].rearrange("p (h d) -> p h d", h=4)
                        k.op("dve", lambda e: e.tensor_tensor(out=yv, in0=o3[:, :, 0:64],
                                                              in1=rec.unsqueeze(2).to_broadcast([128, 4, 64]), op=ALU.mult),
                             r=[psOb[half], recb], w=[yctmb])
                    ta, tb = next_ps()
                    tab = ta[:, :].bitcast(BF16)
                    for j4 in range(4):
                        k.op("pe", lambda e: e.transpose(tab[:, j4 * 128:(j4 + 1) * 128], yctm[:, j4 * 128:(j4 + 1) * 128], ident[:, :]),
                             r=[yctmb, constb], w=[tb])
                    k.op("act", lambda e: e.copy(out=yab[2][:, :, qc], in_=tab[:, 0:512].rearrange("p (j q) -> p j q", j=4)),
                         r=[tb], w=ybuf[2])

                chk("dsa")
                fence(G_M, G_D)
                fence(H_1, H_D)
                psrot["ids"] = list(range(8))
                pre_norm(l, G_MIX_PRE, t0, T, hT, hTb, sqring, rstd, rstdb)
                for c in range(8):
                    for n in range(3):
                        wa_, wb_ = load_w(SCR["w_branch"][l], n * 8 + c, 4)
                        ua_, ub_ = next_ps()
                        for kc in range(4):
                            mm(ua_[:, 0:T], wa_[:, kc, :], yab[n][:, kc, :], kc == 0, kc == 3, [wb_, ybuf[n][kc]], [ub_])
                        wg_, wgb_ = load_w(S_in, GATE_BLK0 + n * 8 + c, 8)
                        ga_, gb_ = next_ps()
                        for kc in range(8):
                            mm(ga_[:, 0:T], wg_[:, kc, :], hT[:, kc, :], kc == 0, kc == 7, [wgb_, hTb[kc]], [gb_])
                        gt, gtb = gtring.next()
                        k.op("act", lambda e: e.activation(out=gt, in_=ga_[:, 0:T], func=AF.Sigmoid,
                                                           bias=vec[:, vb + B_GATE + n * 8 + c:vb + B_GATE + n * 8 + c + 1]),
                             r=[gb_, vecb], w=[gtb])
                        if n == 0:
                            k.op("dve", lambda e: e.tensor_tensor(out=acc, in0=ua_[:, 0:T], in1=gt, op=ALU.mult), r=[ub_, gtb], w=[accb])
                        else:
                            tm_, tmb_ = tmring.next()
                            k.op("dve", lambda e: e.tensor_tensor(out=tm_, in0=ua_[:, 0:T], in1=gt, op=ALU.mult), r=[ub_, gtb], w=[tmb_])
                            if n == 1:
                                k.op("dve", lambda e: e.tensor_tensor(out=acc, in0=acc, in1=tm_, op=ALU.add), r=[accb, tmb_], w=[accb])
                            else:
                                k.op("dve", lambda e: e.tensor_tensor(out=merged[:, c, :], in0=acc, in1=tm_, op=ALU.add),
                                     r=[accb, tmb_], w=[mergedb[c]])
                chk("m6")
                for c in range(8):
                    wa_, wb_ = load_w(SCR["w_out"][l], c, 8)
                    pa, pb = next_ps()
                    for kc in range(8):
                        mm(pa[:, 0:T], wa_[:, kc, :], merged[:, kc, :], kc == 0, kc == 7, [wb_, mergedb[kc]], [pb])
                    k.op("act", lambda e: e.copy(out=ytmpM[:, c, :], in_=pa[:, 0:T]), r=[pb], w=[ytbM[c]])
                post_norm_res(l, G_MIX_POST, t0, T, ytmpM, ytbM, sqring, rstd, rstdb)
                if dbg and l == 0 and tt == 1:
                    tap("ya", yab[0], ybuf[0]); tap("yb", yab[1], ybuf[1]); tap("yc", yab[2], ybuf[2])
                    tap("merged", merged, mergedb)

            chk("m7")
            k.barrier()
            T = TX
            psrot["ids"] = list(range(8))
            o = 0
            hT = carve(o, [8, T], BF16); o += 8192
            hTb = [Buf("hTx%d" % c) for c in range(8)]
            ytmp = carve(o, [8, T], F32); o += 16384
            ytb = [Buf("ytx%d" % c) for c in range(8)]
            rstd = arena[:, o // 4:o // 4 + T]; o += 2048
            rstdb = Buf("rstdx")
            sqring = Ring([arena[:, (o + i * 1024) // 4:(o + (i + 1) * 1024) // 4].bitcast(BF16) for i in range(2)], "sqx"); o += 2048
            qmT = carve(o, [4, T], BF16); o += 4096
            qmb = [Buf("qm%d" % h) for h in range(4)]
            omT = carve(o, [4, T], BF16); o += 4096
            omb = [Buf("om%d" % h) for h in range(4)]
            ptx = Ring([arena[:, (o + i * 1024) // 4:(o + (i + 1) * 1024) // 4].bitcast(BF16) for i in range(4)], "ptx"); o += 4096
            rden, rdenb = rstd, rstdb
            memKT = carve(o, [4, NMEM], BF16); o += 2048
            memV = carve(o, [2, 512], BF16); o += 2048
            memKb, memVb = Buf("memK"), Buf("memV")
            assert o <= ARENA, o
            mT = arena[:, 8192 // 4:8192 // 4 + 8 * NMEM].rearrange("p (c m) -> p c m", c=8)
            mTb = [Buf("mT%d" % c) for c in range(8)]
            mh = carve(0, [8, NMEM], BF16)
            mhb = [Buf("mh%d" % c) for c in range(8)]
            wkv = arena[:, 16384 // 4:16384 // 4 + 2048].bitcast(BF16).rearrange("p (c n) -> p c n", c=8)
            wkvb = Buf("wkv")
            for c in range(8):
                k.dma("sp", mT[:, c, :], memT_d[c * 128:(c + 1) * 128, :], w=[mTb[c]])
            for b4 in range(4):
                k.dma("sp", wkv[:, :, b4 * 128:(b4 + 1) * 128], SCR["w_mkv"][l][4 + b4], w=[wkvb])
            k.barrier()
            rms_rstd([(mT[:, c, :], [mTb[c]]) for c in range(8)], NMEM, sqring, rstd[:, 0:NMEM], rstdb)
            for c in range(8):
                k.op("dve", lambda e: e.scalar_tensor_tensor(out=mh[:, c, :], in0=mT[:, c, :],
                                                             scalar=vec[:, vb + G_MEM_KV + c:vb + G_MEM_KV + c + 1],
                                                             in1=rstd[:, 0:NMEM], op0=ALU.mult, op1=ALU.mult),
                     r=[mTb[c], rstdb, vecb], w=[mhb[c]])
            for h in range(4):
                wa_, wb_ = load_w(SCR["w_mkv"][l], h, 8)
                pa, pb = next_ps()
                for kc in range(8):
                    mm(pa[:, 0:NMEM], wa_[:, kc, :], mh[:, kc, :], kc == 0, kc == 7, [wb_, mhb[kc]], [pb])
                k.op("act", lambda e: e.copy(out=memKT[:, h, :], in_=pa[:, 0:NMEM]), r=[pb], w=[memKb])
            for mbk in range(2):
                pa, pb = next_ps()
                for kc in range(8):
                    mm(pa[:, 0:512], mh[:, kc, mbk * 128:(mbk + 1) * 128], wkv[:, kc, :], kc == 0, kc == 7, [wkvb, mhb[kc]], [pb])
                k.op("act", lambda e: e.copy(out=memV[:, mbk, :], in_=pa[:, 0:512]), r=[pb], w=[memVb])
            k.barrier()
            for tt in range(S // T):
                t0 = tt * T
                pre_norm(l, G_MEM_PRE, t0, T, hT, hTb, sqring, rstd, rstdb)
                for h in range(4):
                    wa_, wb_ = load_w(SCR["w_mq"][l], h, 8)
                    pa, pb = next_ps()
                    for kc in range(8):
                        mm(pa[:, 0:T], wa_[:, kc, :], hT[:, kc, :], kc == 0, kc == 7, [wb_, hTb[kc]], [pb])
                    k.op("act", lambda e: e.copy(out=qmT[:, h, :], in_=pa[:, 0:T]), r=[pb], w=[qmb[h]])
                for h in range(4):
                    pts = []
                    for mbk in range(2):
                        sa, sbf = next_ps()
                        mm(sa[:, 0:T], memKT[:, h, mbk * 128:(mbk + 1) * 128], qmT[:, h, :], True, True, [memKb, qmb[h]], [sbf])
                        pt, ptb = ptx.next()
                        k.op("act", lambda e: e.activation(out=pt[:, 0:T], in_=sa[:, 0:T], func=AF.Exp, scale=float(128 ** -0.5)),
                             r=[sbf], w=[ptb])
                        pts.append((pt, ptb))
                    da, db = next_ps()
                    oa, ob = next_ps()
                    for mbk in range(2):
                        pt, ptb = pts[mbk]
                        mm(da[:, 0:T], ones[:, :], pt[:, 0:T], mbk == 0, mbk == 1, [constb, ptb], [db])
                    for mbk in range(2):
                        pt, ptb = pts[mbk]
                        mm(oa[:, 0:T], memV[:, mbk, h * 128:(h + 1) * 128], pt[:, 0:T], mbk == 0, mbk == 1, [memVb, ptb], [ob])
                    k.op("dve", lambda e: e.reciprocal(out=rden, in_=da[:, 0:T]), r=[db], w=[rdenb])
                    k.op("dve", lambda e: e.tensor_tensor(out=omT[:, h, :], in0=oa[:, 0:T], in1=rden, op=ALU.mult),
                         r=[ob, rdenb], w=[omb[h]])
                for c in range(8):
                    wa_, wb_ = load_w(SCR["w_mo"][l], c, 4)
                    pa, pb = next_ps()
                    for kc in range(4):
                        mm(pa[:, 0:T], wa_[:, kc, :], omT[:, kc, :], kc == 0, kc == 3, [wb_, omb[kc]], [pb])
                    k.op("act", lambda e: e.copy(out=ytmp[:, c, :], in_=pa[:, 0:T]), r=[pb], w=[ytb[c]])
                post_norm_res(l, G_MEM_POST, t0, T, ytmp, ytb, sqring, rstd, rstdb)

            chk("x")
            k.barrier()
            o = 0
            hid = carve(o, [22, T], BF16); o += 22528
            hidb = [Buf("hid%d" % j) for j in range(22)]
            hT = carve(o, [8, T], BF16)
            hTb = [Buf("hTf%d" % c) for c in range(8)]
            sg_aps = [arena[:, (o + 8192 + i * 2048) // 4:(o + 8192 + (i + 1) * 2048) // 4] for i in range(2)]
            sgring = Ring(sg_aps, "sg")
            ytmp = carve(o, [8, T], F32); o += 16384
            ytb = [Buf("ytf%d" % c) for c in range(8)]
            rstd = arena[:, o // 4:o // 4 + T]; o += 2048
            rstdb = Buf("rstdf")
            sqring = Ring([arena[:, (o + i * 1024) // 4:(o + (i + 1) * 1024) // 4].bitcast(BF16) for i in range(2)], "sqf"); o += 2048
            assert o <= ARENA, o
            for tt in range(S // T):
                t0 = tt * T
                k.barrier()
                pre_norm(l, G_FFN_PRE, t0, T, hT, hTb, sqring, rstd, rstdb)
                for j in range(22):
                    wg_, wgb_ = load_w(SCR["w_fi"][l], j, 8)
                    ga_, gb_ = next_ps()
                    for kc in range(8):
                        mm(ga_[:, 0:T], wg_[:, kc, :], hT[:, kc, :], kc == 0, kc == 7, [wgb_, hTb[kc]], [gb_])
                    wu_, wub_ = load_w(SCR["w_fi"][l], 22 + j, 8)
                    ua_, ub_ = next_ps()
                    for kc in range(8):
                        mm(ua_[:, 0:T], wu_[:, kc, :], hT[:, kc, :], kc == 0, kc == 7, [wub_, hTb[kc]], [ub_])
                    sg, sgb = sgring.next()
                    k.op("act", lambda e: e.activation(out=sg, in_=ga_[:, 0:T], func=AF.Silu), r=[gb_], w=[sgb])
                    k.op("dve", lambda e: e.tensor_tensor(out=hid[:, j, :], in0=ua_[:, 0:T], in1=sg, op=ALU.mult),
                         r=[ub_, sgb], w=[hidb[j]])
                k.barrier()
                for c in range(8):
                    pa, pb = next_ps()
                    for part, (k0, kn) in enumerate(((0, 8), (8, 8), (16, 6))):
                        ap_, buf_ = load_w(SCR["w_fo%d" % part][l], c, kn)
                        for kc in range(kn):
                            mm(pa[:, 0:T], ap_[:, kc, :], hid[:, k0 + kc, :], (k0 + kc) == 0, (k0 + kc) == 21,
                               [buf_, hidb[k0 + kc]], [pb])
                    k.op("act", lambda e: e.copy(out=ytmp[:, c, :], in_=pa[:, 0:T]), r=[pb], w=[ytb[c]])
                post_norm_res(l, G_FFN_POST, t0, T, ytmp, ytb, sqring, rstd, rstdb)

    try:
        _layers(n_layers)
    except _Stop:
        pass

    k.barrier()
    outs = []
    for c in range(8):
        outs.append(k.dma("sp", yT_d[c * 128:(c + 1) * 128, :], xT[:, c, :], r=xb[c]))
    for sem, val, _ in outs:
        k._wait("sp", sem, val)
    return nc, taps, k


def _cols(v, n):
    return np.ascontiguousarray(v.reshape(n, 128).T)


def prep_inputs(x, mem, g_mix_pre, w_in, conv_w, conv_b, lru_w_a, lru_b_a, lru_w_x, lru_b_x,
                lru_a_param, w_pool, pool_scale, w_branch, b_gate, w_out, g_mix_post,
                g_mem_pre, g_mem_kv, w_mem_q, w_mem_kv, w_mem_o, g_mem_post,
                g_ffn_pre, w_ffn_in, w_ffn_out, g_ffn_post):
    f = lambda a: np.asarray(a, dtype=np.float32)
    L = DEPTH
    vecs = np.zeros((128, L * NV), np.float32)
    for l in range(L):
        b = l * NV
        for base, arr in ((G_MIX_PRE, g_mix_pre), (G_MIX_POST, g_mix_post), (G_MEM_PRE, g_mem_pre),
                          (G_MEM_KV, g_mem_kv), (G_MEM_POST, g_mem_post), (G_FFN_PRE, g_ffn_pre),
                          (G_FFN_POST, g_ffn_post)):
            vecs[:, b + base:b + base + 8] = _cols(f(arr)[l], 8)
        for kk in range(4):
            vecs[:, b + CONV_W + kk * 4:b + CONV_W + kk * 4 + 4] = _cols(f(conv_w)[l, kk], 4)
        vecs[:, b + CONV_B:b + CONV_B + 4] = _cols(f(conv_b)[l], 4)
        vecs[:, b + B_A:b + B_A + 4] = _cols(f(lru_b_a)[l], 4)
        vecs[:, b + B_X:b + B_X + 4] = _cols(f(lru_b_x)[l], 4)
        vecs[:, b + A_PARAM:b + A_PARAM + 4] = _cols(f(lru_a_param)[l], 4)
        vecs[:, b + POOL_SCALE:b + POOL_SCALE + 4] = _cols(f(pool_scale)[l], 4)
        for n in range(3):
            vecs[:, b + B_GATE + n * 8:b + B_GATE + n * 8 + 8] = _cols(f(b_gate)[l, n], 8)
    cst = np.zeros((128, NCST), np.float32)
    cst[:, 0:128] = np.eye(128, dtype=np.float32)
    cst[0:64, 128 + 64:256] = -1e30
    cst[:, 256:256 + IT + 1] = (0.5 ** np.arange(1, IT + 2, dtype=np.float64)).astype(np.float32)[None, :]
    cst[:, 288:304] = (1.0 / np.arange(1, 17, dtype=np.float64)).astype(np.float32)[None, :]
    perm = list(range(0, 1536))
    for h in range(8):
        perm += list(range(1536 + 64 * h, 1536 + 64 * h + 64)) + list(range(2176 + 64 * h, 2176 + 64 * h + 64))
    perm += list(range(2048, 2112)) + list(range(2688, 2752)) + list(range(2112, 2176)) + list(range(2752, 2760))
    perm += list(range(2760, 5832))
    assert len(perm) == DIN
    w_in_p = np.zeros((L, D, DINP), np.float32)
    w_in_p[:, :, 0:2760] = f(w_in)[:, :, perm[:2760]]
    w_in_p[:, :, 2816:] = f(w_in)[:, :, 2760:]
    bd = np.zeros((L, 128, 1024), np.float32)
    for g in range(4):
        for which, wsrc in ((0, f(lru_w_a)), (1, f(lru_w_x))):
            c0 = (g * 2 + which) * 128
            bd[:, 0:64, c0:c0 + 64] = wsrc[:, 2 * g]
            bd[:, 64:128, c0 + 64:c0 + 128] = wsrc[:, 2 * g + 1]
    wp = np.ascontiguousarray(np.transpose(f(w_pool), (0, 2, 1, 3)).reshape(L, 128, 512))
    shared = dict(vecs=vecs, cst=cst, w_in=w_in_p, lru_bd=bd, w_pool=wp, w_branch=f(w_branch), w_out=f(w_out),
                  w_mem_q=f(w_mem_q), w_mem_kv=f(w_mem_kv), w_mem_o=f(w_mem_o), w_ffn_in=f(w_ffn_in),
                  w_ffn_out=f(w_ffn_out))
    xs = [np.ascontiguousarray(f(x)[b].T) for b in range(NB)]
    ms = [np.ascontiguousarray(f(mem)[b].T) for b in range(NB)]
    return shared, xs, ms


N_LAUNCH = 2


def kernel(**inputs):
    shared, xs, ms = prep_inputs(**inputs)
    lpl = DEPTH // N_LAUNCH
    nc, _, _ = build_nc(n_layers=lpl)
    NCORE = 4
    x_cur = xs
    for part in range(N_LAUNCH):
        sh = {}
        for kk, v in shared.items():
            sh[kk] = np.ascontiguousarray(v[part * lpl:(part + 1) * lpl]) if v.ndim >= 3 else v
        vh = np.zeros_like(shared["vecs"])
        vh[:, 0:lpl * NV] = shared["vecs"][:, part * lpl * NV:(part + 1) * lpl * NV]
        sh["vecs"] = vh
        in_maps = []
        for core in range(NCORE):
            m = dict(sh)
            m["xT"] = x_cur[core % NB]
            m["memT"] = ms[core % NB]
            in_maps.append(m)
        res = run_bass_kernel_spmd(nc, in_maps, core_ids=list(range(NCORE)))
        x_cur = [np.ascontiguousarray(res.results[b]["yT"]) for b in range(NB)]
    out = np.stack([np.ascontiguousarray(x_cur[b].T) for b in range(NB)], axis=0)
    return out.astype(np.float32)
```

```python
import numpy as np
import concourse.bass as bass
import concourse.mybir as mybir
from concourse.bass_utils import run_bass_kernel_spmd

F32 = mybir.dt.float32
BF16 = mybir.dt.bfloat16
AF = mybir.ActivationFunctionType
ALU = mybir.AluOpType
AX = mybir.AxisListType

D = 1024
S = 4096
NB = 4
DEPTH = 4
NMEM = 256
DIN = 5832
DINP = 5888
DFF = 2816
NV = 116
IT = 22
TM = 256
TX = 512
NCST = 128 + 128 + 32 + 16
G_MIX_PRE, G_MIX_POST, G_MEM_PRE, G_MEM_KV, G_MEM_POST, G_FFN_PRE, G_FFN_POST = 0, 8, 16, 24, 32, 40, 48
CONV_W, CONV_B, B_A, B_X, A_PARAM, POOL_SCALE, B_GATE = 56, 72, 76, 80, 84, 88, 92
GATE_BLK0 = 22
RMS_EPS = 1e-6


class Buf:
    __slots__ = ("name", "w", "r", "excl")

    def __init__(self, name="", excl=False):
        self.name = name
        self.w = None
        self.r = {}
        self.excl = excl


class KB:
    def __init__(self, nc):
        self.nc = nc
        self.E = {}
        for nm, obj in (("pe", nc.tensor), ("act", nc.scalar), ("dve", nc.vector),
                        ("pool", nc.gpsimd), ("sp", nc.sync)):
            self.E[nm] = dict(obj=obj, sem=nc.alloc_semaphore("s_" + nm), n=0, known={})
        self.dsem = [[nc.alloc_semaphore("dq%d" % i), 0] for i in range(32)]
        self.di = 0
        self.ninst = 0

    def _wait(self, eng, sem, val):
        E = self.E[eng]
        if E["known"].get(sem.num, 0) < val:
            E["obj"].wait_ge(sem, val)
            E["known"][sem.num] = val

    def _deps(self, eng, r, w):
        deps = {}

        def add(ev):
            if ev is None:
                return
            sem, val, src = ev
            if src == "pe" and eng == "pe":
                return
            if sem.num not in deps or deps[sem.num][1] < val:
                deps[sem.num] = (sem, val)
        for b in r:
            add(b.w)
        for b in w:
            add(b.w)
            for ev in b.r.values():
                add(ev)
        for sem, val in deps.values():
            self._wait(eng, sem, val)

    @staticmethod
    def _mark(ev, r, w):
        for b in r:
            b.r[ev[0].num] = ev
        for b in w:
            b.w = ev
            b.r = {}

    def op(self, eng, fn, r=(), w=()):
        w = list(w) + [b for b in r if b.excl]
        r = [b for b in r if not b.excl]
        self._deps(eng, r, w)
        E = self.E[eng]
        ins = fn(E["obj"])
        E["n"] += 1
        ins.then_inc(E["sem"], 1)
        ev = (E["sem"], E["n"], eng)
        self._mark(ev, r, w)
        self.ninst += 1
        return ev

    def dma(self, q, out, in_, r=(), w=()):
        slot = self.dsem[self.di % len(self.dsem)]
        self.di += 1
        sem, cnt = slot
        if cnt > 0:
            self._wait(q, sem, 16 * cnt)
        self._deps(q, r, w)
        ins = self.E[q]["obj"].dma_start(out=out, in_=in_)
        ins.then_inc(sem, 16)
        slot[1] = cnt + 1
        ev = (sem, 16 * (cnt + 1), "dma")
        self._mark(ev, r, w)
        self.ninst += 1
        return ev

    def barrier(self):
        for a in self.E:
            for b in self.E:
                if a != b and self.E[b]["n"] > 0:
                    self._wait(a, self.E[b]["sem"], self.E[b]["n"])
            for sem, cnt in self.dsem:
                if cnt > 0:
                    self._wait(a, sem, 16 * cnt)


class Ring:
    def __init__(self, aps, name):
        self.aps = aps
        self.bufs = [Buf("%s%d" % (name, i)) for i in range(len(aps))]
        self.i = 0

    def next(self):
        j = self.i % len(self.aps)
        self.i += 1
        return self.aps[j], self.bufs[j]


class _Stop(Exception):
    pass


def build_nc(n_layers=DEPTH, dbg=False, stop=None):
    nc = bass.Bass("TRN2", target_bir_lowering=False)
    L = DEPTH
    LW = n_layers

    def din(name, shape):
        return nc.dram_tensor(name, list(shape), F32, kind="ExternalInput").ap()

    xT_d = din("xT", [D, S])
    memT_d = din("memT", [D, NMEM])
    vecs_d = din("vecs", [128, L * NV])
    cst_d = din("cst", [128, NCST])
    w_in_d = din("w_in", [LW, D, DINP])
    bd_d = din("lru_bd", [LW, 128, 1024])
    wpool_d = din("w_pool", [LW, 128, 512])
    w_branch_d = din("w_branch", [LW, 3, 512, D])
    w_out_d = din("w_out", [LW, D, D])
    w_mq_d = din("w_mem_q", [LW, D, 512])
    w_mkv_d = din("w_mem_kv", [LW, D, 1024])
    w_mo_d = din("w_mem_o", [LW, 512, D])
    w_fi_d = din("w_ffn_in", [LW, D, 2 * DFF])
    w_fo_d = din("w_ffn_out", [LW, DFF, D])
    yT_d = nc.dram_tensor("yT", [D, S], F32, kind="ExternalOutput").ap()
    taps = {}

    k = KB(nc)

    def sb(name, shape, dt):
        return nc.alloc_sbuf_tensor(name, list(shape), dt)

    xT = sb("xTs", [128, 8, S], F32)
    xb = [[Buf("x%d_%d" % (c, t)) for t in range(S // TM)] for c in range(8)]

    def xbufs(c, t0, T):
        return [xb[c][t] for t in range(t0 // TM, (t0 + T) // TM)]

    KK = sb("KK", [128, S], BF16)
    KKb = [Buf("KK%d" % i) for i in range(S // TM)]
    V65 = sb("V65", [128, 32, 66], BF16)
    V65b = [Buf("V%d" % i) for i in range(32)]
    vec = sb("vec", [128, L * NV], F32)
    vecb = Buf("vec")
    cst = sb("cstf", [128, NCST], F32)
    cstb = Buf("cst")
    ident = sb("ident", [128, 128], BF16)
    identrep = sb("identrep", [128, 8, 128], BF16)
    ones = sb("ones", [128, 128], BF16)
    constb = Buf("consts")
    nsp8 = sb("nsp8", [128, L * 4], F32)
    nsp8b = Buf("nsp8")
    bd = sb("bd", [128, 1024], BF16)
    bdb = Buf("bd")
    wpool = sb("wpool", [128, 512], BF16)
    wpoolb = Buf("wpool")
    lruhalo = sb("lruhalo", [128, 4, 4], F32)
    poolhalo = sb("poolhalo", [128, 4, 16], F32)
    carry = sb("carry", [128, 4], F32)
    halob = [Buf("halo%d" % g) for g in range(4)]
    phalob = [Buf("phalo%d" % g) for g in range(4)]
    carryb = [Buf("carry%d" % g) for g in range(4)]
    small = sb("small", [128, 64], F32)
    NRING = 4
    ringt = sb("ring", [128, NRING, 8, 128], BF16)
    ring = Ring([ringt[:, i] for i in range(NRING)], "ring")
    junk = sb("junk", [128, S], mybir.dt.uint8)
    junkb = Buf("junk")
    wvw = sb("wvw", [128, 8, 72], BF16)
    wvwb = Buf("wvw")
    ARENA = 45056
    arena = sb("arena", [128, ARENA // 4], F32)

    def carve(off, shape, dt):
        n = int(np.prod(shape))
        sz = 4 if dt == F32 else 2
        assert off % 4 == 0 and off + n * sz <= ARENA, (off, shape)
        a = arena[:, off // 4:(off + n * sz + 3) // 4]
        if dt != F32:
            a = a.bitcast(dt)
        a = a[:, 0:n]
        if len(shape) == 2:
            a = a.rearrange("p (a b) -> p a b", a=shape[0])
        elif len(shape) == 3:
            a = a.rearrange("p (a b c) -> p a b c", a=shape[0], b=shape[1])
        return a

    ps = [nc.alloc_psum_tensor("ps%d" % i, [128, 512], F32) for i in range(8)]
    psb = [Buf("ps%d" % i, excl=True) for i in range(8)]
    psrot = {"ids": list(range(8)), "i": 0}

    def next_ps():
        ids = psrot["ids"]
        j = ids[psrot["i"] % len(ids)]
        psrot["i"] += 1
        return ps[j], psb[j]

    SM_LO, SM_HI, SM_W0, SM_MID, SM_CNT, SM_TMP, SM_THR = 0, 1, 2, 3, 4, 5, 6
    SM_HW = 8
    SM_REC = 40
    SM_WIDX = 48
    smb = Buf("small")
    recb = Buf("rec")
    widxb = [Buf("widx0"), Buf("widx1")]

    k.dma("sp", vec[:, :], vecs_d[:, :], w=[vecb])
    k.dma("sp", cst[:, :], cst_d[:, :], w=[cstb])
    k.op("dve", lambda e: e.tensor_copy(out=ident[:, :], in_=cst[:, 0:128]), r=[cstb], w=[constb])
    for h in range(8):
        k.op("dve", lambda e: e.tensor_copy(out=identrep[:, h, :], in_=cst[:, 0:128]), r=[cstb], w=[constb])
    k.op("dve", lambda e: e.memset(ones[:, :], 1.0), w=[constb])
    k.op("dve", lambda e: e.memset(V65[:, :, :], 1.0), w=V65b)
    for c in range(8):
        k.dma("sp", xT[:, c, :], xT_d[c * 128:(c + 1) * 128, :], w=xb[c])
    for l in range(L):
        k.op("act", lambda e: e.activation(out=nsp8[:, l * 4:l * 4 + 4],
                                           in_=vec[:, l * NV + A_PARAM:l * NV + A_PARAM + 4],
                                           func=AF.Exp, scale=-1.0), r=[vecb], w=[nsp8b])
    k.op("act", lambda e: e.activation(out=nsp8[:, :], in_=nsp8[:, :], func=AF.Ln, bias=1.0),
         r=[nsp8b], w=[nsp8b])
    k.op("dve", lambda e: e.tensor_scalar(out=nsp8[:, :], in0=nsp8[:, :], scalar1=-8.0, scalar2=None,
                                          op0=ALU.mult), r=[nsp8b], w=[nsp8b])

    cmask = cst[:, 128:256]
    pow2 = cst[:, 256:256 + IT + 1]
    invcnt = cst[:, 288:304]

    def tap(name, ap, bufs):
        if not dbg:
            return
        t = nc.dram_tensor("tap_" + name, list(ap.shape), ap.dtype, kind="ExternalOutput").ap()
        taps[name] = t
        k.dma("sp", t, ap, r=bufs)

    def load_w(scr, blk, kc):
        ap, buf = ring.next()
        k.dma("sp", ap[:, 0:kc, :], scr[blk][:, 0:kc, :], w=[buf])
        return ap, buf

    def wview(w2d):
        return w2d.rearrange("(kc p) n -> p kc n", p=128)

    def mm(out, lhsT, rhs, start, stop, r, w, skip=False):
        if skip:
            k.op("pe", lambda e: e.matmul(out, lhsT, rhs, start=start, stop=stop, skip_group_check=True),
                 r=r, w=w)
        else:
            k.op("pe", lambda e: e.matmul(out, lhsT, rhs, start=start, stop=stop), r=r, w=w)

    def rms_rstd(srcs, T, sqring, rstd, rstdb):
        pa, pb = next_ps()
        n = len(srcs)
        for c, (ap, bufs) in enumerate(srcs):
            sq, sqb = sqring.next()
            k.op("act", lambda e: e.activation(out=sq[:, 0:T], in_=ap, func=AF.Square), r=bufs, w=[sqb])
            mm(pa[:, 0:T], ones[:, :], sq[:, 0:T], c == 0, c == n - 1, [sqb, constb], [pb])
        k.op("act", lambda e: e.activation(out=rstd, in_=pa[:, 0:T], func=AF.Sqrt, scale=1.0 / D,
                                           bias=eps_ap), r=[pb, constb], w=[rstdb])
        k.op("dve", lambda e: e.reciprocal(out=rstd, in_=rstd), r=[rstdb], w=[rstdb])

    eps_t = sb("eps", [128, 1], F32)
    eps_ap = eps_t[:, 0:1]
    k.op("dve", lambda e: e.memset(eps_t[:, :], RMS_EPS), w=[constb])

    def pre_norm(l, gbase, t0, T, hT, hTb, sqring, rstd, rstdb):
        srcs = [(xT[:, c, t0:t0 + T], xbufs(c, t0, T)) for c in range(8)]
        rms_rstd(srcs, T, sqring, rstd, rstdb)
        for c in range(8):
            k.op("dve", lambda e: e.scalar_tensor_tensor(
                out=hT[:, c, :], in0=xT[:, c, t0:t0 + T],
                scalar=vec[:, l * NV + gbase + c:l * NV + gbase + c + 1], in1=rstd,
                op0=ALU.mult, op1=ALU.mult), r=xbufs(c, t0, T) + [rstdb, vecb], w=[hTb[c]])

    def post_norm_res(l, gbase, t0, T, ytmp, ytb, sqring, rstd, rstdb):
        srcs = [(ytmp[:, c, :], [ytb[c]]) for c in range(8)]
        rms_rstd(srcs, T, sqring, rstd, rstdb)
        for c in range(8):
            k.op("dve", lambda e: e.scalar_tensor_tensor(
                out=ytmp[:, c, :], in0=ytmp[:, c, :],
                scalar=vec[:, l * NV + gbase + c:l * NV + gbase + c + 1], in1=rstd,
                op0=ALU.mult, op1=ALU.mult), r=[ytb[c], rstdb, vecb], w=[ytb[c]])
            k.op("dve", lambda e: e.tensor_tensor(out=xT[:, c, t0:t0 + T], in0=xT[:, c, t0:t0 + T],
                                                  in1=ytmp[:, c, :], op=ALU.add),
                 r=[ytb[c]] + xbufs(c, t0, T), w=xbufs(c, t0, T))

    def scratch(name, nblk):
        return nc.dram_tensor("scr_" + name, [LW, nblk, 128, 8, 128], BF16, kind="Internal").ap()

    SCR = dict(w_in=scratch("w_in", 46), w_branch=scratch("w_branch", 24), w_out=scratch("w_out", 8),
               w_mq=scratch("w_mq", 4), w_mkv=scratch("w_mkv", 8), w_mo=scratch("w_mo", 8),
               w_fi=scratch("w_fi", 44), w_fo0=scratch("w_fo0", 8), w_fo1=scratch("w_fo1", 8),
               w_fo2=scratch("w_fo2", 8))
    st32 = Ring([arena[:, i * 2048:(i + 1) * 2048].rearrange("p (k c) -> p k c", k=8) for i in range(2)], "st32")
    st16 = Ring([arena[:, 4096 + i * 1024:4096 + (i + 1) * 1024].bitcast(BF16).rearrange("p (k c) -> p k c", k=8)
                 for i in range(2)], "st16")
    cast_rr = {"i": 0}

    def convert(view, scr_l, blk0, kc, ncols):
        for c0 in range(0, ncols, 256):
            nco = min(256, ncols - c0)
            a32, b32 = st32.next()
            k.dma("sp", a32[:, 0:kc, 0:nco], view[:, :, c0:c0 + nco], w=[b32])
            a16, b16 = st16.next()
            eng = ("act", "dve", "pool")[cast_rr["i"] % 3]
            cast_rr["i"] += 1
            if eng == "act":
                k.op("act", lambda e: e.copy(out=a16[:, 0:kc, 0:nco], in_=a32[:, 0:kc, 0:nco]), r=[b32], w=[b16])
            else:
                k.op(eng, lambda e: e.tensor_copy(out=a16[:, 0:kc, 0:nco], in_=a32[:, 0:kc, 0:nco]), r=[b32], w=[b16])
            for b in range(nco // 128):
                k.dma("sp", scr_l[blk0 + c0 // 128 + b][:, 0:kc, :], a16[:, 0:kc, b * 128:(b + 1) * 128], r=[b16])

    for l in range(n_layers):
        convert(wview(w_in_d[l]), SCR["w_in"][l], 0, 8, DINP)
        for n in range(3):
            convert(wview(w_branch_d[l, n]), SCR["w_branch"][l], n * 8, 4, D)
        convert(wview(w_out_d[l]), SCR["w_out"][l], 0, 8, D)
        convert(wview(w_mq_d[l]), SCR["w_mq"][l], 0, 8, 512)
        convert(wview(w_mkv_d[l]), SCR["w_mkv"][l], 0, 8, 1024)
        convert(wview(w_mo_d[l]), SCR["w_mo"][l], 0, 4, D)
        convert(wview(w_fi_d[l]), SCR["w_fi"][l], 0, 8, 2 * DFF)
        wfo_v = wview(w_fo_d[l])
        for part, (k0, kn) in enumerate(((0, 8), (8, 8), (16, 6))):
            convert(wfo_v[:, k0:k0 + kn, :], SCR["w_fo%d" % part][l], 0, kn, D)
    k.barrier()

    def fence(new, old):
        ev = {}
        for b in old:
            for e in ([b.w] if b.w else []) + list(b.r.values()):
                if e[0].num not in ev or ev[e[0].num][1] < e[1]:
                    ev[e[0].num] = e
        for b in new:
            for key, e in ev.items():
                if key not in b.r or b.r[key][1] < e[1]:
                    b.r[key] = e

    chk_cnt = {}

    def chk(tag):
        chk_cnt[tag] = chk_cnt.get(tag, 0) + 1
        if stop is None:
            return
        st, _, n = stop.partition("@")
        if st == tag and chk_cnt[tag] == int(n or 1):
            raise _Stop()


    def _layers(n_layers):
        for l in range(n_layers):
            vb = l * NV
            k.barrier()
            stg, stgb = arena[:, 0:1536], Buf("stg")
            k.dma("sp", stg[:, 0:1024], bd_d[l], w=[stgb])
            k.dma("sp", stg[:, 1024:1536], wpool_d[l], w=[stgb])
            k.barrier()
            k.op("dve", lambda e: e.tensor_copy(out=bd[:, :], in_=stg[:, 0:1024]), r=[stgb], w=[bdb])
            k.op("dve", lambda e: e.tensor_copy(out=wpool[:, :], in_=stg[:, 1024:1536]), r=[stgb], w=[wpoolb])
            k.barrier()
            for g in range(4):
                k.op("dve", lambda e: e.memset(lruhalo[:, g, :], 0.0), w=[halob[g]])
                k.op("dve", lambda e: e.memset(poolhalo[:, g, :], 0.0), w=[phalob[g]])
                k.op("dve", lambda e: e.memset(carry[:, g:g + 1], 0.0), w=[carryb[g]])
            S_in = SCR["w_in"][l]
            chk("setup")
            T = TM

            score = arena[:, 0:S]
            mbuf = arena[:, S:S + S // 2].bitcast(BF16)
            scoreb, mbufb = Buf("score"), Buf("mbuf")
            o = 24576
            qq = carve(o, [8, T], BF16); o += 8 * T * 2
            qqb = [Buf("qq%d" % h) for h in range(8)]
            yab = [carve(o + n * 4 * T * 2, [4, T], BF16) for n in range(3)]; o += 3 * 4 * T * 2
            ybuf = [[Buf("y%d_%d" % (n, g)) for g in range(4)] for n in range(3)]
            o_free = o
            hT = carve(o_free, [8, T], BF16)
            hTb = [Buf("hT%d" % c) for c in range(8)]
            rstd = arena[:, (o_free + 4096) // 4:(o_free + 4096) // 4 + T]
            rstdb = Buf("rstd")
            sq_aps = [arena[:, (o_free + 5120 + i * 512) // 4:(o_free + 5120 + (i + 1) * 512) // 4].bitcast(BF16)
                      for i in range(2)]
            sqring = Ring(sq_aps, "sq")
            GS = 8256

            def gset(s):
                b = s * GS
                d = {}
                d["lrux"] = arena[:, b // 4:b // 4 + T + 4]; b += 1040
                d["graw"] = arena[:, b // 4:b // 4 + T]; b += 1024
                d["t1"] = arena[:, b // 4:b // 4 + T]; b += 1024
                d["ua"] = arena[:, b // 4:b // 4 + T]; b += 1024
                d["ra"] = arena[:, b // 4:b // 4 + T]; b += 1024
                d["ib"] = arena[:, b // 4:b // 4 + T]; b += 1024
                d["mb"] = arena[:, b // 4:b // 4 + T]; b += 1024
                d["uabf"] = arena[:, b // 4:b // 4 + T // 2].bitcast(BF16); b += 512
                d["ge"] = arena[:, b // 4:b // 4 + T // 2].bitcast(BF16); b += 512
                d["bufs"] = {n: Buf(n + str(s)) for n in ("lrux", "graw", "t1", "ua", "ra", "ib", "mb", "uabf", "ge")}
                return d

            def pset(s):
                b = 16576 + s * 3840
                d = {}
                d["px"] = arena[:, b // 4:b // 4 + T + 16]; b += 1088
                d["pa"] = arena[:, b // 4:b // 4 + T + 16]; b += 1088
                d["pb"] = arena[:, b // 4:b // 4 + T + 16]; b += 1088
                d["pooled"] = arena[:, b // 4:b // 4 + T // 2].bitcast(BF16); b += 512
                d["bufs"] = {n: Buf(n + str(s)) for n in ("px", "pa", "pb", "pooled")}
                return d
            gsets = [gset(0), gset(1)]
            psets = [pset(0), pset(1)]
            o2 = o_free
            relu_aps = []
            for i in range(3):
                relu_aps.append(arena[:, o2 // 4:o2 // 4 + 256].bitcast(BF16)); o2 += 1024
            reluring = Ring(relu_aps, "relu")
            pt_aps = []
            for i in range(4):
                pt_aps.append(arena[:, o2 // 4:o2 // 4 + 256].bitcast(BF16)); o2 += 1024
            ptring = Ring(pt_aps, "pt")
            diag = arena[:, o2 // 4:o2 // 4 + 512].bitcast(BF16).rearrange("p (h q) -> p h q", h=8); o2 += 2048
            diagb = Buf("diag")
            yctm = arena[:, o2 // 4:o2 // 4 + 256].bitcast(BF16); o2 += 1024
            yctmb = Buf("yctm")
            assert o2 <= ARENA
            merged = carve(0, [8, T], BF16)
            mergedb = [Buf("mg%d" % c) for c in range(8)]
            ytmpM = carve(4096, [8, T], F32)
            ytbM = [Buf("yt%d" % c) for c in range(8)]
            acc = arena[:, 12288 // 4:12288 // 4 + T]
            accb = Buf("acc")
            gt_aps = [arena[:, (13312 + i * 1024) // 4:(13312 + i * 1024) // 4 + T] for i in range(2)]
            gtring = Ring(gt_aps, "gt")
            tm_aps = [arena[:, (15360 + i * 1024) // 4:(15360 + i * 1024) // 4 + T] for i in range(2)]
            tmring = Ring(tm_aps, "tm")

            G_A = [b for gs_ in gsets for b in gs_["bufs"].values()] + [b for ps_ in psets for b in ps_["bufs"].values()]
            G_D = [scoreb, mbufb]
            G_M = mergedb + ytbM + [accb] + gtring.bufs + tmring.bufs
            H_1 = hTb + [rstdb] + sqring.bufs
            H_D = reluring.bufs + ptring.bufs + [diagb, yctmb]
            for tt in range(S // T):
                t0 = tt * T
                if tt == 0:
                    k.barrier()
                else:
                    fence(G_A, G_M)
                psrot["ids"] = list(range(8))
                pre_norm(l, G_MIX_PRE, t0, T, hT, hTb, sqring, rstd, rstdb)

                chk("m1")
                def proj(blk, evac):
                    wap, wb = load_w(S_in, blk, 8)
                    pa, pb = next_ps()
                    for kc in range(8):
                        mm(pa[:, 0:T], wap[:, kc, :], hT[:, kc, :], kc == 0, kc == 7, [wb, hTb[kc]], [pb])
                    evac(pa, pb)

                for g in range(4):
                    gs = gsets[g % 2]
                    gb = gs["bufs"]
                    lrux = gs["lrux"]
                    k.op("act", lambda e: e.copy(out=lrux[:, 0:3], in_=lruhalo[:, g, 0:3]), r=[halob[g]], w=[gb["lrux"]])
                    proj(g, lambda pa, pb: k.op("act", lambda e: e.copy(out=lrux[:, 3:3 + T], in_=pa[:, 0:T]),
                                                r=[pb], w=[gb["lrux"]]))
                    k.op("act", lambda e: e.copy(out=lruhalo[:, g, 0:3], in_=lrux[:, T:T + 3]), r=[gb["lrux"]], w=[halob[g]])
                    proj(4 + g, lambda pa, pb: k.op("act", lambda e: e.copy(out=gs["graw"], in_=pa[:, 0:T]),
                                                    r=[pb], w=[gb["graw"]]))
                    graw, t1 = gs["graw"], gs["t1"]
                    k.op("dve", lambda e: e.tensor_tensor(out=t1, in0=graw, in1=graw, op=ALU.mult), r=[gb["graw"]], w=[gb["t1"]])
                    k.op("dve", lambda e: e.tensor_scalar(out=t1, in0=t1, scalar1=0.044715, scalar2=1.0, op0=ALU.mult, op1=ALU.add),
                         r=[gb["t1"]], w=[gb["t1"]])
                    k.op("dve", lambda e: e.tensor_tensor(out=t1, in0=t1, in1=graw, op=ALU.mult), r=[gb["t1"], gb["graw"]], w=[gb["t1"]])
                    k.op("act", lambda e: e.activation(out=t1, in_=t1, func=AF.Sigmoid, scale=1.5957691216057308),
                         r=[gb["t1"]], w=[gb["t1"]])
                    k.op("dve", lambda e: e.tensor_tensor(out=gs["ge"], in0=t1, in1=graw, op=ALU.mult),
                         r=[gb["t1"], gb["graw"]], w=[gb["ge"]])
                    ua = gs["ua"]
                    cw = lambda kk: vec[:, vb + CONV_W + kk * 4 + g:vb + CONV_W + kk * 4 + g + 1]
                    k.op("act", lambda e: e.activation(out=ua, in_=lrux[:, 0:T], func=AF.Identity, scale=cw(0),
                                                       bias=vec[:, vb + CONV_B + g:vb + CONV_B + g + 1]),
                         r=[gb["lrux"], vecb], w=[gb["ua"]])
                    for kk in (1, 2, 3):
                        k.op("dve", lambda e: e.scalar_tensor_tensor(out=ua, in0=lrux[:, kk:kk + T], scalar=cw(kk), in1=ua,
                                                                     op0=ALU.mult, op1=ALU.add),
                             r=[gb["lrux"], gb["ua"], vecb], w=[gb["ua"]])
                    k.op("act", lambda e: e.copy(out=gs["uabf"], in_=ua), r=[gb["ua"]], w=[gb["uabf"]])
                    for which, dst, bcol in ((0, "ra", B_A), (1, "ib", B_X)):
                        pa, pb = next_ps()
                        mm(pa[:, 0:T], bd[:, (g * 2 + which) * 128:(g * 2 + which + 1) * 128], gs["uabf"], True, True,
                           [bdb, gb["uabf"]], [pb])
                        k.op("act", lambda e: e.activation(out=gs[dst], in_=pa[:, 0:T], func=AF.Sigmoid,
                                                           bias=vec[:, vb + bcol + g:vb + bcol + g + 1]),
                             r=[pb, vecb], w=[gb[dst]])
                    ra, ib, mb = gs["ra"], gs["ib"], gs["mb"]
                    k.op("act", lambda e: e.activation(out=ra, in_=ra, func=AF.Exp, scale=nsp8[:, l * 4 + g:l * 4 + g + 1]),
                         r=[gb["ra"], nsp8b], w=[gb["ra"]])
                    k.op("dve", lambda e: e.tensor_tensor(out=mb, in0=ra, in1=ra, op=ALU.mult), r=[gb["ra"]], w=[gb["mb"]])
                    k.op("dve", lambda e: e.tensor_scalar(out=mb, in0=mb, scalar1=-1.0, scalar2=1.0, op0=ALU.mult, op1=ALU.add),
                         r=[gb["mb"]], w=[gb["mb"]])
                    k.op("dve", lambda e: e.tensor_scalar(out=mb, in0=mb, scalar1=0.0, scalar2=None, op0=ALU.max),
                         r=[gb["mb"]], w=[gb["mb"]])
                    k.op("act", lambda e: e.activation(out=mb, in_=mb, func=AF.Sqrt), r=[gb["mb"]], w=[gb["mb"]])
                    k.op("dve", lambda e: e.tensor_tensor(out=ib, in0=ib, in1=ua, op=ALU.mult), r=[gb["ib"], gb["ua"]], w=[gb["ib"]])
                    k.op("dve", lambda e: e.tensor_tensor(out=ib, in0=ib, in1=mb, op=ALU.mult), r=[gb["ib"], gb["mb"]], w=[gb["ib"]])
                    k.op("dve", lambda e: e.tensor_tensor_scan(out=mb, data0=ra, data1=ib, initial=carry[:, g:g + 1],
                                                               op0=ALU.mult, op1=ALU.add),
                         r=[gb["ra"], gb["ib"], carryb[g]], w=[gb["mb"]])
                    k.op("dve", lambda e: e.tensor_copy(out=carry[:, g:g + 1], in_=mb[:, T - 1:T]), r=[gb["mb"]], w=[carryb[g]])
                    k.op("dve", lambda e: e.tensor_tensor(out=yab[0][:, g, :], in0=mb, in1=gs["ge"], op=ALU.mult),
                         r=[gb["mb"], gb["ge"]], w=[ybuf[0][g]])

                chk("A")
                for g in range(4):
                    pset_ = psets[g % 2]
                    pbf = pset_["bufs"]
                    px, pA, pB, pooled = pset_["px"], pset_["pa"], pset_["pb"], pset_["pooled"]
                    k.op("act", lambda e: e.copy(out=px[:, 0:15], in_=poolhalo[:, g, 0:15]), r=[phalob[g]], w=[pbf["px"]])
                    proj(8 + g, lambda pa, pb: k.op("act", lambda e: e.copy(out=px[:, 15:15 + T], in_=pa[:, 0:T]),
                                                    r=[pb], w=[pbf["px"]]))
                    k.op("act", lambda e: e.copy(out=poolhalo[:, g, 0:15], in_=px[:, T:T + 15]), r=[pbf["px"]], w=[phalob[g]])
                    Wd = T + 15
                    cur, curb = px, pbf["px"]
                    seq = [(pA, pbf["pa"]), (pB, pbf["pb"]), (pA, pbf["pa"]), (pB, pbf["pb"])]
                    sh = 1
                    for step in range(g + 1):
                        dst, dstb = seq[step]
                        k.op("dve", lambda e: e.tensor_tensor(out=dst[:, sh:Wd], in0=cur[:, sh:Wd], in1=cur[:, 0:Wd - sh], op=ALU.add),
                             r=[curb], w=[dstb])
                        cur, curb = dst, dstb
                        sh *= 2
                    wdw = 2 ** (g + 1)
                    k.op("dve", lambda e: e.scalar_tensor_tensor(out=pooled, in0=cur[:, 15:15 + T], scalar=1.0 / wdw,
                                                                 in1=px[:, 15:15 + T], op0=ALU.mult, op1=ALU.subtract),
                         r=[curb, pbf["px"]], w=[pbf["pooled"]])
                    if tt == 0:
                        nfix = wdw - 1
                        tmpf = pB[:, 0:nfix] if cur is not pB else pA[:, 0:nfix]
                        tmpb = pbf["pb"] if cur is not pB else pbf["pa"]
                        k.op("dve", lambda e: e.tensor_tensor(out=tmpf, in0=cur[:, 15:15 + nfix], in1=invcnt[:, 0:nfix], op=ALU.mult),
                             r=[curb, cstb], w=[tmpb])
                        k.op("dve", lambda e: e.tensor_tensor(out=pooled[:, 0:nfix], in0=tmpf, in1=px[:, 15:15 + nfix], op=ALU.subtract),
                             r=[tmpb, pbf["px"]], w=[pbf["pooled"]])
                    pa_, pb_ = next_ps()
                    mm(pa_[:, 0:T], wpool[:, g * 128:(g + 1) * 128], pooled, True, True, [wpoolb, pbf["pooled"]], [pb_])
                    k.op("act", lambda e: e.activation(out=yab[1][:, g, :], in_=pa_[:, 0:T], func=AF.Copy,
                                                       scale=vec[:, vb + POOL_SCALE + g:vb + POOL_SCALE + g + 1]),
                         r=[pb_, vecb], w=[ybuf[1][g]])

                chk("B")
                for h in range(8):
                    proj(12 + h, lambda pa, pb: k.op("act", lambda e: e.copy(out=qq[:, h, :], in_=pa[:, 0:T]),
                                                     r=[pb], w=[qqb[h]]))
                chk("q")
                proj(20, lambda pa, pb: k.op("act", lambda e: e.copy(out=KK[:, t0:t0 + T], in_=pa[:, 0:T]),
                                             r=[pb], w=[KKb[tt]]))
                chk("kk")
                k.dma("sp", wvw[:, :, :], S_in[21][:, :, 0:72], w=[wvwb])
                chk("wvw")
                for i in range(T // 128):
                    Bg = t0 // 128 + i
                    pa, pb = next_ps()
                    for kc in range(8):
                        mm(pa[:, 0:72], hT[:, kc, i * 128:(i + 1) * 128], wvw[:, kc, :], kc == 0, kc == 7,
                           [wvwb, hTb[kc]], [pb])
                    k.op("act", lambda e: e.copy(out=V65[:, Bg, 0:64], in_=pa[:, 0:64]), r=[pb], w=[V65b[Bg]])
                    k.op("act", lambda e: e.activation(out=small[:, SM_WIDX + i * 8:SM_WIDX + i * 8 + 8], in_=pa[:, 64:72],
                                                       func=AF.Copy, scale=float(8 ** -0.5 * 64 ** -0.5)),
                         r=[pb], w=[widxb[i]])

                chk("qkv")
                fence(G_D, G_A)
                fence(H_D, H_1)
                psO = [ps[6], ps[7]]
                psOb = [psb[6], psb[7]]
                nqb = T // 128

                def d_index(i):
                    Bq = t0 // 128 + i
                    N = (Bq + 1) * 128
                    qc = slice(i * 128, (i + 1) * 128)
                    for h in range(8):
                        k.op("dve", lambda e: e.tensor_scalar(out=diag[:, h, :], in0=ident[:, :],
                                                              scalar1=small[:, SM_WIDX + i * 8 + h:SM_WIDX + i * 8 + h + 1],
                                                              scalar2=None, op0=ALU.mult),
                             r=[constb, widxb[i]], w=[diagb])
                    psrot["ids"] = [0, 1, 2, 3]
                    for ci, c0 in enumerate(range(0, N, 512)):
                        Wc = min(512, N - c0)
                        kb_ = [KKb[j] for j in range(c0 // TM, (c0 + Wc + TM - 1) // TM)]
                        sca, scb = ps[4 + ci % 2], psb[4 + ci % 2]
                        pend = None
                        for h in range(8):
                            la, lb = next_ps()
                            mm(la[:, 0:Wc], qq[64:128, h, qc], KK[64:128, c0:c0 + Wc], True, True, [qqb[h]] + kb_, [lb])
                            rl, rlb = reluring.next()
                            k.op("act", lambda e: e.activation(out=rl[:, 0:Wc], in_=la[:, 0:Wc], func=AF.Relu), r=[lb], w=[rlb])
                            if pend is not None:
                                ph, prl, prlb = pend
                                mm(sca[:, 0:Wc], diag[:, ph, :], prl[:, 0:Wc], ph == 0, False, [diagb, prlb], [scb])
                            pend = (h, rl, rlb)
                        ph, prl, prlb = pend
                        mm(sca[:, 0:Wc], diag[:, ph, :], prl[:, 0:Wc], False, True, [diagb, prlb], [scb])
                        k.op("dve", lambda e: e.tensor_copy(out=score[:, c0:c0 + Wc], in_=sca[:, 0:Wc]), r=[scb], w=[scoreb])
                    k.op("dve", lambda e: e.tensor_tensor(out=score[:, N - 128:N], in0=score[:, N - 128:N], in1=cmask, op=ALU.add),
                         r=[scoreb, cstb], w=[scoreb])

                def d_thr(i):
                    Bq = t0 // 128 + i
                    N = (Bq + 1) * 128
                    thr = small[:, SM_LO:SM_LO + 1]
                    if Bq < 2:
                        k.op("dve", lambda e: e.memset(thr, -1e29), w=[smb])
                        return
                    lo, hi, w0 = thr, small[:, SM_HI:SM_HI + 1], small[:, SM_W0:SM_W0 + 1]
                    mid, cnt, tmp = small[:, SM_MID:SM_MID + 1], small[:, SM_CNT:SM_CNT + 1], small[:, SM_TMP:SM_TMP + 1]
                    hw = small[:, SM_HW:SM_HW + IT + 1]
                    k.op("dve", lambda e: e.tensor_reduce(out=lo, in_=score[:, 0:N - 64], axis=AX.X, op=ALU.min), r=[scoreb], w=[smb])
                    k.op("dve", lambda e: e.tensor_reduce(out=hi, in_=score[:, 0:N], axis=AX.X, op=ALU.max), r=[scoreb, smb], w=[smb])
                    k.op("dve", lambda e: e.tensor_tensor(out=w0, in0=hi, in1=lo, op=ALU.subtract), r=[smb], w=[smb])
                    k.op("dve", lambda e: e.tensor_scalar(out=hw, in0=pow2, scalar1=w0, scalar2=None, op0=ALU.mult), r=[smb, cstb], w=[smb])
                    k.op("dve", lambda e: e.tensor_tensor(out=mid, in0=lo, in1=hw[:, 0:1], op=ALU.add), r=[smb], w=[smb])
                    for it in range(IT):
                        k.op("dve", lambda e: e.tensor_scalar(out=junk[:, 0:N], in0=score[:, 0:N], scalar1=mid, scalar2=None,
                                                              op0=ALU.is_ge, op1=ALU.add, accum_out=cnt),
                             r=[scoreb, smb, junkb], w=[junkb, smb])
                        k.op("dve", lambda e: e.tensor_scalar(out=tmp, in0=cnt, scalar1=255.5, scalar2=hw[:, it:it + 1],
                                                              op0=ALU.is_ge, op1=ALU.mult), r=[smb], w=[smb])
                        k.op("dve", lambda e: e.scalar_tensor_tensor(out=mid, in0=tmp, scalar=hw[:, it + 1:it + 2], in1=mid,
                                                                     op0=ALU.subtract, op1=ALU.add), r=[smb], w=[smb])
                    k.op("dve", lambda e: e.tensor_tensor(out=lo, in0=mid, in1=hw[:, IT:IT + 1], op=ALU.subtract), r=[smb], w=[smb])

                def d_mb(i):
                    Bq = t0 // 128 + i
                    N = (Bq + 1) * 128
                    thr = small[:, SM_LO:SM_LO + 1]
                    k.op("dve", lambda e: e.tensor_scalar(out=mbuf[:, 0:N], in0=score[:, 0:N], scalar1=thr, scalar2=-30000.0,
                                                          op0=ALU.is_lt, op1=ALU.mult), r=[scoreb, smb, mbufb], w=[mbufb])

                def d_att(i):
                    Bq = t0 // 128 + i
                    qc = slice(i * 128, (i + 1) * 128)
                    psrot["ids"] = [0, 1, 2, 3, 4, 5]

                    def st_pair(j):
                        out = []
                        kbj = [KKb[(j * 128) // TM]]
                        for half in range(2):
                            sa, sbf = next_ps()
                            mm(sa[:, 0:512], KK[0:64, j * 128:(j + 1) * 128], qq[0:64, 4 * half:4 * half + 4, qc], True, False,
                               kbj + qqb[4 * half:4 * half + 4], [sbf])
                            mm(sa[:, 0:512], mbuf[:, j * 128:(j + 1) * 128], identrep[:, 0:4, :], False, True,
                               [mbufb, constb], [sbf])
                            pt, ptb = ptring.next()
                            k.op("act", lambda e: e.activation(out=pt[:, 0:512], in_=sa[:, 0:512], func=AF.Exp, scale=0.125),
                                 r=[sbf], w=[ptb])
                            out.append((pt, ptb))
                        return out

                    def pv_pair(j, pts):
                        for half in range(2):
                            pt, ptb = pts[half]
                            for hh in range(4):
                                mm(psO[half][:, hh * 65:(hh + 1) * 65], pt[:, hh * 128:(hh + 1) * 128], V65[:, j, 0:65],
                                   (j == 0 and hh == 0), (j == Bq and hh == 3), [ptb, V65b[j]], [psOb[half]], skip=True)
                    prev = None
                    for j in range(Bq + 1):
                        cur = st_pair(j)
                        if prev is not None:
                            pv_pair(j - 1, prev)
                        prev = cur
                    pv_pair(Bq, prev)
                    for half in range(2):
                        o3 = psO[half][:, 0:260].rearrange("p (h e) -> p h e", e=65)
                        rec = small[:, SM_REC + 4 * half:SM_REC + 4 * half + 4]
                        k.op("dve", lambda e: e.reciprocal(out=rec, in_=o3[:, :, 64]), r=[psOb[half]], w=[recb])
                        yv = yctm[:, half * 256:(half + 1) * 256].rearrange("p (h d) -> p h d", h=4)
                        k.op("dve", lambda e: e.tensor_tensor(out=yv, in0=o3[:, :, 0:64],
                                                              in1=rec.unsqueeze(2).to_broadcast([128, 4, 64]), op=ALU.mult),
                             r=[psOb[half], recb], w=[yctmb])
                    ta, tb = next_ps()
                    tab = ta[:, :].bitcast(BF16)
                    for j4 in range(4):
                        k.op("pe", lambda e: e.transpose(tab[:, j4 * 128:(j4 + 1) * 128], yctm[:, j4 * 128:(j4 + 1) * 128], ident[:, :]),
                             r=[yctmb, constb], w=[tb])
                    k.op("act", lambda e: e.copy(out=yab[2][:, :, qc], in_=tab[:, 0:512].rearrange("p (j q) -> p j q", j=4)),
                         r=[tb], w=ybuf[2])

                d_index(0)
                d_thr(0)
                d_mb(0)
                for i in range(1, nqb):
                    d_index(i)
                    d_thr(i)
                    d_att(i - 1)
                    d_mb(i)
                d_att(nqb - 1)
                chk("dsa")
                fence(G_M, G_D)
                fence(H_1, H_D)
                psrot["ids"] = list(range(8))
                pre_norm(l, G_MIX_PRE, t0, T, hT, hTb, sqring, rstd, rstdb)
                for c in range(8):
                    for n in range(3):
                        wa_, wb_ = load_w(SCR["w_branch"][l], n * 8 + c, 4)
                        ua_, ub_ = next_ps()
                        for kc in range(4):
                            mm(ua_[:, 0:T], wa_[:, kc, :], yab[n][:, kc, :], kc == 0, kc == 3, [wb_, ybuf[n][kc]], [ub_])
                        wg_, wgb_ = load_w(S_in, GATE_BLK0 + n * 8 + c, 8)
                        ga_, gb_ = next_ps()
                        for kc in range(8):
                            mm(ga_[:, 0:T], wg_[:, kc, :], hT[:, kc, :], kc == 0, kc == 7, [wgb_, hTb[kc]], [gb_])
                        gt, gtb = gtring.next()
                        k.op("act", lambda e: e.activation(out=gt, in_=ga_[:, 0:T], func=AF.Sigmoid,
                                                           bias=vec[:, vb + B_GATE + n * 8 + c:vb + B_GATE + n * 8 + c + 1]),
                             r=[gb_, vecb], w=[gtb])
                        if n == 0:
                            k.op("dve", lambda e: e.tensor_tensor(out=acc, in0=ua_[:, 0:T], in1=gt, op=ALU.mult), r=[ub_, gtb], w=[accb])
                        else:
                            tm_, tmb_ = tmring.next()
                            k.op("dve", lambda e: e.tensor_tensor(out=tm_, in0=ua_[:, 0:T], in1=gt, op=ALU.mult), r=[ub_, gtb], w=[tmb_])
                            if n == 1:
                                k.op("dve", lambda e: e.tensor_tensor(out=acc, in0=acc, in1=tm_, op=ALU.add), r=[accb, tmb_], w=[accb])
                            else:
                                k.op("dve", lambda e: e.tensor_tensor(out=merged[:, c, :], in0=acc, in1=tm_, op=ALU.add),
                                     r=[accb, tmb_], w=[mergedb[c]])
                chk("m6")
                for c in range(8):
                    wa_, wb_ = load_w(SCR["w_out"][l], c, 8)
                    pa, pb = next_ps()
                    for kc in range(8):
                        mm(pa[:, 0:T], wa_[:, kc, :], merged[:, kc, :], kc == 0, kc == 7, [wb_, mergedb[kc]], [pb])
                    k.op("act", lambda e: e.copy(out=ytmpM[:, c, :], in_=pa[:, 0:T]), r=[pb], w=[ytbM[c]])
                post_norm_res(l, G_MIX_POST, t0, T, ytmpM, ytbM, sqring, rstd, rstdb)
                if dbg and l == 0 and tt == 1:
                    tap("ya", yab[0], ybuf[0]); tap("yb", yab[1], ybuf[1]); tap("yc", yab[2], ybuf[2])
                    tap("merged", merged, mergedb)

            chk("m7")
            k.barrier()
            T = TX
            psrot["ids"] = list(range(8))
            o = 0
            hT = carve(o, [8, T], BF16); o += 8192
            hTb = [Buf("hTx%d" % c) for c in range(8)]
            ytmp = carve(o, [8, T], F32); o += 16384
            ytb = [Buf("ytx%d" % c) for c in range(8)]
            rstd = arena[:, o // 4:o // 4 + T]; o += 2048
            rstdb = Buf("rstdx")
            sqring = Ring([arena[:, (o + i * 1024) // 4:(o + (i + 1) * 1024) // 4].bitcast(BF16) for i in range(2)], "sqx"); o += 2048
            qmT = carve(o, [4, T], BF16); o += 4096
            qmb = [Buf("qm%d" % h) for h in range(4)]
            omT = carve(o, [4, T], BF16); o += 4096
            omb = [Buf("om%d" % h) for h in range(4)]
            ptx = Ring([arena[:, (o + i * 1024) // 4:(o + (i + 1) * 1024) // 4].bitcast(BF16) for i in range(4)], "ptx"); o += 4096
            rden, rdenb = rstd, rstdb
            memKT = carve(o, [4, NMEM], BF16); o += 2048
            memV = carve(o, [2, 512], BF16); o += 2048
            memKb, memVb = Buf("memK"), Buf("memV")
            assert o <= ARENA, o
            mT = arena[:, 8192 // 4:8192 // 4 + 8 * NMEM].rearrange("p (c m) -> p c m", c=8)
            mTb = [Buf("mT%d" % c) for c in range(8)]
            mh = carve(0, [8, NMEM], BF16)
            mhb = [Buf("mh%d" % c) for c in range(8)]
            wkv = arena[:, 16384 // 4:16384 // 4 + 2048].bitcast(BF16).rearrange("p (c n) -> p c n", c=8)
            wkvb = Buf("wkv")
            for c in range(8):
                k.dma("sp", mT[:, c, :], memT_d[c * 128:(c + 1) * 128, :], w=[mTb[c]])
            for b4 in range(4):
                k.dma("sp", wkv[:, :, b4 * 128:(b4 + 1) * 128], SCR["w_mkv"][l][4 + b4], w=[wkvb])
            k.barrier()
            rms_rstd([(mT[:, c, :], [mTb[c]]) for c in range(8)], NMEM, sqring, rstd[:, 0:NMEM], rstdb)
            for c in range(8):
                k.op("dve", lambda e: e.scalar_tensor_tensor(out=mh[:, c, :], in0=mT[:, c, :],
                                                             scalar=vec[:, vb + G_MEM_KV + c:vb + G_MEM_KV + c + 1],
                                                             in1=rstd[:, 0:NMEM], op0=ALU.mult, op1=ALU.mult),
                     r=[mTb[c], rstdb, vecb], w=[mhb[c]])
            for h in range(4):
                wa_, wb_ = load_w(SCR["w_mkv"][l], h, 8)
                pa, pb = next_ps()
                for kc in range(8):
                    mm(pa[:, 0:NMEM], wa_[:, kc, :], mh[:, kc, :], kc == 0, kc == 7, [wb_, mhb[kc]], [pb])
                k.op("act", lambda e: e.copy(out=memKT[:, h, :], in_=pa[:, 0:NMEM]), r=[pb], w=[memKb])
            for mbk in range(2):
                pa, pb = next_ps()
                for kc in range(8):
                    mm(pa[:, 0:512], mh[:, kc, mbk * 128:(mbk + 1) * 128], wkv[:, kc, :], kc == 0, kc == 7, [wkvb, mhb[kc]], [pb])
                k.op("act", lambda e: e.copy(out=memV[:, mbk, :], in_=pa[:, 0:512]), r=[pb], w=[memVb])
            k.barrier()
            for tt in range(S // T):
                t0 = tt * T
                pre_norm(l, G_MEM_PRE, t0, T, hT, hTb, sqring, rstd, rstdb)
                for h in range(4):
                    wa_, wb_ = load_w(SCR["w_mq"][l], h, 8)
                    pa, pb = next_ps()
                    for kc in range(8):
                        mm(pa[:, 0:T], wa_[:, kc, :], hT[:, kc, :], kc == 0, kc == 7, [wb_, hTb[kc]], [pb])
                    k.op("act", lambda e: e.copy(out=qmT[:, h, :], in_=pa[:, 0:T]), r=[pb], w=[qmb[h]])
                for h in range(4):
                    pts = []
                    for mbk in range(2):
                        sa, sbf = next_ps()
                        mm(sa[:, 0:T], memKT[:, h, mbk * 128:(mbk + 1) * 128], qmT[:, h, :], True, True, [memKb, qmb[h]], [sbf])
                        pt, ptb = ptx.next()
                        k.op("act", lambda e: e.activation(out=pt[:, 0:T], in_=sa[:, 0:T], func=AF.Exp, scale=float(128 ** -0.5)),
                             r=[sbf], w=[ptb])
                        pts.append((pt, ptb))
                    da, db = next_ps()
                    oa, ob = next_ps()
                    for mbk in range(2):
                        pt, ptb = pts[mbk]
                        mm(da[:, 0:T], ones[:, :], pt[:, 0:T], mbk == 0, mbk == 1, [constb, ptb], [db])
                    for mbk in range(2):
                        pt, ptb = pts[mbk]
                        mm(oa[:, 0:T], memV[:, mbk, h * 128:(h + 1) * 128], pt[:, 0:T], mbk == 0, mbk == 1, [memVb, ptb], [ob])
                    k.op("dve", lambda e: e.reciprocal(out=rden, in_=da[:, 0:T]), r=[db], w=[rdenb])
                    k.op("dve", lambda e: e.tensor_tensor(out=omT[:, h, :], in0=oa[:, 0:T], in1=rden, op=ALU.mult),
                         r=[ob, rdenb], w=[omb[h]])
                for c in range(8):
                    wa_, wb_ = load_w(SCR["w_mo"][l], c, 4)
                    pa, pb = next_ps()
                    for kc in range(4):
                        mm(pa[:, 0:T], wa_[:, kc, :], omT[:, kc, :], kc == 0, kc == 3, [wb_, omb[kc]], [pb])
                    k.op("act", lambda e: e.copy(out=ytmp[:, c, :], in_=pa[:, 0:T]), r=[pb], w=[ytb[c]])
                post_norm_res(l, G_MEM_POST, t0, T, ytmp, ytb, sqring, rstd, rstdb)

            chk("x")
            k.barrier()
            o = 0
            hid = carve(o, [22, T], BF16); o += 22528
            hidb = [Buf("hid%d" % j) for j in range(22)]
            hT = carve(o, [8, T], BF16)
            hTb = [Buf("hTf%d" % c) for c in range(8)]
            sg_aps = [arena[:, (o + 8192 + i * 2048) // 4:(o + 8192 + (i + 1) * 2048) // 4] for i in range(2)]
            sgring = Ring(sg_aps, "sg")
            ytmp = carve(o, [8, T], F32); o += 16384
            ytb = [Buf("ytf%d" % c) for c in range(8)]
            rstd = arena[:, o // 4:o // 4 + T]; o += 2048
            rstdb = Buf("rstdf")
            sqring = Ring([arena[:, (o + i * 1024) // 4:(o + (i + 1) * 1024) // 4].bitcast(BF16) for i in range(2)], "sqf"); o += 2048
            assert o <= ARENA, o
            for tt in range(S // T):
                t0 = tt * T
                k.barrier()
                pre_norm(l, G_FFN_PRE, t0, T, hT, hTb, sqring, rstd, rstdb)
                for j in range(22):
                    wg_, wgb_ = load_w(SCR["w_fi"][l], j, 8)
                    ga_, gb_ = next_ps()
                    for kc in range(8):
                        mm(ga_[:, 0:T], wg_[:, kc, :], hT[:, kc, :], kc == 0, kc == 7, [wgb_, hTb[kc]], [gb_])
                    wu_, wub_ = load_w(SCR["w_fi"][l], 22 + j, 8)
                    ua_, ub_ = next_ps()
                    for kc in range(8):
                        mm(ua_[:, 0:T], wu_[:, kc, :], hT[:, kc, :], kc == 0, kc == 7, [wub_, hTb[kc]], [ub_])
                    sg, sgb = sgring.next()
                    k.op("act", lambda e: e.activation(out=sg, in_=ga_[:, 0:T], func=AF.Silu), r=[gb_], w=[sgb])
                    k.op("dve", lambda e: e.tensor_tensor(out=hid[:, j, :], in0=ua_[:, 0:T], in1=sg, op=ALU.mult),
                         r=[ub_, sgb], w=[hidb[j]])
                k.barrier()
                for c in range(8):
                    pa, pb = next_ps()
                    for part, (k0, kn) in enumerate(((0, 8), (8, 8), (16, 6))):
                        ap_, buf_ = load_w(SCR["w_fo%d" % part][l], c, kn)
                        for kc in range(kn):
                            mm(pa[:, 0:T], ap_[:, kc, :], hid[:, k0 + kc, :], (k0 + kc) == 0, (k0 + kc) == 21,
                               [buf_, hidb[k0 + kc]], [pb])
                    k.op("act", lambda e: e.copy(out=ytmp[:, c, :], in_=pa[:, 0:T]), r=[pb], w=[ytb[c]])
                post_norm_res(l, G_FFN_POST, t0, T, ytmp, ytb, sqring, rstd, rstdb)

    try:
        _layers(n_layers)
    except _Stop:
        pass

    k.barrier()
    outs = []
    for c in range(8):
        outs.append(k.dma("sp", yT_d[c * 128:(c + 1) * 128, :], xT[:, c, :], r=xb[c]))
    for sem, val, _ in outs:
        k._wait("sp", sem, val)
    return nc, taps, k


def _cols(v, n):
    return np.ascontiguousarray(v.reshape(n, 128).T)


def prep_inputs(x, mem, g_mix_pre, w_in, conv_w, conv_b, lru_w_a, lru_b_a, lru_w_x, lru_b_x,
                lru_a_param, w_pool, pool_scale, w_branch, b_gate, w_out, g_mix_post,
                g_mem_pre, g_mem_kv, w_mem_q, w_mem_kv, w_mem_o, g_mem_post,
                g_ffn_pre, w_ffn_in, w_ffn_out, g_ffn_post):
    f = lambda a: np.asarray(a, dtype=np.float32)
    L = DEPTH
    vecs = np.zeros((128, L * NV), np.float32)
    for l in range(L):
        b = l * NV
        for base, arr in ((G_MIX_PRE, g_mix_pre), (G_MIX_POST, g_mix_post), (G_MEM_PRE, g_mem_pre),
                          (G_MEM_KV, g_mem_kv), (G_MEM_POST, g_mem_post), (G_FFN_PRE, g_ffn_pre),
                          (G_FFN_POST, g_ffn_post)):
            vecs[:, b + base:b + base + 8] = _cols(f(arr)[l], 8)
        for kk in range(4):
            vecs[:, b + CONV_W + kk * 4:b + CONV_W + kk * 4 + 4] = _cols(f(conv_w)[l, kk], 4)
        vecs[:, b + CONV_B:b + CONV_B + 4] = _cols(f(conv_b)[l], 4)
        vecs[:, b + B_A:b + B_A + 4] = _cols(f(lru_b_a)[l], 4)
        vecs[:, b + B_X:b + B_X + 4] = _cols(f(lru_b_x)[l], 4)
        vecs[:, b + A_PARAM:b + A_PARAM + 4] = _cols(f(lru_a_param)[l], 4)
        vecs[:, b + POOL_SCALE:b + POOL_SCALE + 4] = _cols(f(pool_scale)[l], 4)
        for n in range(3):
            vecs[:, b + B_GATE + n * 8:b + B_GATE + n * 8 + 8] = _cols(f(b_gate)[l, n], 8)
    cst = np.zeros((128, NCST), np.float32)
    cst[:, 0:128] = np.eye(128, dtype=np.float32)
    cst[0:64, 128 + 64:256] = -1e30
    cst[:, 256:256 + IT + 1] = (0.5 ** np.arange(1, IT + 2, dtype=np.float64)).astype(np.float32)[None, :]
    cst[:, 288:304] = (1.0 / np.arange(1, 17, dtype=np.float64)).astype(np.float32)[None, :]
    perm = list(range(0, 1536))
    for h in range(8):
        perm += list(range(1536 + 64 * h, 1536 + 64 * h + 64)) + list(range(2176 + 64 * h, 2176 + 64 * h + 64))
    perm += list(range(2048, 2112)) + list(range(2688, 2752)) + list(range(2112, 2176)) + list(range(2752, 2760))
    perm += list(range(2760, 5832))
    assert len(perm) == DIN
    w_in_p = np.zeros((L, D, DINP), np.float32)
    w_in_p[:, :, 0:2760] = f(w_in)[:, :, perm[:2760]]
    w_in_p[:, :, 2816:] = f(w_in)[:, :, 2760:]
    bd = np.zeros((L, 128, 1024), np.float32)
    for g in range(4):
        for which, wsrc in ((0, f(lru_w_a)), (1, f(lru_w_x))):
            c0 = (g * 2 + which) * 128
            bd[:, 0:64, c0:c0 + 64] = wsrc[:, 2 * g]
            bd[:, 64:128, c0 + 64:c0 + 128] = wsrc[:, 2 * g + 1]
    wp = np.ascontiguousarray(np.transpose(f(w_pool), (0, 2, 1, 3)).reshape(L, 128, 512))
    shared = dict(vecs=vecs, cst=cst, w_in=w_in_p, lru_bd=bd, w_pool=wp, w_branch=f(w_branch), w_out=f(w_out),
                  w_mem_q=f(w_mem_q), w_mem_kv=f(w_mem_kv), w_mem_o=f(w_mem_o), w_ffn_in=f(w_ffn_in),
                  w_ffn_out=f(w_ffn_out))
    xs = [np.ascontiguousarray(f(x)[b].T) for b in range(NB)]
    ms = [np.ascontiguousarray(f(mem)[b].T) for b in range(NB)]
    return shared, xs, ms


N_LAUNCH = 2


def kernel(**inputs):
    shared, xs, ms = prep_inputs(**inputs)
    lpl = DEPTH // N_LAUNCH
    nc, _, _ = build_nc(n_layers=lpl)
    NCORE = 4
    x_cur = xs
    for part in range(N_LAUNCH):
        sh = {}
        for kk, v in shared.items():
            sh[kk] = np.ascontiguousarray(v[part * lpl:(part + 1) * lpl]) if v.ndim >= 3 else v
        vh = np.zeros_like(shared["vecs"])
        vh[:, 0:lpl * NV] = shared["vecs"][:, part * lpl * NV:(part + 1) * lpl * NV]
        sh["vecs"] = vh
        in_maps = []
        for core in range(NCORE):
            m = dict(sh)
            m["xT"] = x_cur[core % NB]
            m["memT"] = ms[core % NB]
            in_maps.append(m)
        res = run_bass_kernel_spmd(nc, in_maps, core_ids=list(range(NCORE)))
        x_cur = [np.ascontiguousarray(res.results[b]["yT"]) for b in range(NB)]
    out = np.stack([np.ascontiguousarray(x_cur[b].T) for b in range(NB)], axis=0)
    return out.astype(np.float32)
```

```python
import numpy as np
import concourse.bass as bass
import concourse.mybir as mybir
from concourse.bass_utils import run_bass_kernel_spmd

F32 = mybir.dt.float32
BF16 = mybir.dt.bfloat16
AF = mybir.ActivationFunctionType
ALU = mybir.AluOpType
AX = mybir.AxisListType

D = 1024
S = 4096
NB = 4
DEPTH = 4
NMEM = 256
DIN = 5832
DINP = 5888
DFF = 2816
NV = 116
IT = 22
TM = 256
TX = 512
NCST = 128 + 128 + 32 + 16
G_MIX_PRE, G_MIX_POST, G_MEM_PRE, G_MEM_KV, G_MEM_POST, G_FFN_PRE, G_FFN_POST = 0, 8, 16, 24, 32, 40, 48
CONV_W, CONV_B, B_A, B_X, A_PARAM, POOL_SCALE, B_GATE = 56, 72, 76, 80, 84, 88, 92
GATE_BLK0 = 22
RMS_EPS = 1e-6


class Buf:
    __slots__ = ("name", "w", "r", "excl")

    def __init__(self, name="", excl=False):
        self.name = name
        self.w = None
        self.r = {}
        self.excl = excl


class KB:
    def __init__(self, nc):
        self.nc = nc
        self.E = {}
        for nm, obj in (("pe", nc.tensor), ("act", nc.scalar), ("dve", nc.vector),
                        ("pool", nc.gpsimd), ("sp", nc.sync)):
            self.E[nm] = dict(obj=obj, sem=nc.alloc_semaphore("s_" + nm), n=0, known={})
        self.dsem = [[nc.alloc_semaphore("dq%d" % i), 0] for i in range(32)]
        self.di = 0
        self.ninst = 0

    def _wait(self, eng, sem, val):
        E = self.E[eng]
        if E["known"].get(sem.num, 0) < val:
            E["obj"].wait_ge(sem, val)
            E["known"][sem.num] = val

    def _deps(self, eng, r, w):
        deps = {}

        def add(ev):
            if ev is None:
                return
            sem, val, src = ev
            if src == "pe" and eng == "pe":
                return
            if sem.num not in deps or deps[sem.num][1] < val:
                deps[sem.num] = (sem, val)
        for b in r:
            add(b.w)
        for b in w:
            add(b.w)
            for ev in b.r.values():
                add(ev)
        for sem, val in deps.values():
            self._wait(eng, sem, val)

    @staticmethod
    def _mark(ev, r, w):
        for b in r:
            b.r[ev[0].num] = ev
        for b in w:
            b.w = ev
            b.r = {}

    def op(self, eng, fn, r=(), w=()):
        w = list(w) + [b for b in r if b.excl]
        r = [b for b in r if not b.excl]
        self._deps(eng, r, w)
        E = self.E[eng]
        ins = fn(E["obj"])
        E["n"] += 1
        ins.then_inc(E["sem"], 1)
        ev = (E["sem"], E["n"], eng)
        self._mark(ev, r, w)
        self.ninst += 1
        return ev

    def dma(self, q, out, in_, r=(), w=()):
        slot = self.dsem[self.di % len(self.dsem)]
        self.di += 1
        sem, cnt = slot
        if cnt > 0:
            self._wait(q, sem, 16 * cnt)
        self._deps(q, r, w)
        ins = self.E[q]["obj"].dma_start(out=out, in_=in_)
        ins.then_inc(sem, 16)
        slot[1] = cnt + 1
        ev = (sem, 16 * (cnt + 1), "dma")
        self._mark(ev, r, w)
        self.ninst += 1
        return ev

    def barrier(self):
        for a in self.E:
            for b in self.E:
                if a != b and self.E[b]["n"] > 0:
                    self._wait(a, self.E[b]["sem"], self.E[b]["n"])
            for sem, cnt in self.dsem:
                if cnt > 0:
                    self._wait(a, sem, 16 * cnt)


class Ring:
    def __init__(self, aps, name):
        self.aps = aps
        self.bufs = [Buf("%s%d" % (name, i)) for i in range(len(aps))]
        self.i = 0

    def next(self):
        j = self.i % len(self.aps)
        self.i += 1
        return self.aps[j], self.bufs[j]


class _Stop(Exception):
    pass


def build_nc(n_layers=DEPTH, dbg=False, stop=None):
    nc = bass.Bass("TRN2", target_bir_lowering=False)
    L = DEPTH
    LW = n_layers

    def din(name, shape):
        return nc.dram_tensor(name, list(shape), F32, kind="ExternalInput").ap()

    xT_d = din("xT", [D, S])
    memT_d = din("memT", [D, NMEM])
    vecs_d = din("vecs", [128, L * NV])
    cst_d = din("cst", [128, NCST])
    w_in_d = din("w_in", [LW, D, DINP])
    bd_d = din("lru_bd", [LW, 128, 1024])
    wpool_d = din("w_pool", [LW, 128, 512])
    w_branch_d = din("w_branch", [LW, 3, 512, D])
    w_out_d = din("w_out", [LW, D, D])
    w_mq_d = din("w_mem_q", [LW, D, 512])
    w_mkv_d = din("w_mem_kv", [LW, D, 1024])
    w_mo_d = din("w_mem_o", [LW, 512, D])
    w_fi_d = din("w_ffn_in", [LW, D, 2 * DFF])
    w_fo_d = din("w_ffn_out", [LW, DFF, D])
    yT_d = nc.dram_tensor("yT", [D, S], F32, kind="ExternalOutput").ap()
    taps = {}

    k = KB(nc)

    def sb(name, shape, dt):
        return nc.alloc_sbuf_tensor(name, list(shape), dt)

    xT = sb("xTs", [128, 8, S], F32)
    xb = [[Buf("x%d_%d" % (c, t)) for t in range(S // TM)] for c in range(8)]

    def xbufs(c, t0, T):
        return [xb[c][t] for t in range(t0 // TM, (t0 + T) // TM)]

    KK = sb("KK", [128, S], BF16)
    KKb = [Buf("KK%d" % i) for i in range(S // TM)]
    V65 = sb("V65", [128, 32, 66], BF16)
    V65b = [Buf("V%d" % i) for i in range(32)]
    vec = sb("vec", [128, L * NV], F32)
    vecb = Buf("vec")
    cst = sb("cstf", [128, NCST], F32)
    cstb = Buf("cst")
    ident = sb("ident", [128, 128], BF16)
    identrep = sb("identrep", [128, 8, 128], BF16)
    ones = sb("ones", [128, 128], BF16)
    constb = Buf("consts")
    nsp8 = sb("nsp8", [128, L * 4], F32)
    nsp8b = Buf("nsp8")
    bd = sb("bd", [128, 1024], BF16)
    bdb = Buf("bd")
    wpool = sb("wpool", [128, 512], BF16)
    wpoolb = Buf("wpool")
    lruhalo = sb("lruhalo", [128, 4, 4], F32)
    poolhalo = sb("poolhalo", [128, 4, 16], F32)
    carry = sb("carry", [128, 4], F32)
    halob = [Buf("halo%d" % g) for g in range(4)]
    phalob = [Buf("phalo%d" % g) for g in range(4)]
    carryb = [Buf("carry%d" % g) for g in range(4)]
    small = sb("small", [128, 64], F32)
    NRING = 4
    ringt = sb("ring", [128, NRING, 8, 128], BF16)
    ring = Ring([ringt[:, i] for i in range(NRING)], "ring")
    junk = sb("junk", [128, S], mybir.dt.uint8)
    junkb = Buf("junk")
    wvw = sb("wvw", [128, 8, 72], BF16)
    wvwb = Buf("wvw")
    ARENA = 45056
    arena = sb("arena", [128, ARENA // 4], F32)

    def carve(off, shape, dt):
        n = int(np.prod(shape))
        sz = 4 if dt == F32 else 2
        assert off % 4 == 0 and off + n * sz <= ARENA, (off, shape)
        a = arena[:, off // 4:(off + n * sz + 3) // 4]
        if dt != F32:
            a = a.bitcast(dt)
        a = a[:, 0:n]
        if len(shape) == 2:
            a = a.rearrange("p (a b) -> p a b", a=shape[0])
        elif len(shape) == 3:
            a = a.rearrange("p (a b c) -> p a b c", a=shape[0], b=shape[1])
        return a

    ps = [nc.alloc_psum_tensor("ps%d" % i, [128, 512], F32) for i in range(8)]
    psb = [Buf("ps%d" % i, excl=True) for i in range(8)]
    psrot = {"ids": list(range(8)), "i": 0}

    def next_ps():
        ids = psrot["ids"]
        j = ids[psrot["i"] % len(ids)]
        psrot["i"] += 1
        return ps[j], psb[j]

    SM_LO, SM_HI, SM_W0, SM_MID, SM_CNT, SM_TMP, SM_THR = 0, 1, 2, 3, 4, 5, 6
    SM_HW = 8
    SM_REC = 40
    SM_WIDX = 48
    smb = Buf("small")
    recb = Buf("rec")
    widxb = [Buf("widx0"), Buf("widx1")]

    k.dma("sp", vec[:, :], vecs_d[:, :], w=[vecb])
    k.dma("sp", cst[:, :], cst_d[:, :], w=[cstb])
    k.op("dve", lambda e: e.tensor_copy(out=ident[:, :], in_=cst[:, 0:128]), r=[cstb], w=[constb])
    for h in range(8):
        k.op("dve", lambda e: e.tensor_copy(out=identrep[:, h, :], in_=cst[:, 0:128]), r=[cstb], w=[constb])
    k.op("dve", lambda e: e.memset(ones[:, :], 1.0), w=[constb])
    k.op("dve", lambda e: e.memset(V65[:, :, :], 1.0), w=V65b)
    for c in range(8):
        k.dma("sp", xT[:, c, :], xT_d[c * 128:(c + 1) * 128, :], w=xb[c])
    for l in range(L):
        k.op("act", lambda e: e.activation(out=nsp8[:, l * 4:l * 4 + 4],
                                           in_=vec[:, l * NV + A_PARAM:l * NV + A_PARAM + 4],
                                           func=AF.Exp, scale=-1.0), r=[vecb], w=[nsp8b])
    k.op("act", lambda e: e.activation(out=nsp8[:, :], in_=nsp8[:, :], func=AF.Ln, bias=1.0),
         r=[nsp8b], w=[nsp8b])
    k.op("dve", lambda e: e.tensor_scalar(out=nsp8[:, :], in0=nsp8[:, :], scalar1=-8.0, scalar2=None,
                                          op0=ALU.mult), r=[nsp8b], w=[nsp8b])

    cmask = cst[:, 128:256]
    pow2 = cst[:, 256:256 + IT + 1]
    invcnt = cst[:, 288:304]

    def tap(name, ap, bufs):
        if not dbg:
            return
        t = nc.dram_tensor("tap_" + name, list(ap.shape), ap.dtype, kind="ExternalOutput").ap()
        taps[name] = t
        k.dma("sp", t, ap, r=bufs)

    def load_w(scr, blk, kc):
        ap, buf = ring.next()
        k.dma("sp", ap[:, 0:kc, :], scr[blk][:, 0:kc, :], w=[buf])
        return ap, buf

    def wview(w2d):
        return w2d.rearrange("(kc p) n -> p kc n", p=128)

    def mm(out, lhsT, rhs, start, stop, r, w, skip=False):
        if skip:
            k.op("pe", lambda e: e.matmul(out, lhsT, rhs, start=start, stop=stop, skip_group_check=True),
                 r=r, w=w)
        else:
            k.op("pe", lambda e: e.matmul(out, lhsT, rhs, start=start, stop=stop), r=r, w=w)

    def rms_rstd(srcs, T, sqring, rstd, rstdb):
        pa, pb = next_ps()
        n = len(srcs)
        for c, (ap, bufs) in enumerate(srcs):
            sq, sqb = sqring.next()
            k.op("act", lambda e: e.activation(out=sq[:, 0:T], in_=ap, func=AF.Square), r=bufs, w=[sqb])
            mm(pa[:, 0:T], ones[:, :], sq[:, 0:T], c == 0, c == n - 1, [sqb, constb], [pb])
        k.op("act", lambda e: e.activation(out=rstd, in_=pa[:, 0:T], func=AF.Sqrt, scale=1.0 / D,
                                           bias=eps_ap), r=[pb, constb], w=[rstdb])
        k.op("dve", lambda e: e.reciprocal(out=rstd, in_=rstd), r=[rstdb], w=[rstdb])

    eps_t = sb("eps", [128, 1], F32)
    eps_ap = eps_t[:, 0:1]
    k.op("dve", lambda e: e.memset(eps_t[:, :], RMS_EPS), w=[constb])

    def pre_norm(l, gbase, t0, T, hT, hTb, sqring, rstd, rstdb):
        srcs = [(xT[:, c, t0:t0 + T], xbufs(c, t0, T)) for c in range(8)]
        rms_rstd(srcs, T, sqring, rstd, rstdb)
        for c in range(8):
            k.op("dve", lambda e: e.scalar_tensor_tensor(
                out=hT[:, c, :], in0=xT[:, c, t0:t0 + T],
                scalar=vec[:, l * NV + gbase + c:l * NV + gbase + c + 1], in1=rstd,
                op0=ALU.mult, op1=ALU.mult), r=xbufs(c, t0, T) + [rstdb, vecb], w=[hTb[c]])

    def post_norm_res(l, gbase, t0, T, ytmp, ytb, sqring, rstd, rstdb):
        srcs = [(ytmp[:, c, :], [ytb[c]]) for c in range(8)]
        rms_rstd(srcs, T, sqring, rstd, rstdb)
        for c in range(8):
            k.op("dve", lambda e: e.scalar_tensor_tensor(
                out=ytmp[:, c, :], in0=ytmp[:, c, :],
                scalar=vec[:, l * NV + gbase + c:l * NV + gbase + c + 1], in1=rstd,
                op0=ALU.mult, op1=ALU.mult), r=[ytb[c], rstdb, vecb], w=[ytb[c]])
            k.op("dve", lambda e: e.tensor_tensor(out=xT[:, c, t0:t0 + T], in0=xT[:, c, t0:t0 + T],
                                                  in1=ytmp[:, c, :], op=ALU.add),
                 r=[ytb[c]] + xbufs(c, t0, T), w=xbufs(c, t0, T))

    def scratch(name, nblk):
        return nc.dram_tensor("scr_" + name, [LW, nblk, 128, 8, 128], BF16, kind="Internal").ap()

    SCR = dict(w_in=scratch("w_in", 46), w_branch=scratch("w_branch", 24), w_out=scratch("w_out", 8),
               w_mq=scratch("w_mq", 4), w_mkv=scratch("w_mkv", 8), w_mo=scratch("w_mo", 8),
               w_fi=scratch("w_fi", 44), w_fo0=scratch("w_fo0", 8), w_fo1=scratch("w_fo1", 8),
               w_fo2=scratch("w_fo2", 8))
    st32 = Ring([arena[:, i * 2048:(i + 1) * 2048].rearrange("p (k c) -> p k c", k=8) for i in range(2)], "st32")
    st16 = Ring([arena[:, 4096 + i * 1024:4096 + (i + 1) * 1024].bitcast(BF16).rearrange("p (k c) -> p k c", k=8)
                 for i in range(2)], "st16")
    cast_rr = {"i": 0}

    def convert(view, scr_l, blk0, kc, ncols):
        for c0 in range(0, ncols, 256):
            nco = min(256, ncols - c0)
            a32, b32 = st32.next()
            k.dma("sp", a32[:, 0:kc, 0:nco], view[:, :, c0:c0 + nco], w=[b32])
            a16, b16 = st16.next()
            eng = ("act", "dve", "pool")[cast_rr["i"] % 3]
            cast_rr["i"] += 1
            if eng == "act":
                k.op("act", lambda e: e.copy(out=a16[:, 0:kc, 0:nco], in_=a32[:, 0:kc, 0:nco]), r=[b32], w=[b16])
            else:
                k.op(eng, lambda e: e.tensor_copy(out=a16[:, 0:kc, 0:nco], in_=a32[:, 0:kc, 0:nco]), r=[b32], w=[b16])
            for b in range(nco // 128):
                k.dma("sp", scr_l[blk0 + c0 // 128 + b][:, 0:kc, :], a16[:, 0:kc, b * 128:(b + 1) * 128], r=[b16])

    for l in range(n_layers):
        convert(wview(w_in_d[l]), SCR["w_in"][l], 0, 8, DINP)
        for n in range(3):
            convert(wview(w_branch_d[l, n]), SCR["w_branch"][l], n * 8, 4, D)
        convert(wview(w_out_d[l]), SCR["w_out"][l], 0, 8, D)
        convert(wview(w_mq_d[l]), SCR["w_mq"][l], 0, 8, 512)
        convert(wview(w_mkv_d[l]), SCR["w_mkv"][l], 0, 8, 1024)
        convert(wview(w_mo_d[l]), SCR["w_mo"][l], 0, 4, D)
        convert(wview(w_fi_d[l]), SCR["w_fi"][l], 0, 8, 2 * DFF)
        wfo_v = wview(w_fo_d[l])
        for part, (k0, kn) in enumerate(((0, 8), (8, 8), (16, 6))):
            convert(wfo_v[:, k0:k0 + kn, :], SCR["w_fo%d" % part][l], 0, kn, D)
    k.barrier()

    def fence(new, old):
        ev = {}
        for b in old:
            for e in ([b.w] if b.w else []) + list(b.r.values()):
                if e[0].num not in ev or ev[e[0].num][1] < e[1]:
                    ev[e[0].num] = e
        for b in new:
            for key, e in ev.items():
                if key not in b.r or b.r[key][1] < e[1]:
                    b.r[key] = e

    chk_cnt = {}

    def chk(tag):
        chk_cnt[tag] = chk_cnt.get(tag, 0) + 1
        if stop is None:
            return
        st, _, n = stop.partition("@")
        if st == tag and chk_cnt[tag] == int(n or 1):
            raise _Stop()


    def _layers(n_layers):
        for l in range(n_layers):
            vb = l * NV
            k.barrier()
            stg, stgb = arena[:, 0:1536], Buf("stg")
            k.dma("sp", stg[:, 0:1024], bd_d[l], w=[stgb])
            k.dma("sp", stg[:, 1024:1536], wpool_d[l], w=[stgb])
            k.barrier()
            k.op("dve", lambda e: e.tensor_copy(out=bd[:, :], in_=stg[:, 0:1024]), r=[stgb], w=[bdb])
            k.op("dve", lambda e: e.tensor_copy(out=wpool[:, :], in_=stg[:, 1024:1536]), r=[stgb], w=[wpoolb])
            k.barrier()
            for g in range(4):
                k.op("dve", lambda e: e.memset(lruhalo[:, g, :], 0.0), w=[halob[g]])
                k.op("dve", lambda e: e.memset(poolhalo[:, g, :], 0.0), w=[phalob[g]])
                k.op("dve", lambda e: e.memset(carry[:, g:g + 1], 0.0), w=[carryb[g]])
            S_in = SCR["w_in"][l]
            chk("setup")
            T = TM

            score = arena[:, 0:S]
            mbuf = arena[:, S:S + S // 2].bitcast(BF16)
            scoreb, mbufb = Buf("score"), Buf("mbuf")
            o = 24576
            qq = carve(o, [8, T], BF16); o += 8 * T * 2
            qqb = [Buf("qq%d" % h) for h in range(8)]
            yab = [carve(o + n * 4 * T * 2, [4, T], BF16) for n in range(3)]; o += 3 * 4 * T * 2
            ybuf = [[Buf("y%d_%d" % (n, g)) for g in range(4)] for n in range(3)]
            o_free = o
            hT = carve(o_free, [8, T], BF16)
            hTb = [Buf("hT%d" % c) for c in range(8)]
            rstd = arena[:, (o_free + 4096) // 4:(o_free + 4096) // 4 + T]
            rstdb = Buf("rstd")
            sq_aps = [arena[:, (o_free + 5120 + i * 512) // 4:(o_free + 5120 + (i + 1) * 512) // 4].bitcast(BF16)
                      for i in range(2)]
            sqring = Ring(sq_aps, "sq")
            GS = 8256

            def gset(s):
                b = s * GS
                d = {}
                d["lrux"] = arena[:, b // 4:b // 4 + T + 4]; b += 1040
                d["graw"] = arena[:, b // 4:b // 4 + T]; b += 1024
                d["t1"] = arena[:, b // 4:b // 4 + T]; b += 1024
                d["ua"] = arena[:, b // 4:b // 4 + T]; b += 1024
                d["ra"] = arena[:, b // 4:b // 4 + T]; b += 1024
                d["ib"] = arena[:, b // 4:b // 4 + T]; b += 1024
                d["mb"] = arena[:, b // 4:b // 4 + T]; b += 1024
                d["uabf"] = arena[:, b // 4:b // 4 + T // 2].bitcast(BF16); b += 512
                d["ge"] = arena[:, b // 4:b // 4 + T // 2].bitcast(BF16); b += 512
                d["bufs"] = {n: Buf(n + str(s)) for n in ("lrux", "graw", "t1", "ua", "ra", "ib", "mb", "uabf", "ge")}
                return d

            def pset(s):
                b = 16576 + s * 3840
                d = {}
                d["px"] = arena[:, b // 4:b // 4 + T + 16]; b += 1088
                d["pa"] = arena[:, b // 4:b // 4 + T + 16]; b += 1088
                d["pb"] = arena[:, b // 4:b // 4 + T + 16]; b += 1088
                d["pooled"] = arena[:, b // 4:b // 4 + T // 2].bitcast(BF16); b += 512
                d["bufs"] = {n: Buf(n + str(s)) for n in ("px", "pa", "pb", "pooled")}
                return d
            gsets = [gset(0), gset(1)]
            psets = [pset(0), pset(1)]
            o2 = o_free
            relu_aps = []
            for i in range(3):
                relu_aps.append(arena[:, o2 // 4:o2 // 4 + 256].bitcast(BF16)); o2 += 1024
            reluring = Ring(relu_aps, "relu")
            pt_aps = []
            for i in range(4):
                pt_aps.append(arena[:, o2 // 4:o2 // 4 + 256].bitcast(BF16)); o2 += 1024
            ptring = Ring(pt_aps, "pt")
            diag = arena[:, o2 // 4:o2 // 4 + 512].bitcast(BF16).rearrange("p (h q) -> p h q", h=8); o2 += 2048
            diagb = Buf("diag")
            yctm = arena[:, o2 // 4:o2 // 4 + 256].bitcast(BF16); o2 += 1024
            yctmb = Buf("yctm")
            assert o2 <= ARENA
            merged = carve(0, [8, T], BF16)
            mergedb = [Buf("mg%d" % c) for c in range(8)]
            ytmpM = carve(4096, [8, T], F32)
            ytbM = [Buf("yt%d" % c) for c in range(8)]
            acc = arena[:, 12288 // 4:12288 // 4 + T]
            accb = Buf("acc")
            gt_aps = [arena[:, (13312 + i * 1024) // 4:(13312 + i * 1024) // 4 + T] for i in range(2)]
            gtring = Ring(gt_aps, "gt")
            tm_aps = [arena[:, (15360 + i * 1024) // 4:(15360 + i * 1024) // 4 + T] for i in range(2)]
            tmring = Ring(tm_aps, "tm")

            G_A = [b for gs_ in gsets for b in gs_["bufs"].values()] + [b for ps_ in psets for b in ps_["bufs"].values()]
            G_D = [scoreb, mbufb]
            G_M = mergedb + ytbM + [accb] + gtring.bufs + tmring.bufs
            H_1 = hTb + [rstdb] + sqring.bufs
            H_D = reluring.bufs + ptring.bufs + [diagb, yctmb]
            for tt in range(S // T):
                t0 = tt * T
                if tt == 0:
                    k.barrier()
                else:
                    fence(G_A, G_M)
                psrot["ids"] = list(range(8))
                pre_norm(l, G_MIX_PRE, t0, T, hT, hTb, sqring, rstd, rstdb)

                chk("m1")
                def proj(blk, evac):
                    wap, wb = load_w(S_in, blk, 8)
                    pa, pb = next_ps()
                    for kc in range(8):
                        mm(pa[:, 0:T], wap[:, kc, :], hT[:, kc, :], kc == 0, kc == 7, [wb, hTb[kc]], [pb])
                    evac(pa, pb)

                for g in range(4):
                    gs = gsets[g % 2]
                    gb = gs["bufs"]
                    lrux = gs["lrux"]
                    k.op("act", lambda e: e.copy(out=lrux[:, 0:3], in_=lruhalo[:, g, 0:3]), r=[halob[g]], w=[gb["lrux"]])
                    proj(g, lambda pa, pb: k.op("act", lambda e: e.copy(out=lrux[:, 3:3 + T], in_=pa[:, 0:T]),
                                                r=[pb], w=[gb["lrux"]]))
                    k.op("act", lambda e: e.copy(out=lruhalo[:, g, 0:3], in_=lrux[:, T:T + 3]), r=[gb["lrux"]], w=[halob[g]])
                    proj(4 + g, lambda pa, pb: k.op("act", lambda e: e.copy(out=gs["graw"], in_=pa[:, 0:T]),
                                                    r=[pb], w=[gb["graw"]]))
                    graw, t1 = gs["graw"], gs["t1"]
                    k.op("dve", lambda e: e.tensor_tensor(out=t1, in0=graw, in1=graw, op=ALU.mult), r=[gb["graw"]], w=[gb["t1"]])
                    k.op("dve", lambda e: e.tensor_scalar(out=t1, in0=t1, scalar1=0.044715, scalar2=1.0, op0=ALU.mult, op1=ALU.add),
                         r=[gb["t1"]], w=[gb["t1"]])
                    k.op("dve", lambda e: e.tensor_tensor(out=t1, in0=t1, in1=graw, op=ALU.mult), r=[gb["t1"], gb["graw"]], w=[gb["t1"]])
                    k.op("act", lambda e: e.activation(out=t1, in_=t1, func=AF.Sigmoid, scale=1.5957691216057308),
                         r=[gb["t1"]], w=[gb["t1"]])
                    k.op("dve", lambda e: e.tensor_tensor(out=gs["ge"], in0=t1, in1=graw, op=ALU.mult),
                         r=[gb["t1"], gb["graw"]], w=[gb["ge"]])
                    ua = gs["ua"]
                    cw = lambda kk: vec[:, vb + CONV_W + kk * 4 + g:vb + CONV_W + kk * 4 + g + 1]
                    k.op("act", lambda e: e.activation(out=ua, in_=lrux[:, 0:T], func=AF.Identity, scale=cw(0),
                                                       bias=vec[:, vb + CONV_B + g:vb + CONV_B + g + 1]),
                         r=[gb["lrux"], vecb], w=[gb["ua"]])
                    for kk in (1, 2, 3):
                        k.op("dve", lambda e: e.scalar_tensor_tensor(out=ua, in0=lrux[:, kk:kk + T], scalar=cw(kk), in1=ua,
                                                                     op0=ALU.mult, op1=ALU.add),
                             r=[gb["lrux"], gb["ua"], vecb], w=[gb["ua"]])
                    k.op("act", lambda e: e.copy(out=gs["uabf"], in_=ua), r=[gb["ua"]], w=[gb["uabf"]])
                    for which, dst, bcol in ((0, "ra", B_A), (1, "ib", B_X)):
                        pa, pb = next_ps()
                        mm(pa[:, 0:T], bd[:, (g * 2 + which) * 128:(g * 2 + which + 1) * 128], gs["uabf"], True, True,
                           [bdb, gb["uabf"]], [pb])
                        k.op("act", lambda e: e.activation(out=gs[dst], in_=pa[:, 0:T], func=AF.Sigmoid,
                                                           bias=vec[:, vb + bcol + g:vb + bcol + g + 1]),
                             r=[pb, vecb], w=[gb[dst]])
                    ra, ib, mb = gs["ra"], gs["ib"], gs["mb"]
                    k.op("act", lambda e: e.activation(out=ra, in_=ra, func=AF.Exp, scale=nsp8[:, l * 4 + g:l * 4 + g + 1]),
                         r=[gb["ra"], nsp8b], w=[gb["ra"]])
                    k.op("dve", lambda e: e.tensor_tensor(out=mb, in0=ra, in1=ra, op=ALU.mult), r=[gb["ra"]], w=[gb["mb"]])
                    k.op("dve", lambda e: e.tensor_scalar(out=mb, in0=mb, scalar1=-1.0, scalar2=1.0, op0=ALU.mult, op1=ALU.add),
                         r=[gb["mb"]], w=[gb["mb"]])
                    k.op("dve", lambda e: e.tensor_scalar(out=mb, in0=mb, scalar1=0.0, scalar2=None, op0=ALU.max),
                         r=[gb["mb"]], w=[gb["mb"]])
                    k.op("act", lambda e: e.activation(out=mb, in_=mb, func=AF.Sqrt), r=[gb["mb"]], w=[gb["mb"]])
                    k.op("dve", lambda e: e.tensor_tensor(out=ib, in0=ib, in1=ua, op=ALU.mult), r=[gb["ib"], gb["ua"]], w=[gb["ib"]])
                    k.op("dve", lambda e: e.tensor_tensor(out=ib, in0=ib, in1=mb, op=ALU.mult), r=[gb["ib"], gb["mb"]], w=[gb["ib"]])
                    k.op("dve", lambda e: e.tensor_tensor_scan(out=mb, data0=ra, data1=ib, initial=carry[:, g:g + 1],
                                                               op0=ALU.mult, op1=ALU.add),
                         r=[gb["ra"], gb["ib"], carryb[g]], w=[gb["mb"]])
                    k.op("dve", lambda e: e.tensor_copy(out=carry[:, g:g + 1], in_=mb[:, T - 1:T]), r=[gb["mb"]], w=[carryb[g]])
                    k.op("dve", lambda e: e.tensor_tensor(out=yab[0][:, g, :], in0=mb, in1=gs["ge"], op=ALU.mult),
                         r=[gb["mb"], gb["ge"]], w=[ybuf[0][g]])

                chk("A")
                for g in range(4):
                    pset_ = psets[g % 2]
                    pbf = pset_["bufs"]
                    px, pA, pB, pooled = pset_["px"], pset_["pa"], pset_["pb"], pset_["pooled"]
                    k.op("act", lambda e: e.copy(out=px[:, 0:15], in_=poolhalo[:, g, 0:15]), r=[phalob[g]], w=[pbf["px"]])
                    proj(8 + g, lambda pa, pb: k.op("act", lambda e: e.copy(out=px[:, 15:15 + T], in_=pa[:, 0:T]),
                                                    r=[pb], w=[pbf["px"]]))
                    k.op("act", lambda e: e.copy(out=poolhalo[:, g, 0:15], in_=px[:, T:T + 15]), r=[pbf["px"]], w=[phalob[g]])
                    Wd = T + 15
                    cur, curb = px, pbf["px"]
                    seq = [(pA, pbf["pa"]), (pB, pbf["pb"]), (pA, pbf["pa"]), (pB, pbf["pb"])]
                    sh = 1
                    for step in range(g + 1):
                        dst, dstb = seq[step]
                        k.op("dve", lambda e: e.tensor_tensor(out=dst[:, sh:Wd], in0=cur[:, sh:Wd], in1=cur[:, 0:Wd - sh], op=ALU.add),
                             r=[curb], w=[dstb])
                        cur, curb = dst, dstb
                        sh *= 2
                    wdw = 2 ** (g + 1)
                    k.op("dve", lambda e: e.scalar_tensor_tensor(out=pooled, in0=cur[:, 15:15 + T], scalar=1.0 / wdw,
                                                                 in1=px[:, 15:15 + T], op0=ALU.mult, op1=ALU.subtract),
                         r=[curb, pbf["px"]], w=[pbf["pooled"]])
                    if tt == 0:
                        nfix = wdw - 1
                        tmpf = pB[:, 0:nfix] if cur is not pB else pA[:, 0:nfix]
                        tmpb = pbf["pb"] if cur is not pB else pbf["pa"]
                        k.op("dve", lambda e: e.tensor_tensor(out=tmpf, in0=cur[:, 15:15 + nfix], in1=invcnt[:, 0:nfix], op=ALU.mult),
                             r=[curb, cstb], w=[tmpb])
                        k.op("dve", lambda e: e.tensor_tensor(out=pooled[:, 0:nfix], in0=tmpf, in1=px[:, 15:15 + nfix], op=ALU.subtract),
                             r=[tmpb, pbf["px"]], w=[pbf["pooled"]])
                    pa_, pb_ = next_ps()
                    mm(pa_[:, 0:T], wpool[:, g * 128:(g + 1) * 128], pooled, True, True, [wpoolb, pbf["pooled"]], [pb_])
                    k.op("act", lambda e: e.activation(out=yab[1][:, g, :], in_=pa_[:, 0:T], func=AF.Copy,
                                                       scale=vec[:, vb + POOL_SCALE + g:vb + POOL_SCALE + g + 1]),
                         r=[pb_, vecb], w=[ybuf[1][g]])

                chk("B")
                for h in range(8):
                    proj(12 + h, lambda pa, pb: k.op("act", lambda e: e.copy(out=qq[:, h, :], in_=pa[:, 0:T]),
                                                     r=[pb], w=[qqb[h]]))
                chk("q")
                proj(20, lambda pa, pb: k.op("act", lambda e: e.copy(out=KK[:, t0:t0 + T], in_=pa[:, 0:T]),
                                             r=[pb], w=[KKb[tt]]))
                chk("kk")
                k.dma("sp", wvw[:, :, :], S_in[21][:, :, 0:72], w=[wvwb])
                chk("wvw")
                for i in range(T // 128):
                    Bg = t0 // 128 + i
                    pa, pb = next_ps()
                    for kc in range(8):
                        mm(pa[:, 0:72], hT[:, kc, i * 128:(i + 1) * 128], wvw[:, kc, :], kc == 0, kc == 7,
                           [wvwb, hTb[kc]], [pb])
                    k.op("act", lambda e: e.copy(out=V65[:, Bg, 0:64], in_=pa[:, 0:64]), r=[pb], w=[V65b[Bg]])
                    k.op("act", lambda e: e.activation(out=small[:, SM_WIDX + i * 8:SM_WIDX + i * 8 + 8], in_=pa[:, 64:72],
                                                       func=AF.Copy, scale=float(8 ** -0.5 * 64 ** -0.5)),
                         r=[pb], w=[widxb[i]])

                chk("qkv")
                fence(G_D, G_A)
                fence(H_D, H_1)
                psO = [ps[6], ps[7]]
                psOb = [psb[6], psb[7]]
                nqb = T // 128

                def d_index(i):
                    Bq = t0 // 128 + i
                    N = (Bq + 1) * 128
                    qc = slice(i * 128, (i + 1) * 128)
                    for h in range(8):
                        k.op("dve", lambda e: e.tensor_scalar(out=diag[:, h, :], in0=ident[:, :],
                                                              scalar1=small[:, SM_WIDX + i * 8 + h:SM_WIDX + i * 8 + h + 1],
                                                              scalar2=None, op0=ALU.mult),
                             r=[constb, widxb[i]], w=[diagb])
                    psrot["ids"] = [0, 1, 2, 3]
                    for ci, c0 in enumerate(range(0, N, 512)):
                        Wc = min(512, N - c0)
                        kb_ = [KKb[j] for j in range(c0 // TM, (c0 + Wc + TM - 1) // TM)]
                        sca, scb = ps[4 + ci % 2], psb[4 + ci % 2]
                        pend = None
                        for h in range(8):
                            la, lb = next_ps()
                            mm(la[:, 0:Wc], qq[64:128, h, qc], KK[64:128, c0:c0 + Wc], True, True, [qqb[h]] + kb_, [lb])
                            rl, rlb = reluring.next()
                            k.op("act", lambda e: e.activation(out=rl[:, 0:Wc], in_=la[:, 0:Wc], func=AF.Relu), r=[lb], w=[rlb])
                            if pend is not None:
                                ph, prl, prlb = pend
                                mm(sca[:, 0:Wc], diag[:, ph, :], prl[:, 0:Wc], ph == 0, False, [diagb, prlb], [scb])
                            pend = (h, rl, rlb)
                        ph, prl, prlb = pend
                        mm(sca[:, 0:Wc], diag[:, ph, :], prl[:, 0:Wc], False, True, [diagb, prlb], [scb])
                        k.op("dve", lambda e: e.tensor_copy(out=score[:, c0:c0 + Wc], in_=sca[:, 0:Wc]), r=[scb], w=[scoreb])
                    k.op("dve", lambda e: e.tensor_tensor(out=score[:, N - 128:N], in0=score[:, N - 128:N], in1=cmask, op=ALU.add),
                         r=[scoreb, cstb], w=[scoreb])

                def d_thr(i):
                    Bq = t0 // 128 + i
                    N = (Bq + 1) * 128
                    thr = small[:, SM_LO:SM_LO + 1]
                    if Bq < 2:
                        k.op("dve", lambda e: e.memset(thr, -1e29), w=[smb])
                        return
                    lo, hi, w0 = thr, small[:, SM_HI:SM_HI + 1], small[:, SM_W0:SM_W0 + 1]
                    mid, cnt, tmp = small[:, SM_MID:SM_MID + 1], small[:, SM_CNT:SM_CNT + 1], small[:, SM_TMP:SM_TMP + 1]
                    hw = small[:, SM_HW:SM_HW + IT + 1]
                    k.op("dve", lambda e: e.tensor_reduce(out=lo, in_=score[:, 0:N - 64], axis=AX.X, op=ALU.min), r=[scoreb], w=[smb])
                    k.op("dve", lambda e: e.tensor_reduce(out=hi, in_=score[:, 0:N], axis=AX.X, op=ALU.max), r=[scoreb, smb], w=[smb])
                    k.op("dve", lambda e: e.tensor_tensor(out=w0, in0=hi, in1=lo, op=ALU.subtract), r=[smb], w=[smb])
                    k.op("dve", lambda e: e.tensor_scalar(out=hw, in0=pow2, scalar1=w0, scalar2=None, op0=ALU.mult), r=[smb, cstb], w=[smb])
                    k.op("dve", lambda e: e.tensor_tensor(out=mid, in0=lo, in1=hw[:, 0:1], op=ALU.add), r=[smb], w=[smb])
                    for it in range(IT):
                        k.op("dve", lambda e: e.tensor_scalar(out=junk[:, 0:N], in0=score[:, 0:N], scalar1=mid, scalar2=None,
                                                              op0=ALU.is_ge, op1=ALU.add, accum_out=cnt),
                             r=[scoreb, smb, junkb], w=[junkb, smb])
                        k.op("dve", lambda e: e.tensor_scalar(out=tmp, in0=cnt, scalar1=255.5, scalar2=hw[:, it:it + 1],
                                                              op0=ALU.is_ge, op1=ALU.mult), r=[smb], w=[smb])
                        k.op("dve", lambda e: e.scalar_tensor_tensor(out=mid, in0=tmp, scalar=hw[:, it + 1:it + 2], in1=mid,
                                                                     op0=ALU.subtract, op1=ALU.add), r=[smb], w=[smb])
                    k.op("dve", lambda e: e.tensor_tensor(out=lo, in0=mid, in1=hw[:, IT:IT + 1], op=ALU.subtract), r=[smb], w=[smb])

                def d_mb(i):
                    Bq = t0 // 128 + i
                    N = (Bq + 1) * 128
                    thr = small[:, SM_LO:SM_LO + 1]
                    k.op("dve", lambda e: e.tensor_scalar(out=mbuf[:, 0:N], in0=score[:, 0:N], scalar1=thr, scalar2=-30000.0,
                                                          op0=ALU.is_lt, op1=ALU.mult), r=[scoreb, smb, mbufb], w=[mbufb])

                def d_att(i):
                    Bq = t0 // 128 + i
                    qc = slice(i * 128, (i + 1) * 128)
                    psrot["ids"] = [0, 1, 2, 3, 4, 5]

                    def st_pair(j):
                        out = []
                        kbj = [KKb[(j * 128) // TM]]
                        for half in range(2):
                            sa, sbf = next_ps()
                            mm(sa[:, 0:512], KK[0:64, j * 128:(j + 1) * 128], qq[0:64, 4 * half:4 * half + 4, qc], True, False,
                               kbj + qqb[4 * half:4 * half + 4], [sbf])
                            mm(sa[:, 0:512], mbuf[:, j * 128:(j + 1) * 128], identrep[:, 0:4, :], False, True,
                               [mbufb, constb], [sbf])
                            pt, ptb = ptring.next()
                            k.op("act", lambda e: e.activation(out=pt[:, 0:512], in_=sa[:, 0:512], func=AF.Exp, scale=0.125),
                                 r=[sbf], w=[ptb])
                            out.append((pt, ptb))
                        return out

                    def pv_pair(j, pts):
                        for half in range(2):
                            pt, ptb = pts[half]
                            for hh in range(4):
                                mm(psO[half][:, hh * 65:(hh + 1) * 65], pt[:, hh * 128:(hh + 1) * 128], V65[:, j, 0:65],
                                   (j == 0 and hh == 0), (j == Bq and hh == 3), [ptb, V65b[j]], [psOb[half]], skip=True)
                    prev = None
                    for j in range(Bq + 1):
                        cur = st_pair(j)
                        if prev is not None:
                            pv_pair(j - 1, prev)
                        prev = cur
                    pv_pair(Bq, prev)
                    for half in range(2):
                        o3 = psO[half][:, 0:260].rearrange("p (h e) -> p h e", e=65)
                        rec = small[:, SM_REC + 4 * half:SM_REC + 4 * half + 4]
                        k.op("dve", lambda e: e.reciprocal(out=rec, in_=o3[:, :, 64]), r=[psOb[half]], w=[recb])
                        yv = yctm[:, half * 256:(half + 1) * 256].rearrange("p (h d) -> p h d", h=4)
                        k.op("dve", lambda e: e.tensor_tensor(out=yv, in0=o3[:, :, 0:64],
                                                              in1=rec.unsqueeze(2).to_broadcast([128, 4, 64]), op=ALU.mult),
                             r=[psOb[half], recb], w=[yctmb])
                    ta, tb = next_ps()
                    tab = ta[:, :].bitcast(BF16)
                    for j4 in range(4):
                        k.op("pe", lambda e: e.transpose(tab[:, j4 * 128:(j4 + 1) * 128], yctm[:, j4 * 128:(j4 + 1) * 128], ident[:, :]),
                             r=[yctmb, constb], w=[tb])
                    k.op("act", lambda e: e.copy(out=yab[2][:, :, qc], in_=tab[:, 0:512].rearrange("p (j q) -> p j q", j=4)),
                         r=[tb], w=ybuf[2])

                d_index(0)
                d_thr(0)
                d_mb(0)
                for i in range(1, nqb):
                    d_index(i)
                    d_thr(i)
                    d_att(i - 1)
                    d_mb(i)
                d_att(nqb - 1)
                chk("dsa")
                fence(G_M, G_D)
                fence(H_1, H_D)
                psrot["ids"] = list(range(8))
                pre_norm(l, G_MIX_PRE, t0, T, hT, hTb, sqring, rstd, rstdb)
                for c in range(8):
                    for n in range(3):
                        wa_, wb_ = load_w(SCR["w_branch"][l], n * 8 + c, 4)
                        ua_, ub_ = next_ps()
                        for kc in range(4):
                            mm(ua_[:, 0:T], wa_[:, kc, :], yab[n][:, kc, :], kc == 0, kc == 3, [wb_, ybuf[n][kc]], [ub_])
                        wg_, wgb_ = load_w(S_in, GATE_BLK0 + n * 8 + c, 8)
                        ga_, gb_ = next_ps()
                        for kc in range(8):
                            mm(ga_[:, 0:T], wg_[:, kc, :], hT[:, kc, :], kc == 0, kc == 7, [wgb_, hTb[kc]], [gb_])
                        gt, gtb = gtring.next()
                        k.op("act", lambda e: e.activation(out=gt, in_=ga_[:, 0:T], func=AF.Sigmoid,
                                                           bias=vec[:, vb + B_GATE + n * 8 + c:vb + B_GATE + n * 8 + c + 1]),
                             r=[gb_, vecb], w=[gtb])
                        if n == 0:
                            k.op("dve", lambda e: e.tensor_tensor(out=acc, in0=ua_[:, 0:T], in1=gt, op=ALU.mult), r=[ub_, gtb], w=[accb])
                        else:
                            tm_, tmb_ = tmring.next()
                            k.op("dve", lambda e: e.tensor_tensor(out=tm_, in0=ua_[:, 0:T], in1=gt, op=ALU.mult), r=[ub_, gtb], w=[tmb_])
                            if n == 1:
                                k.op("dve", lambda e: e.tensor_tensor(out=acc, in0=acc, in1=tm_, op=ALU.add), r=[accb, tmb_], w=[accb])
                            else:
                                k.op("dve", lambda e: e.tensor_tensor(out=merged[:, c, :], in0=acc, in1=tm_, op=ALU.add),
                                     r=[accb, tmb_], w=[mergedb[c]])
                chk("m6")
                for c in range(8):
                    wa_, wb_ = load_w(SCR["w_out"][l], c, 8)
                    pa, pb = next_ps()
                    for kc in range(8):
                        mm(pa[:, 0:T], wa_[:, kc, :], merged[:, kc, :], kc == 0, kc == 7, [wb_, mergedb[kc]], [pb])
                    k.op("act", lambda e: e.copy(out=ytmpM[:, c, :], in_=pa[:, 0:T]), r=[pb], w=[ytbM[c]])
                post_norm_res(l, G_MIX_POST, t0, T, ytmpM, ytbM, sqring, rstd, rstdb)
                if dbg and l == 0 and tt == 1:
                    tap("ya", yab[0], ybuf[0]); tap("yb", yab[1], ybuf[1]); tap("yc", yab[2], ybuf[2])
                    tap("merged", merged, mergedb)

            chk("m7")
            k.barrier()
            T = TX
            psrot["ids"] = list(range(8))
            o = 0
            hT = carve(o, [8, T], BF16); o += 8192
            hTb = [Buf("hTx%d" % c) for c in range(8)]
            ytmp = carve(o, [8, T], F32); o += 16384
            ytb = [Buf("ytx%d" % c) for c in range(8)]
            rstd = arena[:, o // 4:o // 4 + T]; o += 2048
            rstdb = Buf("rstdx")
            sqring = Ring([arena[:, (o + i * 1024) // 4:(o + (i + 1) * 1024) // 4].bitcast(BF16) for i in range(2)], "sqx"); o += 2048
            qmT = carve(o, [4, T], BF16); o += 4096
            qmb = [Buf("qm%d" % h) for h in range(4)]
            omT = carve(o, [4, T], BF16); o += 4096
            omb = [Buf("om%d" % h) for h in range(4)]
            ptx = Ring([arena[:, (o + i * 1024) // 4:(o + (i + 1) * 1024) // 4].bitcast(BF16) for i in range(4)], "ptx"); o += 4096
            rden, rdenb = rstd, rstdb
            memKT = carve(o, [4, NMEM], BF16); o += 2048
            memV = carve(o, [2, 512], BF16); o += 2048
            memKb, memVb = Buf("memK"), Buf("memV")
            assert o <= ARENA, o
            mT = arena[:, 8192 // 4:8192 // 4 + 8 * NMEM].rearrange("p (c m) -> p c m", c=8)
            mTb = [Buf("mT%d" % c) for c in range(8)]
            mh = carve(0, [8, NMEM], BF16)
            mhb = [Buf("mh%d" % c) for c in range(8)]
            wkv = arena[:, 16384 // 4:16384 // 4 + 2048].bitcast(BF16).rearrange("p (c n) -> p c n", c=8)
            wkvb = Buf("wkv")
            for c in range(8):
                k.dma("sp", mT[:, c, :], memT_d[c * 128:(c + 1) * 128, :], w=[mTb[c]])
            for b4 in range(4):
                k.dma("sp", wkv[:, :, b4 * 128:(b4 + 1) * 128], SCR["w_mkv"][l][4 + b4], w=[wkvb])
            k.barrier()
            rms_rstd([(mT[:, c, :], [mTb[c]]) for c in range(8)], NMEM, sqring, rstd[:, 0:NMEM], rstdb)
            for c in range(8):
                k.op("dve", lambda e: e.scalar_tensor_tensor(out=mh[:, c, :], in0=mT[:, c, :],
                                                             scalar=vec[:, vb + G_MEM_KV + c:vb + G_MEM_KV + c + 1],
                                                             in1=rstd[:, 0:NMEM], op0=ALU.mult, op1=ALU.mult),
                     r=[mTb[c], rstdb, vecb], w=[mhb[c]])
            for h in range(4):
                wa_, wb_ = load_w(SCR["w_mkv"][l], h, 8)
                pa, pb = next_ps()
                for kc in range(8):
                    mm(pa[:, 0:NMEM], wa_[:, kc, :], mh[:, kc, :], kc == 0, kc == 7, [wb_, mhb[kc]], [pb])
                k.op("act", lambda e: e.copy(out=memKT[:, h, :], in_=pa[:, 0:NMEM]), r=[pb], w=[memKb])
            for mbk in range(2):
                pa, pb = next_ps()
                for kc in range(8):
                    mm(pa[:, 0:512], mh[:, kc, mbk * 128:(mbk + 1) * 128], wkv[:, kc, :], kc == 0, kc == 7, [wkvb, mhb[kc]], [pb])
                k.op("act", lambda e: e.copy(out=memV[:, mbk, :], in_=pa[:, 0:512]), r=[pb], w=[memVb])
            k.barrier()
            for tt in range(S // T):
                t0 = tt * T
                pre_norm(l, G_MEM_PRE, t0, T, hT, hTb, sqring, rstd, rstdb)
                for h in range(4):
                    wa_, wb_ = load_w(SCR["w_mq"][l], h, 8)
                    pa, pb = next_ps()
                    for kc in range(8):
                        mm(pa[:, 0:T], wa_[:, kc, :], hT[:, kc, :], kc == 0, kc == 7, [wb_, hTb[kc]], [pb])
                    k.op("act", lambda e: e.copy(out=qmT[:, h, :], in_=pa[:, 0:T]), r=[pb], w=[qmb[h]])
                for h in range(4):
                    pts = []
                    for mbk in range(2):
                        sa, sbf = next_ps()
                        mm(sa[:, 0:T], memKT[:, h, mbk * 128:(mbk + 1) * 128], qmT[:, h, :], True, True, [memKb, qmb[h]], [sbf])
                        pt, ptb = ptx.next()
                        k.op("act", lambda e: e.activation(out=pt[:, 0:T], in_=sa[:, 0:T], func=AF.Exp, scale=float(128 ** -0.5)),
                             r=[sbf], w=[ptb])
                        pts.append((pt, ptb))
                    da, db = next_ps()
                    oa, ob = next_ps()
                    for mbk in range(2):
                        pt, ptb = pts[mbk]
                        mm(da[:, 0:T], ones[:, :], pt[:, 0:T], mbk == 0, mbk == 1, [constb, ptb], [db])
                    for mbk in range(2):
                        pt, ptb = pts[mbk]
                        mm(oa[:, 0:T], memV[:, mbk, h * 128:(h + 1) * 128], pt[:, 0:T], mbk == 0, mbk == 1, [memVb, ptb], [ob])
                    k.op("dve", lambda e: e.reciprocal(out=rden, in_=da[:, 0:T]), r=[db], w=[rdenb])
                    k.op("dve", lambda e: e.tensor_tensor(out=omT[:, h, :], in0=oa[:, 0:T], in1=rden, op=ALU.mult),
                         r=[ob, rdenb], w=[omb[h]])
                for c in range(8):
                    wa_, wb_ = load_w(SCR["w_mo"][l], c, 4)
                    pa, pb = next_ps()
                    for kc in range(4):
                        mm(pa[:, 0:T], wa_[:, kc, :], omT[:, kc, :], kc == 0, kc == 3, [wb_, omb[kc]], [pb])
                    k.op("act", lambda e: e.copy(out=ytmp[:, c, :], in_=pa[:, 0:T]), r=[pb], w=[ytb[c]])
                post_norm_res(l, G_MEM_POST, t0, T, ytmp, ytb, sqring, rstd, rstdb)

            chk("x")
            k.barrier()
            o = 0
            hid = carve(o, [22, T], BF16); o += 22528
            hidb = [Buf("hid%d" % j) for j in range(22)]
            hT = carve(o, [8, T], BF16)
            hTb = [Buf("hTf%d" % c) for c in range(8)]
            sg_aps = [arena[:, (o + 8192 + i * 2048) // 4:(o + 8192 + (i + 1) * 2048) // 4] for i in range(2)]
            sgring = Ring(sg_aps, "sg")
            ytmp = carve(o, [8, T], F32); o += 16384
            ytb = [Buf("ytf%d" % c) for c in range(8)]
            rstd = arena[:, o // 4:o // 4 + T]; o += 2048
            rstdb = Buf("rstdf")
            sqring = Ring([arena[:, (o + i * 1024) // 4:(o + (i + 1) * 1024) // 4].bitcast(BF16) for i in range(2)], "sqf"); o += 2048
            assert o <= ARENA, o
            for tt in range(S // T):
                t0 = tt * T
                k.barrier()
                pre_norm(l, G_FFN_PRE, t0, T, hT, hTb, sqring, rstd, rstdb)
                for j in range(22):
                    wg_, wgb_ = load_w(SCR["w_fi"][l], j, 8)
                    ga_, gb_ = next_ps()
                    for kc in range(8):
                        mm(ga_[:, 0:T], wg_[:, kc, :], hT[:, kc, :], kc == 0, kc == 7, [wgb_, hTb[kc]], [gb_])
                    wu_, wub_ = load_w(SCR["w_fi"][l], 22 + j, 8)
                    ua_, ub_ = next_ps()
                    for kc in range(8):
                        mm(ua_[:, 0:T], wu_[:, kc, :], hT[:, kc, :], kc == 0, kc == 7, [wub_, hTb[kc]], [ub_])
                    sg, sgb = sgring.next()
                    k.op("act", lambda e: e.activation(out=sg, in_=ga_[:, 0:T], func=AF.Silu), r=[gb_], w=[sgb])
                    k.op("dve", lambda e: e.tensor_tensor(out=hid[:, j, :], in0=ua_[:, 0:T], in1=sg, op=ALU.mult),
                         r=[ub_, sgb], w=[hidb[j]])
                k.barrier()
                for c in range(8):
                    pa, pb = next_ps()
                    for part, (k0, kn) in enumerate(((0, 8), (8, 8), (16, 6))):
                        ap_, buf_ = load_w(SCR["w_fo%d" % part][l], c, kn)
                        for kc in range(kn):
                            mm(pa[:, 0:T], ap_[:, kc, :], hid[:, k0 + kc, :], (k0 + kc) == 0, (k0 + kc) == 21,
                               [buf_, hidb[k0 + kc]], [pb])
                    k.op("act", lambda e: e.copy(out=ytmp[:, c, :], in_=pa[:, 0:T]), r=[pb], w=[ytb[c]])
                post_norm_res(l, G_FFN_POST, t0, T, ytmp, ytb, sqring, rstd, rstdb)

    try:
        _layers(n_layers)
    except _Stop:
        pass

    k.barrier()
    outs = []
    for c in range(8):
        outs.append(k.dma("sp", yT_d[c * 128:(c + 1) * 128, :], xT[:, c, :], r=xb[c]))
    for sem, val, _ in outs:
        k._wait("sp", sem, val)
    return nc, taps, k


def _cols(v, n):
    return np.ascontiguousarray(v.reshape(n, 128).T)


def prep_inputs(x, mem, g_mix_pre, w_in, conv_w, conv_b, lru_w_a, lru_b_a, lru_w_x, lru_b_x,
                lru_a_param, w_pool, pool_scale, w_branch, b_gate, w_out, g_mix_post,
                g_mem_pre, g_mem_kv, w_mem_q, w_mem_kv, w_mem_o, g_mem_post,
                g_ffn_pre, w_ffn_in, w_ffn_out, g_ffn_post):
    f = lambda a: np.asarray(a, dtype=np.float32)
    L = DEPTH
    vecs = np.zeros((128, L * NV), np.float32)
    for l in range(L):
        b = l * NV
        for base, arr in ((G_MIX_PRE, g_mix_pre), (G_MIX_POST, g_mix_post), (G_MEM_PRE, g_mem_pre),
                          (G_MEM_KV, g_mem_kv), (G_MEM_POST, g_mem_post), (G_FFN_PRE, g_ffn_pre),
                          (G_FFN_POST, g_ffn_post)):
            vecs[:, b + base:b + base + 8] = _cols(f(arr)[l], 8)
        for kk in range(4):
            vecs[:, b + CONV_W + kk * 4:b + CONV_W + kk * 4 + 4] = _cols(f(conv_w)[l, kk], 4)
        vecs[:, b + CONV_B:b + CONV_B + 4] = _cols(f(conv_b)[l], 4)
        vecs[:, b + B_A:b + B_A + 4] = _cols(f(lru_b_a)[l], 4)
        vecs[:, b + B_X:b + B_X + 4] = _cols(f(lru_b_x)[l], 4)
        vecs[:, b + A_PARAM:b + A_PARAM + 4] = _cols(f(lru_a_param)[l], 4)
        vecs[:, b + POOL_SCALE:b + POOL_SCALE + 4] = _cols(f(pool_scale)[l], 4)
        for n in range(3):
            vecs[:, b + B_GATE + n * 8:b + B_GATE + n * 8 + 8] = _cols(f(b_gate)[l, n], 8)
    cst = np.zeros((128, NCST), np.float32)
    cst[:, 0:128] = np.eye(128, dtype=np.float32)
    cst[0:64, 128 + 64:256] = -1e30
    cst[:, 256:256 + IT + 1] = (0.5 ** np.arange(1, IT + 2, dtype=np.float64)).astype(np.float32)[None, :]
    cst[:, 288:304] = (1.0 / np.arange(1, 17, dtype=np.float64)).astype(np.float32)[None, :]
    perm = list(range(0, 1536))
    for h in range(8):
        perm += list(range(1536 + 64 * h, 1536 + 64 * h + 64)) + list(range(2176 + 64 * h, 2176 + 64 * h + 64))
    perm += list(range(2048, 2112)) + list(range(2688, 2752)) + list(range(2112, 2176)) + list(range(2752, 2760))
    perm += list(range(2760, 5832))
    assert len(perm) == DIN
    w_in_p = np.zeros((L, D, DINP), np.float32)
    w_in_p[:, :, 0:2760] = f(w_in)[:, :, perm[:2760]]
    w_in_p[:, :, 2816:] = f(w_in)[:, :, 2760:]
    bd = np.zeros((L, 128, 1024), np.float32)
    for g in range(4):
        for which, wsrc in ((0, f(lru_w_a)), (1, f(lru_w_x))):
            c0 = (g * 2 + which) * 128
            bd[:, 0:64, c0:c0 + 64] = wsrc[:, 2 * g]
            bd[:, 64:128, c0 + 64:c0 + 128] = wsrc[:, 2 * g + 1]
    wp = np.ascontiguousarray(np.transpose(f(w_pool), (0, 2, 1, 3)).reshape(L, 128, 512))
    shared = dict(vecs=vecs, cst=cst, w_in=w_in_p, lru_bd=bd, w_pool=wp, w_branch=f(w_branch), w_out=f(w_out),
                  w_mem_q=f(w_mem_q), w_mem_kv=f(w_mem_kv), w_mem_o=f(w_mem_o), w_ffn_in=f(w_ffn_in),
                  w_ffn_out=f(w_ffn_out))
    xs = [np.ascontiguousarray(f(x)[b].T) for b in range(NB)]
    ms = [np.ascontiguousarray(f(mem)[b].T) for b in range(NB)]
    return shared, xs, ms


N_LAUNCH = 1


def kernel(**inputs):
    shared, xs, ms = prep_inputs(**inputs)
    lpl = DEPTH // N_LAUNCH
    nc, _, _ = build_nc(n_layers=lpl)
    NCORE = 4
    x_cur = xs
    for part in range(N_LAUNCH):
        sh = {}
        for kk, v in shared.items():
            sh[kk] = np.ascontiguousarray(v[part * lpl:(part + 1) * lpl]) if v.ndim >= 3 else v
        vh = np.zeros_like(shared["vecs"])
        vh[:, 0:lpl * NV] = shared["vecs"][:, part * lpl * NV:(part + 1) * lpl * NV]
        sh["vecs"] = vh
        in_maps = []
        for core in range(NCORE):
            m = dict(sh)
            m["xT"] = x_cur[core % NB]
            m["memT"] = ms[core % NB]
            in_maps.append(m)
        res = run_bass_kernel_spmd(nc, in_maps, core_ids=list(range(NCORE)))
        x_cur = [np.ascontiguousarray(res.results[b]["yT"]) for b in range(NB)]
    out = np.stack([np.ascontiguousarray(x_cur[b].T) for b in range(NB)], axis=0)
    return out.astype(np.float32)
```
